# Optimizing a Trainium2 kernel written in Bass

```python
import jax, jax.numpy as jnp
from jax import lax
import numpy as np

D_MODEL = 1024
BATCH = 8
SEQ = 2048
DEPTH = 4

CHUNK = 64
POOL_WINDOWS = (2, 4, 8, 16)
POOL_GROUPS = len(POOL_WINDOWS)
POOL_WIDTH = D_MODEL // 2
POOL_GROUP_DIM = POOL_WIDTH // POOL_GROUPS
POOL_MAX_WIN = max(POOL_WINDOWS)
CONV_WIDTH = D_MODEL // 2
CONV_KERNEL = 31
EVEN_IN = POOL_WIDTH + 2 * CONV_WIDTH
EVEN_MIX = POOL_WIDTH + CONV_WIDTH
SGU_WIDTH = 2 * D_MODEL
SGU_HEADS = 8
SGU_HEAD_DIM = SGU_WIDTH // SGU_HEADS
SGU_CHUNK = 128
N_EXPERTS = 32
TOP_K = 4
D_EXPERT = D_MODEL
SWIGLU_LIMIT = 7.0
SWIGLU_ALPHA = 1.702
MOE_BLOCK = 128
DN_ALPHA = (2 * DEPTH) ** 0.25
DN_BETA = (8 * DEPTH) ** -0.25
LN_EPS = 1e-5
N_EVEN = (DEPTH + 1) // 2
N_ODD = DEPTH // 2

kernel_name = "hybrid_pool_conv_gmlp_moe_deepnorm"


def layer_norm(x, g, b):
    xf = x.astype(jnp.float32)
    mu = xf.mean(-1, keepdims=True)
    var = jnp.square(xf - mu).mean(-1, keepdims=True)
    return ((xf - mu) * lax.rsqrt(var + LN_EPS) * g + b).astype(x.dtype)


def multiscale_pool(p, w, b, scale):
    bsz, s, _ = p.shape
    groups = p.astype(jnp.float32).reshape(bsz, s, POOL_GROUPS, POOL_GROUP_DIM)
    cs = jnp.pad(jnp.cumsum(groups, axis=1), ((0, 0), (POOL_MAX_WIN, 0), (0, 0), (0, 0)))
    pos = jnp.arange(s)
    outs = []
    for g, win in enumerate(POOL_WINDOWS):
        window_sum = (cs[:, POOL_MAX_WIN:POOL_MAX_WIN + s, g]
                      - cs[:, POOL_MAX_WIN - win:POOL_MAX_WIN - win + s, g])
        count = jnp.minimum(pos + 1, win).astype(jnp.float32)[None, :, None]
        outs.append(window_sum / count - groups[:, :, g])
    pooled = jnp.stack(outs, axis=2).astype(p.dtype)
    mixed = jnp.einsum('bsgc,gcd->bsgd', pooled, w) + b
    return mixed.reshape(bsz, s, POOL_WIDTH) * scale


def conformer_conv(a, gate, w_dw, b_dw, ln_g, ln_b):
    glu = a * jax.nn.sigmoid(gate)
    padded = jnp.pad(glu, ((0, 0), (CONV_KERNEL - 1, 0), (0, 0)))
    y = lax.conv_general_dilated(
        padded, w_dw[:, None, :], window_strides=(1,), padding='VALID',
        dimension_numbers=('NWC', 'WIO', 'NWC'), feature_group_count=CONV_WIDTH) + b_dw
    return jax.nn.silu(layer_norm(y, ln_g, ln_b))


def even_mixer(x, w_in, b_in, pool_w, pool_b, pool_scale, conv_w, conv_b,
               conv_ln_g, conv_ln_b, w_out, b_out):
    h = jnp.einsum('bsd,de->bse', x, w_in) + b_in
    p, a, gate = jnp.split(h, [POOL_WIDTH, POOL_WIDTH + CONV_WIDTH], axis=-1)
    y_pool = multiscale_pool(p, pool_w, pool_b, pool_scale)
    y_conv = conformer_conv(a, gate, conv_w, conv_b, conv_ln_g, conv_ln_b)
    y = jnp.concatenate([y_pool, y_conv], axis=-1)
    return jnp.einsum('bse,ed->bsd', y, w_out) + b_out


def odd_mixer(x, w_in, b_in, ln_g, ln_b, w_s, b_s, w_out, b_out):
    bsz, s, _ = x.shape
    h = jax.nn.gelu(jnp.einsum('bsd,de->bse', x, w_in) + b_in, approximate=False)
    u, v = jnp.split(h, 2, axis=-1)
    v = layer_norm(v, ln_g, ln_b)
    v = v.reshape(bsz, s // SGU_CHUNK, SGU_CHUNK, SGU_HEADS, SGU_HEAD_DIM)
    blk = jnp.arange(SGU_CHUNK) // CHUNK
    mask = blk[:, None] >= blk[None, :]
    w_masked = jnp.where(mask[None], w_s, 0.0)
    sv = jnp.einsum('hij,bnjhc->bnihc', w_masked, v) + b_s.T[:, :, None]
    gated = u * sv.reshape(bsz, s, SGU_WIDTH)
    return jnp.einsum('bse,ed->bsd', gated, w_out) + b_out


def moe(x, router_w, router_b, w_up, b_up, w_down, b_down):
    bsz, s, d = x.shape
    tokens = x.reshape(-1, d)
    n_tok = tokens.shape[0]
    logits = (tokens @ router_w + router_b).astype(jnp.float32)
    top_vals, top_idx = lax.top_k(logits, TOP_K)
    gates = jax.nn.softmax(top_vals, axis=-1).astype(x.dtype)
    n_assign = n_tok * TOP_K
    flat_exp = top_idx.reshape(-1).astype(jnp.int32)
    flat_tok = jnp.arange(n_assign, dtype=jnp.int32) // TOP_K
    flat_gate = gates.reshape(-1)
    order = jnp.argsort(flat_exp)
    sorted_exp = flat_exp[order]
    counts = jnp.zeros(N_EXPERTS, jnp.int32).at[flat_exp].add(1)
    padded_counts = (counts + MOE_BLOCK - 1) // MOE_BLOCK * MOE_BLOCK
    start = jnp.cumsum(counts) - counts
    padded_end = jnp.cumsum(padded_counts)
    padded_start = padded_end - padded_counts
    dest = padded_start[sorted_exp] + jnp.arange(n_assign, dtype=jnp.int32) - start[sorted_exp]
    n_blocks = -(-n_assign // MOE_BLOCK) + N_EXPERTS
    n_rows = n_blocks * MOE_BLOCK
    row_tok = jnp.zeros(n_rows, jnp.int32).at[dest].set(flat_tok[order])
    row_gate = jnp.zeros(n_rows, x.dtype).at[dest].set(flat_gate[order])
    block_start = jnp.arange(n_blocks, dtype=jnp.int32) * MOE_BLOCK
    block_exp = jnp.minimum(jnp.searchsorted(padded_end, block_start, side='right'),
                            N_EXPERTS - 1)
    xs = tokens[row_tok].reshape(n_blocks, MOE_BLOCK, d)

    def expert_block(args):
        xb, e = args
        hb = xb @ w_up[e] + b_up[e]
        g, lin = jnp.split(hb, 2, axis=-1)
        g = jnp.minimum(g, SWIGLU_LIMIT)
        lin = jnp.clip(lin, -SWIGLU_LIMIT, SWIGLU_LIMIT)
        act = g * jax.nn.sigmoid(SWIGLU_ALPHA * g) * (lin + 1.0)
        return act @ w_down[e] + b_down[e]

    ys = lax.map(expert_block, (xs, block_exp)).reshape(n_rows, d)
    out = jnp.zeros_like(tokens).at[row_tok].add(ys * row_gate[:, None])
    return out.reshape(bsz, s, d)


def setup_inputs(seed: int = 0) -> dict:
    key = jax.random.key(seed)
    ks = jax.random.split(key, 29)
    f32 = jnp.float32
    nrm = lambda k, shape, sc: jax.random.normal(k, shape, f32) * sc
    ones_n = lambda k, shape: 1.0 + 0.1 * jax.random.normal(k, shape, f32)
    return {
        "x": jax.random.normal(ks[0], (BATCH, SEQ, D_MODEL), f32),
        "ev_w_in": nrm(ks[1], (N_EVEN, D_MODEL, EVEN_IN), D_MODEL ** -0.5),
        "ev_b_in": nrm(ks[2], (N_EVEN, EVEN_IN), 0.02),
        "pool_w": nrm(ks[3], (N_EVEN, POOL_GROUPS, POOL_GROUP_DIM, POOL_GROUP_DIM), POOL_GROUP_DIM ** -0.5),
        "pool_b": nrm(ks[4], (N_EVEN, POOL_GROUPS, POOL_GROUP_DIM), 0.02),
        "pool_scale": ones_n(ks[5], (N_EVEN, POOL_WIDTH)),
        "conv_w": nrm(ks[6], (N_EVEN, CONV_KERNEL, CONV_WIDTH), CONV_KERNEL ** -0.5),
        "conv_b": nrm(ks[7], (N_EVEN, CONV_WIDTH), 0.02),
        "conv_ln_g": ones_n(ks[8], (N_EVEN, CONV_WIDTH)),
        "conv_ln_b": nrm(ks[9], (N_EVEN, CONV_WIDTH), 0.02),
        "ev_w_out": nrm(ks[10], (N_EVEN, EVEN_MIX, D_MODEL), EVEN_MIX ** -0.5 * DN_BETA),
        "ev_b_out": nrm(ks[11], (N_EVEN, D_MODEL), 0.02),
        "od_w_in": nrm(ks[12], (N_ODD, D_MODEL, 2 * SGU_WIDTH), D_MODEL ** -0.5),
        "od_b_in": nrm(ks[13], (N_ODD, 2 * SGU_WIDTH), 0.02),
        "sgu_ln_g": ones_n(ks[14], (N_ODD, SGU_WIDTH)),
        "sgu_ln_b": nrm(ks[15], (N_ODD, SGU_WIDTH), 0.02),
        "sgu_w": nrm(ks[16], (N_ODD, SGU_HEADS, SGU_CHUNK, SGU_CHUNK), SGU_CHUNK ** -0.5),
        "sgu_b": ones_n(ks[17], (N_ODD, SGU_HEADS, SGU_CHUNK)),
        "od_w_out": nrm(ks[18], (N_ODD, SGU_WIDTH, D_MODEL), SGU_WIDTH ** -0.5 * DN_BETA),
        "od_b_out": nrm(ks[19], (N_ODD, D_MODEL), 0.02),
        "router_w": nrm(ks[20], (DEPTH, D_MODEL, N_EXPERTS), D_MODEL ** -0.5),
        "router_b": nrm(ks[21], (DEPTH, N_EXPERTS), 0.01),
        "exp_w_up": nrm(ks[22], (DEPTH, N_EXPERTS, D_MODEL, 2 * D_EXPERT), D_MODEL ** -0.5),
        "exp_b_up": nrm(ks[23], (DEPTH, N_EXPERTS, 2 * D_EXPERT), 0.02),
        "exp_w_down": nrm(ks[24], (DEPTH, N_EXPERTS, D_EXPERT, D_MODEL), D_EXPERT ** -0.5 * DN_BETA),
        "exp_b_down": nrm(ks[25], (DEPTH, N_EXPERTS, D_MODEL), 0.02),
        "ln_g": ones_n(ks[26], (DEPTH, 2, D_MODEL)),
        "ln_b": nrm(ks[27], (DEPTH, 2, D_MODEL), 0.02),
    }


def reference(x, ev_w_in, ev_b_in, pool_w, pool_b, pool_scale, conv_w, conv_b,
              conv_ln_g, conv_ln_b, ev_w_out, ev_b_out, od_w_in, od_b_in,
              sgu_ln_g, sgu_ln_b, sgu_w, sgu_b, od_w_out, od_b_out,
              router_w, router_b, exp_w_up, exp_b_up, exp_w_down, exp_b_down,
              ln_g, ln_b):
    for layer in range(DEPTH):
        i = layer // 2
        if layer % 2 == 0:
            mix = even_mixer(x, ev_w_in[i], ev_b_in[i], pool_w[i], pool_b[i], pool_scale[i],
                             conv_w[i], conv_b[i], conv_ln_g[i], conv_ln_b[i],
                             ev_w_out[i], ev_b_out[i])
        else:
            mix = odd_mixer(x, od_w_in[i], od_b_in[i], sgu_ln_g[i], sgu_ln_b[i],
                            sgu_w[i], sgu_b[i], od_w_out[i], od_b_out[i])
        x = layer_norm(DN_ALPHA * x + mix, ln_g[layer, 0], ln_b[layer, 0])
        ff = moe(x, router_w[layer], router_b[layer], exp_w_up[layer], exp_b_up[layer],
                 exp_w_down[layer], exp_b_down[layer])
        x = layer_norm(DN_ALPHA * x + ff, ln_g[layer, 1], ln_b[layer, 1])
    return x
```

```python
from contextlib import ExitStack

import numpy as np
import concourse.bass as bass
import concourse.mybir as mybir
from concourse.bass_utils import run_bass_kernel_spmd

F32 = mybir.dt.float32
BF16 = mybir.dt.bfloat16
I32 = mybir.dt.int32
U32 = mybir.dt.uint32
AF = mybir.ActivationFunctionType
ALU = mybir.AluOpType

T = 2048
D = 1024
NT = 16
DC = 8
NE = 32
TOPK = 4
import os
CAP = int(os.environ.get("K_CAP", "384"))
NSB = CAP // 128
DEPTH = 4
ALPHA = float((2 * DEPTH) ** 0.25)
EPS = 1e-5
LIMIT = 7.0
SW_ALPHA = 1.702

EV_FM = 12 + 4 + 4 + 4 + 4 + 4 + 124
OD_FM = 16 + 16 + 16
UP_FM = NE * 16


class Sem:
    _n = 0

    def __init__(self, h):
        self.h = h
        self.cnt = 0
        Sem._n += 1
        self.id = Sem._n


class Res:
    __slots__ = ("name", "w", "r", "dsem")

    def __init__(self, name):
        self.name = name
        self.w = {}
        self.r = {}
        self.dsem = None


class Sched:
    def __init__(self, nc, es):
        self.nc = nc
        self.es = es
        self.eng = {"pe": nc.tensor, "dve": nc.vector, "act": nc.scalar, "pool": nc.gpsimd, "sp": nc.sync}
        self.esem = {}
        self.waited = {k: {} for k in self.eng}
        self.dma_pending = {}
        self.bsem = None
        self.nsem = 0
        self.free_dsems = []
        self.stage_dsems = []

    def new_sem(self, name):
        s = Sem(self.es.enter_context(self.nc.semaphore(f"{name}{self.nsem}")))
        self.nsem += 1
        return s

    def _mark(self, e, instr):
        if e not in self.esem:
            self.esem[e] = self.new_sem("e" + e)
        s = self.esem[e]
        s.cnt += 1
        instr.then_inc(s.h, 1)
        return (s, s.cnt, e)

    def wait(self, e, tok):
        if tok is None:
            return
        s, val, prod = tok
        if prod == "pe" and e == "pe":
            return
        w = self.waited[e]
        if w.get(s.id, 0) >= val:
            return
        w[s.id] = val
        self.eng[e].wait_ge(s.h, val)

    def _pre(self, e, reads, writes, extra):
        for r in reads:
            for t in r.w.values():
                self.wait(e, t)
        for w in writes:
            for t in w.w.values():
                self.wait(e, t)
            for t in w.r.values():
                self.wait(e, t)
        for t in extra:
            self.wait(e, t)

    def _post(self, tok, reads, writes):
        for r in reads:
            r.r[tok[0].id] = tok
        for w in writes:
            w.w = {tok[0].id: tok}
            w.r = {}

    def op(self, e, fn, reads=(), writes=(), extra=()):
        self._pre(e, reads, writes, extra)
        instr = fn(self.eng[e])
        tok = self._mark(e, instr)
        self._post(tok, reads, writes)
        return tok

    def dma(self, e, fn, reads=(), writes=(), extra=()):
        self._pre(e, reads, writes, extra)
        owner = writes[0] if writes else reads[0]
        if owner.dsem is None:
            owner.dsem = self.free_dsems.pop() if self.free_dsems else self.new_sem("d")
            self.stage_dsems.append(owner.dsem)
        s = owner.dsem
        instr = fn(self.eng[e])
        s.cnt += 16
        instr.then_inc(s.h, 16)
        tok = (s, s.cnt, "dma")
        self._post(tok, reads, writes)
        self.dma_pending[s.id] = tok
        return tok

    def barrier(self):
        for e, s in self.esem.items():
            if e != "sp" and s.cnt:
                self.wait("sp", (s, s.cnt, e))
        for tok in self.dma_pending.values():
            self.wait("sp", tok)
        self.dma_pending = {}
        self.free_dsems.extend(self.stage_dsems)
        self.stage_dsems = []
        if self.bsem is None:
            self.bsem = self.new_sem("bar")
        b = self.bsem
        b.cnt += 1
        self.nc.sync.sem_inc(b.h, 1)
        tok = (b, b.cnt, "sp")
        for e in self.eng:
            if e != "sp":
                self.wait(e, tok)


class Prog:
    def __init__(self, layers, ne=NE, debug=False):
        self.layers = list(layers)
        self.ne = ne
        self.debug = debug
        self.n_even = sum(1 for l in self.layers if l % 2 == 0)
        self.n_odd = sum(1 for l in self.layers if l % 2 == 1)
        self.nl = len(self.layers)

    def sb(self, es, name, shape, dt):
        self._uid += 1
        return es.enter_context(self.nc.sbuf_tensor(f"{name}_{self._uid}", list(shape), dt))

    def ps(self, es, name, shape, dt):
        self._uid += 1
        return es.enter_context(self.nc.psum_tensor(f"{name}_{self._uid}", list(shape), dt))

    def din(self, name, shape, dt=F32):
        return self.nc.dram_tensor(name, list(shape), dt, kind="ExternalInput").ap()

    def dscratch(self, name, shape, dt):
        kind = "ExternalOutput" if self.debug else "Internal"
        return self.nc.dram_tensor(name, list(shape), dt, kind=kind).ap()

    def load(self, e, out, in_, res, extra=()):
        return self.S.dma(e, lambda g: g.dma_start(out=out, in_=in_), writes=[res], extra=extra)

    def bcast_load(self, out, row_ap, res):
        return self.S.dma("sp", lambda g: g.dma_start(out=out, in_=row_ap.partition_broadcast(128)), writes=[res])

    def build(self):
        nc = bass.Bass("TRN2", target_bir_lowering=False)
        self.nc = nc
        self._uid = 0
        nl, ne = self.nl, self.ne
        n_even, n_odd = max(self.n_even, 1), max(self.n_odd, 1)
        self.x_in = self.din("x", [T, D])
        self.ev_w_in = self.din("ev_w_in", [n_even, D, 1536])
        self.ev_w_out = self.din("ev_w_out", [n_even, D, D])
        self.pool_w = self.din("pool_w", [n_even, 4, 128, 128])
        self.od_w_in = self.din("od_w_in", [n_odd, D, 4096])
        self.od_w_out = self.din("od_w_out", [n_odd, 2048, D])
        self.sgu_wT = self.din("sgu_wT", [n_odd, 8, 128, 128])
        self.od_bv = self.din("od_bv", [n_odd, 2048])
        self.sgu_b = self.din("sgu_b", [n_odd, 1024])
        self.b_out = self.din("b_out", [nl, D])
        self.ln_g = self.din("ln_g", [nl, 2, D])
        self.ln_b = self.din("ln_b", [nl, 2, D])
        self.router_w = self.din("router_w", [nl, D, ne])
        self.router_b = self.din("router_b", [nl, ne])
        self.w_up = self.din("exp_w_up", [nl, ne, D, 2 * D])
        self.w_down = self.din("exp_w_down", [nl, ne, D, D])
        self.b_down = self.din("exp_b_down", [nl, ne, D])
        self.fm_ev = self.din("fm_ev", [n_even, 128, EV_FM])
        self.fm_od = self.din("fm_od", [n_odd, 128, OD_FM])
        self.fm_up = self.din("fm_up", [nl, 128, ne * 16])
        self.consts = self.din("consts", [128, 128 * 3 + 32 + 32 + 64])
        self.y_out = nc.dram_tensor("y", [T, D], F32, kind="ExternalOutput").ap()
        self.x1_d = self.dscratch("x1_d", [T, D], F32)
        self.x2_d = self.dscratch("x2_d", [T, D], F32)
        self.xT_d = self.dscratch("xT_d", [D, T], BF16)
        self.xs_d = self.dscratch("xs_d", [ne * CAP, D], BF16)
        self.ys_d = self.dscratch("ys_d", [ne * CAP, D], F32)

        with ExitStack() as top:
            S = self.S = Sched(nc, top)
            self.bc_reg = nc.gpsimd.to_reg(ne * CAP - 1)
            self.c_f32 = self.sb(top, "c_f32", [128, 128 * 3 + 32 + 32 + 64], F32)
            self.c_bf = self.sb(top, "c_bf", [128, 128 * 3], BF16)
            self.gates_all = self.sb(top, "gates_all", [128, NT, TOPK], F32)
            self.slots_all = self.sb(top, "slots_all", [128, NT, TOPK], I32)
            self.GT_all = self.sb(top, "GT_all", [32, NT, 128], BF16)
            self.R_const = Res("const")
            self.R_route = Res("route")
            self.load("sp", self.c_f32[:], self.consts, self.R_const)
            S.dma("pool", lambda g: g.dma_start(out=self.c_bf[:], in_=self.consts[:, 0:384]), writes=[self.R_const])
            self.ident_f = self.c_f32[:, 0:128]
            self.ident_b = self.c_bf[:, 0:128]
            self.tri_b = self.c_bf[:, 128:256]
            self.ones_b = self.c_bf[:, 256:384]
            self.iota_e = self.c_f32[:, 384:416]
            self.slotbase = self.c_f32[:, 416:448]
            self.invcnt = self.c_f32[:, 448:512]
            S.barrier()

            self.stage_xT0()
            x_res = self.x_in
            for li, l in enumerate(self.layers):
                last = li == nl - 1
                if l % 2 == 0:
                    self.stage_mix_even(li, l, x_res)
                else:
                    self.stage_mix_odd(li, l, x_res)
                self.stage_router(li)
                self.stage_experts(li)
                self.stage_combine(li, self.y_out if last else self.x2_d, make_xT=not last)
                x_res = self.x2_d
            S.barrier()
        return nc

    def ln_tile(self, es_bufs, s_ap, s_res, g_bc, b_bc, out_ap, out_res, p_res):
        S = self.S
        stats, mv, sc, z, st_res, z_res = es_bufs
        for h in range(2):
            S.op("dve", lambda v, h=h: v.bn_stats(stats[:, h * 6:(h + 1) * 6], s_ap[:, h * 512:(h + 1) * 512]),
                 reads=[s_res], writes=[st_res])
        S.op("dve", lambda v: v.bn_aggr(mv[:, 0:2], stats[:, 0:12]), reads=[st_res], writes=[st_res])
        S.op("act", lambda a: a.activation(out=sc[:, 0:1], in_=mv[:, 1:2], func=AF.Sqrt, bias=self.eps_ap, scale=1.0),
             reads=[st_res, self.R_eps], writes=[st_res])
        S.op("dve", lambda v: v.reciprocal(sc[:, 1:2], sc[:, 0:1]), reads=[st_res], writes=[st_res])
        S.op("dve", lambda v: v.tensor_scalar(out=sc[:, 2:3], in0=mv[:, 0:1], scalar1=sc[:, 1:2], scalar2=-1.0,
                                              op0=ALU.mult, op1=ALU.mult), reads=[st_res], writes=[st_res])
        S.op("act", lambda a: a.activation(out=z[:], in_=s_ap, func=AF.Identity, bias=sc[:, 2:3], scale=sc[:, 1:2]),
             reads=[s_res, st_res], writes=[z_res])
        S.op("dve", lambda v: v.tensor_tensor(out=z[:], in0=z[:], in1=g_bc, op=ALU.mult),
             reads=[p_res], writes=[z_res])
        S.op("dve", lambda v: v.tensor_tensor(out=out_ap, in0=z[:], in1=b_bc, op=ALU.add),
             reads=[z_res, p_res], writes=[out_res])

    def alloc_ln_bufs(self, es, tag):
        stats = self.sb(es, "lnstats" + tag, [128, 12], F32)
        mv = self.sb(es, "lnmv" + tag, [128, 2], F32)
        sc = self.sb(es, "lnsc" + tag, [128, 4], F32)
        z = self.sb(es, "lnz" + tag, [128, D], F32)
        return (stats, mv, sc, z, Res("lnst" + tag), Res("lnz" + tag))

    def alloc_eps(self, es):
        self.eps_t = self.sb(es, "eps", [128, 1], F32)
        self.R_eps = Res("eps")
        self.S.op("dve", lambda v: v.memset(self.eps_t[:], EPS), writes=[self.R_eps])
        self.eps_ap = self.eps_t[:, 0:1]

    def xT_from_tile(self, xb, xb_res, pst, pst_res, stage, stage_res, q, eng="dve"):
        S = self.S
        for c in range(DC):
            S.op("pe", lambda p, c=c: p.transpose(out=pst[:, c * 128:(c + 1) * 128], in_=xb[:, c * 128:(c + 1) * 128],
                                                  identity=self.ident_b),
                 reads=[xb_res, self.R_const], writes=[pst_res])
        if eng == "dve":
            S.op("dve", lambda v: v.tensor_copy(out=stage[:, :, q * 128:(q + 1) * 128],
                                                in_=pst[:].rearrange("p (c t) -> p c t", c=DC)),
                 reads=[pst_res], writes=[stage_res])
        else:
            S.op("act", lambda a: a.activation(out=stage[:, :, q * 128:(q + 1) * 128],
                                               in_=pst[:].rearrange("p (c t) -> p c t", c=DC), func=AF.Identity),
                 reads=[pst_res], writes=[stage_res])

    def store_xT_block(self, stage, stage_res, blk):
        dst = self.xT_d.rearrange("(c p) t -> p c t", p=128)[:, :, blk * 512:(blk + 1) * 512]
        self.S.dma("sp", lambda g: g.dma_start(out=dst, in_=stage[:]), reads=[stage_res])

    def stage_xT0(self):
        S = self.S
        with ExitStack() as es:
            xt = [self.sb(es, "s0x", [128, D], F32) for _ in range(2)]
            xb = [self.sb(es, "s0b", [128, D], BF16) for _ in range(2)]
            stage = [self.sb(es, "s0st", [128, DC, 512], BF16) for _ in range(2)]
            pst = [self.ps(es, "s0ps", [128, D], BF16) for _ in range(2)]
            R_xt = [Res("xt") for _ in range(2)]
            R_xb = [Res("xb") for _ in range(2)]
            R_st = [Res("st") for _ in range(2)]
            R_ps = [Res("ps") for _ in range(2)]
            for i in range(NT):
                b = i % 2
                self.load("sp", xt[b][:], self.x_in[i * 128:(i + 1) * 128, :], R_xt[b])
                S.op("act", lambda a, b=b: a.activation(out=xb[b][:], in_=xt[b][:], func=AF.Identity),
                     reads=[R_xt[b]], writes=[R_xb[b]])
                sb_ = (i // 4) % 2
                self.xT_from_tile(xb[b], R_xb[b], pst[b], R_ps[b], stage[sb_], R_st[sb_], i % 4)
                if i % 4 == 3:
                    self.store_xT_block(stage[sb_], R_st[sb_], i // 4)
            S.barrier()

    def alloc_epilogue(self, es, li, n_xn=2):
        S = self.S
        ep = {}
        ep["bout"] = self.sb(es, "bout", [128, D], F32)
        ep["g"] = self.sb(es, "ln1g", [128, D], F32)
        ep["b"] = self.sb(es, "ln1b", [128, D], F32)
        ep["R_p"] = Res("ep_params")
        self.bcast_load(ep["bout"][:], self.b_out[li], ep["R_p"])
        self.bcast_load(ep["g"][:], self.ln_g[li, 0], ep["R_p"])
        self.bcast_load(ep["b"][:], self.ln_b[li, 0], ep["R_p"])
        ep["xres"] = [self.sb(es, "xres", [128, D], F32) for _ in range(2)]
        ep["R_xres"] = [Res("xres") for _ in range(2)]
        ep["s"] = self.sb(es, "eps_s", [128, D], F32)
        ep["R_s"] = Res("s")
        ep["xn"] = [self.sb(es, "xn", [128, D], F32) for _ in range(n_xn)]
        ep["R_xn"] = [Res("xn") for _ in range(n_xn)]
        ep["ln"] = self.alloc_ln_bufs(es, "m")
        return ep

    def prefetch_xres(self, ep, x_src, i):
        b = i % 2
        self.load("sp", ep["xres"][b][:], x_src[i * 128:(i + 1) * 128, :], ep["R_xres"][b])

    def mixer_epilogue(self, ep, i, mix_ps, R_mix):
        S = self.S
        b = i % 2
        xres, R_x = ep["xres"][b], ep["R_xres"][b]
        s, R_s = ep["s"], ep["R_s"]
        for h in range(2):
            S.op("dve", lambda v, h=h: v.scalar_tensor_tensor(out=s[:, h * 512:(h + 1) * 512],
                                                              in0=xres[:, h * 512:(h + 1) * 512], scalar=ALPHA,
                                                              in1=mix_ps[h][:], op0=ALU.mult, op1=ALU.add),
                 reads=[R_x, R_mix[h]], writes=[R_s])
        S.op("dve", lambda v: v.tensor_tensor(out=s[:], in0=s[:], in1=ep["bout"][:], op=ALU.add),
             reads=[ep["R_p"]], writes=[R_s])
        nx = i % len(ep["xn"])
        xn, R_xn = ep["xn"][nx], ep["R_xn"][nx]
        self.ln_tile(ep["ln"], s[:], R_s, ep["g"][:], ep["b"][:], xn[:], R_xn, ep["R_p"])
        S.dma("sp", lambda g: g.dma_start(out=self.x1_d[i * 128:(i + 1) * 128, :], in_=xn[:]), reads=[R_xn])

    def stage_mix_even(self, li, l, x_src):
        S = self.S
        ei = sum(1 for q in self.layers[:li] if q % 2 == 0)
        with ExitStack() as es:
            self.alloc_eps(es)
            w_in = self.sb(es, "ev_win", [128, DC, 1536], BF16)
            w_out = self.sb(es, "ev_wout", [128, DC, D], BF16)
            pw = self.sb(es, "ev_pw", [128, 4, 128], BF16)
            fm = self.sb(es, "ev_fm", [128, EV_FM], F32)
            diag = self.sb(es, "ev_diag", [128, 4, 31, 128], BF16)
            R_w = Res("ev_w")
            R_diag = Res("diag")
            src = self.ev_w_in[ei].rearrange("(c p) n -> p c n", p=128)
            for c0 in range(0, DC, 4):
                S.dma("pool", lambda g, c0=c0: g.dma_start(out=w_in[:, c0:c0 + 4, :], in_=src[:, c0:c0 + 4, :]), writes=[R_w])
            src2 = self.ev_w_out[ei].rearrange("(c p) n -> p c n", p=128)
            for c0 in range(0, DC, 4):
                S.dma("pool", lambda g, c0=c0: g.dma_start(out=w_out[:, c0:c0 + 4, :], in_=src2[:, c0:c0 + 4, :]), writes=[R_w])
            S.dma("pool", lambda g: g.dma_start(out=pw[:], in_=self.pool_w[ei].rearrange("g c d -> c g d")), writes=[R_w])
            self.load("sp", fm[:], self.fm_ev[ei], R_w)
            b_in = fm[:, 0:12]
            pool_b = fm[:, 12:16]
            pool_s = fm[:, 16:20]
            conv_b = fm[:, 20:24]
            cln_g = fm[:, 24:28]
            cln_b = fm[:, 28:32]
            conv_w = fm[:, 32:156]
            for j in range(4):
                for k in range(31):
                    S.op("dve", lambda v, j=j, k=k: v.tensor_scalar(out=diag[:, j, k, :], in0=self.ident_b,
                                                                    scalar1=conv_w[:, j * 31 + k:j * 31 + k + 1],
                                                                    scalar2=None, op0=ALU.mult),
                         reads=[R_w, self.R_const], writes=[R_diag])
            ep = self.alloc_epilogue(es, li)
            xTb = [self.sb(es, "ev_xT", [128, DC, 512], BF16) for _ in range(2)]
            R_xTb = [Res("xTb") for _ in range(2)]
            pbuf = [self.sb(es, "ev_p", [128, 4, 528], F32) for _ in range(2)]
            R_pbuf = [Res("pbuf") for _ in range(2)]
            R_phalo = [Res("phalo") for _ in range(2)]
            glu = [self.sb(es, "ev_glu", [128, 4, 544], BF16) for _ in range(2)]
            R_glu = [Res("glu") for _ in range(2)]
            R_ghalo = [Res("ghalo") for _ in range(2)]
            tA = self.sb(es, "ev_tA", [128, 528], F32)
            tB = self.sb(es, "ev_tB", [128, 528], F32)
            R_tA, R_tB = Res("tA"), Res("tB")
            t16 = self.sb(es, "ev_t16", [128, 16], F32)
            R_t16 = Res("t16")
            pooled = self.sb(es, "ev_pooled", [128, 4, 512], BF16)
            R_pooled = [Res("pooled") for _ in range(4)]
            ymix = self.sb(es, "ev_ymix", [128, 8, 512], BF16)
            R_ymix = [Res("ymix") for _ in range(8)]
            sg = [self.sb(es, "ev_sg", [128, 512], F32) for _ in range(2)]
            R_sg = [Res("sg") for _ in range(2)]
            yc = self.sb(es, "ev_yc", [128, 4, 512], F32)
            ycb = self.sb(es, "ev_ycb", [128, 4, 512], BF16)
            ysq = self.sb(es, "ev_ysq", [128, 4, 512], BF16)
            R_yc = [Res("yc") for _ in range(4)]
            mean = self.sb(es, "ev_mean", [128, 512], F32)
            msq = self.sb(es, "ev_msq", [128, 512], F32)
            rstd = self.sb(es, "ev_rstd", [128, 512], F32)
            R_stat = Res("stat")
            t1 = [self.sb(es, "ev_t1", [128, 512], F32) for _ in range(2)]
            R_t1 = [Res("t1") for _ in range(2)]
            A = [self.ps(es, "ev_A", [128, 512], F32) for _ in range(4)]
            R_A = [Res("A") for _ in range(4)]
            S1 = self.ps(es, "ev_S1", [128, 512], F32)
            S2 = self.ps(es, "ev_S2", [128, 512], F32)
            R_S1, R_S2 = Res("S1"), Res("S2")
            M = [self.ps(es, "ev_M", [128, 512], F32) for _ in range(2)]
            R_M = [Res("M") for _ in range(2)]
            rot = [0]

            def nextA():
                k = rot[0] % 4
                rot[0] += 1
                return A[k], R_A[k]

            S.op("dve", lambda v: v.memset(pbuf[0][:, :, 0:16], 0.0), writes=[R_phalo[0]])
            S.op("dve", lambda v: v.memset(glu[0][:, :, 0:32], 0.0), writes=[R_ghalo[0]])
            xT_src = self.xT_d.rearrange("(c p) t -> p c t", p=128)
            self.load("sp", xTb[0][:], xT_src[:, :, 0:512], R_xTb[0])
            self.prefetch_xres(ep, x_src, 0)
            for tb in range(4):
                cb = tb % 2
                if tb + 1 < 4:
                    self.load("sp", xTb[1 - cb][:], xT_src[:, :, (tb + 1) * 512:(tb + 2) * 512], R_xTb[1 - cb])
                xt = xTb[cb]
                for j in range(4):
                    ps_, R_ps = nextA()
                    for c in range(DC):
                        S.op("pe", lambda p, c=c, j=j, ps_=ps_: p.matmul(ps_[:], lhsT=w_in[:, c, j * 128:(j + 1) * 128],
                                                                           rhs=xt[:, c, :], start=(c == 0), stop=(c == DC - 1)),
                             reads=[R_w, R_xTb[cb]], writes=[R_ps])
                    S.op("act", lambda a, j=j, ps_=ps_: a.activation(out=pbuf[cb][:, j, 16:528], in_=ps_[:], func=AF.Identity,
                                                                     bias=b_in[:, j:j + 1], scale=1.0),
                         reads=[R_ps, R_w], writes=[R_pbuf[cb]])
                for j in range(4):
                    psa, R_psa = nextA()
                    psg, R_psg = nextA()
                    for c in range(DC):
                        S.op("pe", lambda p, c=c, j=j, psa=psa: p.matmul(psa[:], lhsT=w_in[:, c, (4 + j) * 128:(5 + j) * 128],
                                                                           rhs=xt[:, c, :], start=(c == 0), stop=(c == DC - 1)),
                             reads=[R_w, R_xTb[cb]], writes=[R_psa])
                    for c in range(DC):
                        S.op("pe", lambda p, c=c, j=j, psg=psg: p.matmul(psg[:], lhsT=w_in[:, c, (8 + j) * 128:(9 + j) * 128],
                                                                           rhs=xt[:, c, :], start=(c == 0), stop=(c == DC - 1)),
                             reads=[R_w, R_xTb[cb]], writes=[R_psg])
                    sgb = sg[j % 2]
                    S.op("act", lambda a, j=j, psg=psg, sgb=sgb: a.activation(out=sgb[:], in_=psg[:], func=AF.Sigmoid,
                                                                               bias=b_in[:, 8 + j:9 + j], scale=1.0),
                         reads=[R_psg, R_w], writes=[R_sg[j % 2]])
                    S.op("dve", lambda v, j=j, psa=psa, sgb=sgb: v.scalar_tensor_tensor(
                        out=glu[cb][:, j, 32:544], in0=psa[:], scalar=b_in[:, 4 + j:5 + j], in1=sgb[:],
                        op0=ALU.add, op1=ALU.mult), reads=[R_psa, R_sg[j % 2], R_w], writes=[R_glu[cb]])
                for g in range(4):
                    win = 2 ** (g + 1)
                    cur, R_cur = pbuf[cb][:, g, :], None
                    bufs = [(tA, R_tA), (tB, R_tB)]
                    for s_ in range(g + 1):
                        sh = 2 ** s_
                        nxt, R_nxt = bufs[s_ % 2]
                        rd = [R_pbuf[cb], R_phalo[cb]] if R_cur is None else [R_cur]
                        S.op("dve", lambda v, cur=cur, nxt=nxt, sh=sh: v.tensor_tensor(
                            out=nxt[:, sh:528], in0=cur[:, sh:528], in1=cur[:, 0:528 - sh], op=ALU.add),
                            reads=rd, writes=[R_nxt])
                        cur, R_cur = nxt[:, :], R_nxt
                    S.op("dve", lambda v, cur=cur, g=g, win=win: v.scalar_tensor_tensor(
                        out=pooled[:, g, :], in0=cur[:, 16:528], scalar=1.0 / win, in1=pbuf[cb][:, g, 16:528],
                        op0=ALU.mult, op1=ALU.subtract), reads=[R_cur, R_pbuf[cb]], writes=[R_pooled[g]])
                    if tb == 0:
                        S.op("dve", lambda v, cur=cur, g=g: v.tensor_tensor(out=t16[:], in0=cur[:, 16:32],
                                                                           in1=self.invcnt[:, g * 16:(g + 1) * 16], op=ALU.mult),
                             reads=[R_cur, self.R_const], writes=[R_t16])
                        S.op("dve", lambda v, g=g: v.tensor_tensor(out=pooled[:, g, 0:16], in0=t16[:],
                                                                   in1=pbuf[cb][:, g, 16:32], op=ALU.subtract),
                             reads=[R_t16, R_pbuf[cb]], writes=[R_pooled[g]])
                if tb + 1 < 4:
                    S.op("act", lambda a: a.activation(out=pbuf[1 - cb][:, :, 0:16], in_=pbuf[cb][:, :, 512:528], func=AF.Identity),
                         reads=[R_pbuf[cb]], writes=[R_phalo[1 - cb]])
                    S.op("act", lambda a: a.activation(out=glu[1 - cb][:, :, 0:32], in_=glu[cb][:, :, 512:544], func=AF.Identity),
                         reads=[R_glu[cb]], writes=[R_ghalo[1 - cb]])
                for g in range(4):
                    ps_, R_ps = nextA()
                    S.op("pe", lambda p, g=g, ps_=ps_: p.matmul(ps_[:], lhsT=pw[:, g, :], rhs=pooled[:, g, :], start=True, stop=True),
                         reads=[R_w, R_pooled[g]], writes=[R_ps])
                    S.op("dve", lambda v, g=g, ps_=ps_: v.tensor_scalar(out=ymix[:, g, :], in0=ps_[:], scalar1=pool_b[:, g:g + 1],
                                                                        scalar2=pool_s[:, g:g + 1], op0=ALU.add, op1=ALU.mult),
                         reads=[R_ps, R_w], writes=[R_ymix[g]])
                for j in range(4):
                    ps_, R_ps = nextA()
                    for k in range(31):
                        S.op("pe", lambda p, j=j, k=k, ps_=ps_: p.matmul(ps_[:], lhsT=diag[:, j, k, :],
                                                                           rhs=glu[cb][:, j, 2 + k:2 + k + 512],
                                                                           start=(k == 0), stop=(k == 30)),
                             reads=[R_diag, R_glu[cb], R_ghalo[cb]], writes=[R_ps])
                    S.op("act", lambda a, j=j, ps_=ps_: a.activation(out=yc[:, j, :], in_=ps_[:], func=AF.Identity,
                                                                     bias=conv_b[:, j:j + 1], scale=1.0),
                         reads=[R_ps, R_w], writes=[R_yc[j]])
                    S.op("act", lambda a, j=j, ps_=ps_: a.activation(out=ysq[:, j, :], in_=ps_[:], func=AF.Square,
                                                                     bias=conv_b[:, j:j + 1], scale=1.0),
                         reads=[R_ps, R_w], writes=[R_yc[j]])
                    S.op("dve", lambda v, j=j: v.tensor_copy(out=ycb[:, j, :], in_=yc[:, j, :]), reads=[R_yc[j]], writes=[R_yc[j]])
                for j in range(4):
                    S.op("pe", lambda p, j=j: p.matmul(S1[:], lhsT=self.ones_b, rhs=ycb[:, j, :], start=(j == 0), stop=(j == 3)),
                         reads=[R_yc[j], self.R_const], writes=[R_S1])
                for j in range(4):
                    S.op("pe", lambda p, j=j: p.matmul(S2[:], lhsT=self.ones_b, rhs=ysq[:, j, :], start=(j == 0), stop=(j == 3)),
                         reads=[R_yc[j], self.R_const], writes=[R_S2])
                S.op("act", lambda a: a.activation(out=mean[:], in_=S1[:], func=AF.Identity, scale=1.0 / 512), reads=[R_S1], writes=[R_stat])
                S.op("act", lambda a: a.activation(out=msq[:], in_=S1[:], func=AF.Square, scale=1.0 / 512), reads=[R_S1], writes=[R_stat])
                S.op("dve", lambda v: v.scalar_tensor_tensor(out=rstd[:], in0=S2[:], scalar=1.0 / 512, in1=msq[:],
                                                             op0=ALU.mult, op1=ALU.subtract), reads=[R_S2, R_stat], writes=[R_stat])
                S.op("act", lambda a: a.activation(out=rstd[:], in_=rstd[:], func=AF.Ln, bias=self.eps_ap, scale=1.0),
                     reads=[self.R_eps], writes=[R_stat])
                S.op("act", lambda a: a.activation(out=rstd[:], in_=rstd[:], func=AF.Exp, scale=-0.5), writes=[R_stat])
                for j in range(4):
                    tt_, R_tt = t1[j % 2], R_t1[j % 2]
                    S.op("dve", lambda v, j=j, tt_=tt_: v.tensor_tensor(out=tt_[:], in0=yc[:, j, :], in1=mean[:], op=ALU.subtract),
                         reads=[R_yc[j], R_stat], writes=[R_tt])
                    S.op("dve", lambda v, tt_=tt_: v.tensor_tensor(out=tt_[:], in0=tt_[:], in1=rstd[:], op=ALU.mult),
                         reads=[R_stat], writes=[R_tt])
                    S.op("act", lambda a, j=j, tt_=tt_: a.activation(out=ymix[:, 4 + j, :], in_=tt_[:], func=AF.Silu,
                                                                     bias=cln_b[:, j:j + 1], scale=cln_g[:, j:j + 1]),
                         reads=[R_tt, R_w], writes=[R_ymix[4 + j]])
                for q in range(4):
                    i = tb * 4 + q
                    if i + 1 < NT:
                        self.prefetch_xres(ep, x_src, i + 1)
                    for h in range(2):
                        for e_ in range(8):
                            S.op("pe", lambda p, e_=e_, h=h, q=q: p.matmul(M[h][:], lhsT=ymix[:, e_, q * 128:(q + 1) * 128],
                                                                            rhs=w_out[:, e_, h * 512:(h + 1) * 512],
                                                                            start=(e_ == 0), stop=(e_ == 7)),
                                 reads=[R_ymix[e_], R_w], writes=[R_M[h]])
                    self.mixer_epilogue(ep, i, M, R_M)
            S.barrier()

    def stage_mix_odd(self, li, l, x_src):
        S = self.S
        oi = sum(1 for q in self.layers[:li] if q % 2 == 1)
        with ExitStack() as es:
            self.alloc_eps(es)
            w_in = self.sb(es, "od_win", [128, DC, 4096], BF16)
            w_out = self.sb(es, "od_wout", [128, 16, D], BF16)
            wT = self.sb(es, "od_wT", [128, 8, 128], BF16)
            fm = self.sb(es, "od_fm", [128, OD_FM], F32)
            bv = self.sb(es, "od_bv", [128, 2048], F32)
            bs = self.sb(es, "od_bs", [128, 1024], F32)
            Kc = self.sb(es, "od_Kc", [128, 16, 128], F32)
            R_w = Res("od_w")
            R_wT = Res("od_wT")
            R_Kc = Res("Kc")
            src = self.od_w_in[oi].rearrange("(c p) n -> p c n", p=128)
            for n0 in range(0, 4096, 1024):
                S.dma("pool", lambda g, n0=n0: g.dma_start(out=w_in[:, :, n0:n0 + 1024], in_=src[:, :, n0:n0 + 1024]), writes=[R_w])
            src2 = self.od_w_out[oi].rearrange("(c p) n -> p c n", p=128)
            for c0 in range(0, 16, 8):
                S.dma("pool", lambda g, c0=c0: g.dma_start(out=w_out[:, c0:c0 + 8, :], in_=src2[:, c0:c0 + 8, :]), writes=[R_w])
            S.dma("pool", lambda g: g.dma_start(out=wT[:], in_=self.sgu_wT[oi].rearrange("h j i -> j h i")), writes=[R_wT])
            self.load("sp", fm[:], self.fm_od[oi], R_w)
            self.bcast_load(bv[:], self.od_bv[oi], R_w)
            self.bcast_load(bs[:], self.sgu_b[oi], R_w)
            b_u = fm[:, 0:16]
            g_fm = fm[:, 16:32]
            b_fm = fm[:, 32:48]
            S.op("dve", lambda v: v.memset(wT[64:128, :, 0:64], 0.0), writes=[R_wT])
            ep = self.alloc_epilogue(es, li, n_xn=1)
            xTb = [self.sb(es, "od_xT", [128, DC, 256], BF16) for _ in range(2)]
            R_xTb = [Res("xTb") for _ in range(2)]
            u = [self.sb(es, "od_u", [128, 16, 256], BF16) for _ in range(2)]
            R_u = [[Res("u") for _ in range(16)] for _ in range(2)]
            t1 = [self.sb(es, "od_t1", [128, 2048], F32) for _ in range(2)]
            R_t1 = [Res("t1") for _ in range(2)]
            z = [self.sb(es, "od_z", [128, 2048], BF16) for _ in range(2)]
            R_z = [Res("z") for _ in range(2)]
            vst = [self.sb(es, "od_vst", [128, 24], F32) for _ in range(2)]
            vmv = [self.sb(es, "od_vmv", [128, 2], F32) for _ in range(2)]
            vsc = [self.sb(es, "od_vsc", [128, 4], F32) for _ in range(2)]
            R_vst = [Res("vst") for _ in range(2)]
            tmp = [self.sb(es, "od_tmp", [128, 128], F32) for _ in range(2)]
            R_tmp = [Res("tmp") for _ in range(2)]
            PU = [self.ps(es, "od_PU", [128, 512], F32) for _ in range(2)]
            R_PU = [Res("PU") for _ in range(2)]
            PV = [self.ps(es, "od_PV", [128, 512], F32) for _ in range(2)]
            R_PV = [Res("PV") for _ in range(2)]
            PR = [self.ps(es, "od_PR", [128, 512], F32) for _ in range(2)]
            R_PR = [Res("PR") for _ in range(2)]
            M = [self.ps(es, "od_M", [128, 512], F32) for _ in range(2)]
            R_M = [Res("M") for _ in range(2)]
            for hh in range(2):
                S.op("pe", lambda p, hh=hh: p.matmul(PR[hh][:], lhsT=self.ones_b,
                                                     rhs=wT[:, hh * 4:(hh + 1) * 4, :],
                                                     start=True, stop=True), reads=[R_wT, self.R_const], writes=[R_PR[hh]])
            for blk in range(16):
                h = blk // 2
                S.op("dve", lambda v, blk=blk, h=h: v.scalar_tensor_tensor(
                    out=Kc[:, blk, :], in0=PR[h // 4][:, (h % 4) * 128:(h % 4 + 1) * 128], scalar=b_fm[:, blk:blk + 1],
                    in1=bs[:, h * 128:(h + 1) * 128], op0=ALU.mult, op1=ALU.add),
                    reads=[R_PR[h // 4], R_w], writes=[R_Kc])
            xT_src = self.xT_d.rearrange("(c p) t -> p c t", p=128)
            self.load("sp", xTb[0][:], xT_src[:, :, 0:256], R_xTb[0])
            self.prefetch_xres(ep, x_src, 0)
            rot = {"u": 0, "v": 0}

            def U(b):
                cb = b % 2
                xt, ub = xTb[cb], u[cb]
                for ec in range(16):
                    ps_, R_ps = PU[rot["u"] % 2], R_PU[rot["u"] % 2]
                    rot["u"] += 1
                    for c in range(DC):
                        S.op("pe", lambda p, c=c, ec=ec, ps_=ps_: p.matmul(ps_[:, 0:256], lhsT=w_in[:, c, ec * 128:(ec + 1) * 128],
                                                                             rhs=xt[:, c, :], start=(c == 0), stop=(c == DC - 1)),
                             reads=[R_w, R_xTb[cb]], writes=[R_ps])
                    S.op("act", lambda a, ec=ec, ps_=ps_: a.activation(out=ub[:, ec, :], in_=ps_[:, 0:256], func=AF.Gelu,
                                                                       bias=b_u[:, ec:ec + 1], scale=1.0),
                         reads=[R_ps, R_w], writes=[R_u[cb][ec]])

            def A(i):
                b, q = divmod(i, 2)
                cb = b % 2
                xt = xTb[cb]
                pi = i % 2
                zb, R_zb = z[pi], R_z[pi]
                t1b, R_t1b = t1[pi], R_t1[pi]
                vst_, vmv_, vsc_, R_vs = vst[pi], vmv[pi], vsc[pi], R_vst[pi]
                for vb in range(4):
                    ps_, R_ps = PV[rot["v"] % 2], R_PV[rot["v"] % 2]
                    rot["v"] += 1
                    for c in range(DC):
                        S.op("pe", lambda p, c=c, vb=vb, ps_=ps_, q=q: p.matmul(
                            ps_[:], lhsT=xt[:, c, q * 128:(q + 1) * 128],
                            rhs=w_in[:, c, 2048 + vb * 512:2048 + (vb + 1) * 512],
                            start=(c == 0), stop=(c == DC - 1)), reads=[R_w, R_xTb[cb]], writes=[R_ps])
                    S.op("dve", lambda v, vb=vb, ps_=ps_: v.tensor_tensor(out=t1b[:, vb * 512:(vb + 1) * 512], in0=ps_[:],
                                                                          in1=bv[:, vb * 512:(vb + 1) * 512], op=ALU.add),
                         reads=[R_ps, R_w], writes=[R_t1b])
                S.op("act", lambda a: a.activation(out=t1b[:], in_=t1b[:], func=AF.Gelu), writes=[R_t1b])
                for vb in range(4):
                    S.op("dve", lambda v, vb=vb: v.bn_stats(vst_[:, vb * 6:(vb + 1) * 6], t1b[:, vb * 512:(vb + 1) * 512]),
                         reads=[R_t1b], writes=[R_vs])
                S.op("dve", lambda v: v.bn_aggr(vmv_[:, 0:2], vst_[:, 0:24]), writes=[R_vs])
                S.op("act", lambda a: a.activation(out=vsc_[:, 0:1], in_=vmv_[:, 1:2], func=AF.Sqrt, bias=self.eps_ap, scale=1.0),
                     reads=[self.R_eps], writes=[R_vs])
                S.op("dve", lambda v: v.reciprocal(vsc_[:, 1:2], vsc_[:, 0:1]), writes=[R_vs])
                S.op("dve", lambda v: v.tensor_scalar(out=vsc_[:, 2:3], in0=vmv_[:, 0:1], scalar1=vsc_[:, 1:2], scalar2=-1.0,
                                                      op0=ALU.mult, op1=ALU.mult), writes=[R_vs])
                S.op("act", lambda a: a.activation(out=zb[:], in_=t1b[:], func=AF.Identity, bias=vsc_[:, 2:3], scale=vsc_[:, 1:2]),
                     reads=[R_t1b, R_vs], writes=[R_zb])

            def B(i):
                b, q = divmod(i, 2)
                cb = b % 2
                ub = u[cb]
                zb, R_zb = z[i % 2], R_z[i % 2]
                if i + 1 < NT:
                    self.prefetch_xres(ep, x_src, i + 1)
                for blk in range(16):
                    pr, R_pr = PR[(blk // 4) % 2], R_PR[(blk // 4) % 2]
                    S.op("pe", lambda p, blk=blk, pr=pr: p.matmul(pr[:, (blk % 4) * 128:(blk % 4 + 1) * 128],
                                                                  lhsT=zb[:, blk * 128:(blk + 1) * 128],
                                                                  rhs=wT[:, blk // 2, :], start=True, stop=True),
                         reads=[R_zb, R_wT], writes=[R_pr])
                    if blk % 4 == 3:
                        for b2 in range(blk - 3, blk + 1):
                            tm, R_tm = tmp[b2 % 2], R_tmp[b2 % 2]
                            S.op("dve", lambda v, b2=b2, pr=pr, tm=tm: v.scalar_tensor_tensor(
                                out=tm[:], in0=pr[:, (b2 % 4) * 128:(b2 % 4 + 1) * 128], scalar=g_fm[:, b2:b2 + 1],
                                in1=Kc[:, b2, :], op0=ALU.mult, op1=ALU.add), reads=[R_pr, R_Kc, R_w], writes=[R_tm])
                            S.op("dve", lambda v, b2=b2, tm=tm: v.tensor_tensor(
                                out=ub[:, b2, q * 128:(q + 1) * 128], in0=tm[:], in1=ub[:, b2, q * 128:(q + 1) * 128],
                                op=ALU.mult), reads=[R_tm], writes=[R_u[cb][b2]])
                for h in range(2):
                    for ec in range(16):
                        S.op("pe", lambda p, ec=ec, h=h: p.matmul(M[h][:], lhsT=ub[:, ec, q * 128:(q + 1) * 128],
                                                                  rhs=w_out[:, ec, h * 512:(h + 1) * 512],
                                                                  start=(ec == 0), stop=(ec == 15)),
                             reads=[R_u[cb][ec], R_w], writes=[R_M[h]])
                self.mixer_epilogue(ep, i, M, R_M)

            for i in range(NT):
                b, q = divmod(i, 2)
                if q == 0:
                    if b + 1 < 8:
                        nb_ = (b + 1) % 2
                        self.load("sp", xTb[nb_][:], xT_src[:, :, (b + 1) * 256:(b + 2) * 256], R_xTb[nb_])
                    U(b)
                A(i)
                if i >= 1:
                    B(i - 1)
            B(NT - 1)
            S.barrier()

    def stage_router(self, li):
        S = self.S
        ne = self.ne
        with ExitStack() as es:
            rw = self.sb(es, "rw", [128, DC, ne], F32)
            rb = self.sb(es, "rb", [128, ne], F32)
            R_p = Res("rp")
            self.load("sp", rw[:], self.router_w[li].rearrange("(c p) e -> p c e", p=128), R_p)
            self.bcast_load(rb[:], self.router_b[li], R_p)
            xn = [self.sb(es, "r_xn", [128, D], F32) for _ in range(2)]
            R_xn = [Res("xn") for _ in range(2)]
            xb = [self.sb(es, "r_xb", [128, D], BF16) for _ in range(2)]
            R_xb = [Res("xb") for _ in range(2)]
            xTf = self.sb(es, "r_xTf", [128, DC, 128], F32)
            R_xTf = [Res("xTf") for _ in range(2)]
            lg = self.sb(es, "r_lg", [128, ne], F32)
            mx = self.sb(es, "r_mx", [128, 8], F32)
            ix = self.sb(es, "r_ix", [128, 8], U32)
            ixf = self.sb(es, "r_ixf", [128, 8], F32)
            sm = self.sb(es, "r_sm", [128, 8], F32)
            e4 = self.sb(es, "r_e4", [128, 4], F32)
            mask = self.sb(es, "r_mask", [128, ne], BF16)
            cm = self.sb(es, "r_cm", [128, ne], BF16)
            slotfull = self.sb(es, "r_sf", [128, ne], F32)
            ovf = self.sb(es, "r_ovf", [128, ne], F32)
            oh = self.sb(es, "r_oh", [128, ne], F32)
            junk = self.sb(es, "r_junk", [128, ne], F32)
            slotf = self.sb(es, "r_slotf", [128, 4], F32)
            G = self.sb(es, "r_G", [128, ne], F32)
            R_t = Res("rt")
            R_cm = Res("cm")
            PT = [self.ps(es, "r_PT", [128, 512], F32) for _ in range(2)]
            R_PT = [Res("PT") for _ in range(2)]
            PL = self.ps(es, "r_PL", [128, ne], F32)
            PP = self.ps(es, "r_PP", [128, ne], F32)
            PG = self.ps(es, "r_PG", [32, 128], F32)
            R_PL, R_PP, R_PG = Res("PL"), Res("PP"), Res("PG")
            S.op("dve", lambda v: v.memset(cm[:], 0.0), writes=[R_cm])
            self.load("sp", xn[0][:], self.x1_d[0:128, :], R_xn[0])
            scat_toks = []
            for i in range(NT):
                b = i % 2
                if i + 1 < NT:
                    self.load("sp", xn[1 - b][:], self.x1_d[(i + 1) * 128:(i + 2) * 128, :], R_xn[1 - b])
                S.op("act", lambda a, b=b: a.activation(out=xb[b][:], in_=xn[b][:], func=AF.Identity), reads=[R_xn[b]], writes=[R_xb[b]])
                for half in range(2):
                    for cc in range(4):
                        c = half * 4 + cc
                        S.op("pe", lambda p, c=c, cc=cc, half=half, b=b: p.transpose(
                            out=PT[half][:, cc * 128:(cc + 1) * 128], in_=xn[b][:, c * 128:(c + 1) * 128], identity=self.ident_f),
                            reads=[R_xn[b], self.R_const], writes=[R_PT[half]])
                    S.op("dve", lambda v, half=half: v.tensor_copy(out=xTf[:, half * 4:(half + 1) * 4, :],
                                                                    in_=PT[half][:].rearrange("p (c t) -> p c t", c=4)),
                         reads=[R_PT[half]], writes=[R_xTf[half]])
                for c in range(DC):
                    S.op("pe", lambda p, c=c: p.matmul(PL[:], lhsT=xTf[:, c, :], rhs=rw[:, c, :], start=(c == 0), stop=(c == DC - 1)),
                         reads=[R_xTf[c // 4], R_p], writes=[R_PL])
                S.op("dve", lambda v: v.tensor_tensor(out=lg[:], in0=PL[:], in1=rb[:], op=ALU.add), reads=[R_PL, R_p], writes=[R_t])
                S.op("dve", lambda v: v.max(out=mx[:], in_=lg[:]), writes=[R_t])
                S.op("dve", lambda v: v.max_index(out=ix[:], in_max=mx[:], in_values=lg[:]), writes=[R_t])
                S.op("dve", lambda v: v.tensor_scalar(out=sm[:, 0:1], in0=mx[:, 0:1], scalar1=-1.0, scalar2=None, op0=ALU.mult), writes=[R_t])
                S.op("act", lambda a: a.activation(out=e4[:], in_=mx[:, 0:4], func=AF.Exp, bias=sm[:, 0:1], scale=1.0,
                                                   accum_out=sm[:, 1:2]), writes=[R_t])
                S.op("dve", lambda v: v.reciprocal(sm[:, 2:3], sm[:, 1:2]), writes=[R_t])
                S.op("dve", lambda v, i=i: v.tensor_scalar(out=self.gates_all[:, i, :], in0=e4[:], scalar1=sm[:, 2:3], scalar2=None,
                                                           op0=ALU.mult), reads=[R_t], writes=[self.R_route])
                S.op("dve", lambda v: v.tensor_scalar(out=mask[:], in0=lg[:], scalar1=mx[:, 3:4], scalar2=None, op0=ALU.is_ge), writes=[R_t])
                S.op("pe", lambda p: p.matmul(PP[:], lhsT=self.ones_b, rhs=cm[:], start=True, stop=False),
                     reads=[R_cm, self.R_const], writes=[R_PP])
                S.op("pe", lambda p: p.matmul(PP[:], lhsT=self.tri_b, rhs=mask[:], start=False, stop=True),
                     reads=[R_t, self.R_const], writes=[R_PP])
                S.op("dve", lambda v: v.tensor_tensor(out=cm[:], in0=cm[:], in1=mask[:], op=ALU.add), reads=[R_t], writes=[R_cm])
                S.op("dve", lambda v: v.tensor_tensor(out=slotfull[:], in0=PP[:], in1=self.slotbase[:, 0:ne], op=ALU.add),
                     reads=[R_PP, self.R_const], writes=[R_t])
                S.op("dve", lambda v: v.tensor_scalar(out=ovf[:], in0=PP[:], scalar1=float(CAP), scalar2=1.0e7, op0=ALU.is_ge, op1=ALU.mult),
                     reads=[R_PP], writes=[R_t])
                S.op("dve", lambda v: v.tensor_tensor(out=slotfull[:], in0=slotfull[:], in1=ovf[:], op=ALU.add), writes=[R_t])
                S.op("dve", lambda v: v.tensor_copy(out=ixf[:], in_=ix[:]), writes=[R_t])
                for k in range(TOPK):
                    S.op("dve", lambda v, k=k: v.tensor_scalar(out=oh[:], in0=self.iota_e[:, 0:ne], scalar1=ixf[:, k:k + 1], scalar2=None,
                                                               op0=ALU.is_equal), reads=[self.R_const], writes=[R_t])
                    S.op("dve", lambda v, k=k: v.tensor_tensor(out=junk[:], in0=oh[:], in1=slotfull[:], op=ALU.mult), writes=[R_t])
                    S.op("dve", lambda v, k=k: v.tensor_reduce(out=slotf[:, k:k + 1], in_=junk[:], axis=mybir.AxisListType.X, op=ALU.add),
                         writes=[R_t])
                    if k == 0:
                        S.op("dve", lambda v, i=i: v.tensor_scalar(out=G[:], in0=oh[:], scalar1=self.gates_all[:, i, 0:1], scalar2=None,
                                                                   op0=ALU.mult), reads=[self.R_route], writes=[R_t])
                    else:
                        S.op("dve", lambda v, i=i, k=k: v.scalar_tensor_tensor(out=G[:], in0=oh[:], scalar=self.gates_all[:, i, k:k + 1],
                                                                               in1=G[:], op0=ALU.mult, op1=ALU.add),
                             reads=[self.R_route], writes=[R_t])
                tok_sl = S.op("dve", lambda v, i=i: v.tensor_copy(out=self.slots_all[:, i, :], in_=slotf[:]), reads=[R_t], writes=[self.R_route])
                S.op("pe", lambda p: p.transpose(out=PG[:], in_=G[:], identity=self.ident_f), reads=[R_t, self.R_const], writes=[R_PG])
                S.op("act", lambda a, i=i: a.activation(out=self.GT_all[:, i, :], in_=PG[:], func=AF.Identity), reads=[R_PG], writes=[self.R_route])
                for k in range(TOPK):
                    t_ = S.dma("pool", lambda g, i=i, k=k, b=b: g.indirect_dma_start(
                        out=self.xs_d[:, :], out_offset=bass.IndirectOffsetOnAxis(ap=self.slots_all[:, i, k:k + 1], axis=0),
                        in_=xb[b][:, :], in_offset=None, bounds_check=self.bc_reg, oob_is_err=False),
                        reads=[R_xb[b]], extra=[tok_sl])
                    scat_toks.append(t_)
            S.barrier()

    def stage_experts(self, li):
        S = self.S
        ne = self.ne
        NSLOT = 6
        with ExitStack() as es:
            ring = [self.sb(es, "ring", [128, DC, 1024], BF16) for _ in range(NSLOT)]
            R_ring = [Res("ring") for _ in range(NSLOT)]
            bup = self.sb(es, "bup", [128, ne * 16], F32)
            bl1 = self.sb(es, "bl1", [128, ne * 16], F32)
            R_b = Res("bup")
            self.load("sp", bup[:], self.fm_up[li], R_b)
            S.op("dve", lambda v: v.tensor_scalar(out=bl1[:], in0=bup[:], scalar1=1.0, scalar2=None, op0=ALU.add), reads=[R_b], writes=[R_b])
            rows = [self.sb(es, "rows", [128, NSB, D], BF16) for _ in range(2)]
            R_rows = [Res("rows") for _ in range(2)]
            xsT = [self.sb(es, "xsT", [128, DC, CAP], BF16) for _ in range(2)]
            R_xsT = [Res("xsT") for _ in range(2)]
            act = [self.sb(es, "act", [128, DC, CAP], BF16) for _ in range(2)]
            R_act = [[Res("act") for _ in range(DC)] for _ in range(2)]
            gc = [self.sb(es, "gc", [128, CAP], F32) for _ in range(2)]
            m1 = [self.sb(es, "m1", [128, CAP], F32) for _ in range(2)]
            l2 = [self.sb(es, "l2", [128, CAP], F32) for _ in range(2)]
            R_gc = [Res("gc") for _ in range(2)]
            R_m1 = [Res("m1") for _ in range(2)]
            R_l2 = [Res("l2") for _ in range(2)]
            ysb = [self.sb(es, "ysb", [128, D], F32) for _ in range(3)]
            R_ysb = [Res("ysb") for _ in range(3)]
            PGp = [self.ps(es, "e_PG", [128, 512], F32) for _ in range(2)]
            PLp = [self.ps(es, "e_PL", [128, 512], F32) for _ in range(2)]
            PY = [self.ps(es, "e_PY", [128, 512], F32) for _ in range(2)]
            PTp = [self.ps(es, "e_PT", [128, D], BF16) for _ in range(2)]
            R_PGp = [Res("PG") for _ in range(2)]
            R_PLp = [Res("PL") for _ in range(2)]
            R_PY = [Res("PY") for _ in range(2)]
            R_PTp = [Res("PT") for _ in range(2)]
            nload = [0]
            slot_of = {}

            def issue_weights(e, parts=("g", "l", "d")):
                srcu = self.w_up[li, e].rearrange("(c p) n -> p c n", p=128)
                srcd = self.w_down[li, e].rearrange("(c p) n -> p c n", p=128)
                srcs = {"g": srcu[:, :, 0:1024], "l": srcu[:, :, 1024:2048], "d": srcd}
                for part in parts:
                    src = srcs[part]
                    s_ = (3 * e + "gld".index(part)) % NSLOT
                    slot_of[(e, part)] = s_
                    S.dma("pool", lambda g, s_=s_, src=src: g.dma_start(out=ring[s_][:], in_=src), writes=[R_ring[s_]])

            def issue_rows(e):
                b = e % 2
                src = self.xs_d[e * CAP:(e + 1) * CAP, :].rearrange("(s p) d -> p s d", p=128)
                self.load("sp", rows[b][:], src, R_rows[b])

            issue_weights(0)
            issue_rows(0)
            issue_weights(1)
            rp = 0
            ry = 0
            rt = 0
            for e in range(ne):
                b = e % 2
                if e + 1 < ne:
                    issue_rows(e + 1)
                for sb_ in range(NSB):
                    pt, R_pt = PTp[rt % 2], R_PTp[rt % 2]
                    rt += 1
                    for c in range(DC):
                        S.op("pe", lambda p, c=c, sb_=sb_, pt=pt, b=b: p.transpose(out=pt[:, c * 128:(c + 1) * 128],
                                                                                    in_=rows[b][:, sb_, c * 128:(c + 1) * 128],
                                                                                    identity=self.ident_b),
                             reads=[R_rows[b], self.R_const], writes=[R_pt])
                    S.op("act", lambda a, sb_=sb_, pt=pt, b=b: a.activation(out=xsT[b][:, :, sb_ * 128:(sb_ + 1) * 128],
                                                                            in_=pt[:].rearrange("p (c t) -> p c t", c=DC), func=AF.Identity),
                         reads=[R_pt], writes=[R_xsT[b]])
                wg, R_wg = ring[slot_of[(e, "g")]], R_ring[slot_of[(e, "g")]]
                wl, R_wl = ring[slot_of[(e, "l")]], R_ring[slot_of[(e, "l")]]
                wd, R_wd = ring[slot_of[(e, "d")]], R_ring[slot_of[(e, "d")]]
                for fb in range(8):
                    pg, R_pg = PGp[rp % 2], R_PGp[rp % 2]
                    pl, R_pl = PLp[rp % 2], R_PLp[rp % 2]
                    w_ = rp % 2
                    rp += 1
                    for c in range(DC):
                        S.op("pe", lambda p, c=c, fb=fb, pg=pg, wg=wg, b=b: p.matmul(pg[:, 0:CAP], lhsT=wg[:, c, fb * 128:(fb + 1) * 128],
                                                                                      rhs=xsT[b][:, c, :], start=(c == 0), stop=(c == DC - 1)),
                             reads=[R_wg, R_xsT[b]], writes=[R_pg])
                    for c in range(DC):
                        S.op("pe", lambda p, c=c, fb=fb, pl=pl, wl=wl, b=b: p.matmul(pl[:, 0:CAP], lhsT=wl[:, c, fb * 128:(fb + 1) * 128],
                                                                                      rhs=xsT[b][:, c, :], start=(c == 0), stop=(c == DC - 1)),
                             reads=[R_wl, R_xsT[b]], writes=[R_pl])
                    col_g = e * 16 + fb
                    col_l = e * 16 + 8 + fb
                    S.op("dve", lambda v, pg=pg, w_=w_, col_g=col_g: v.tensor_scalar(out=gc[w_][:], in0=pg[:, 0:CAP], scalar1=bup[:, col_g:col_g + 1],
                                                                                     scalar2=LIMIT, op0=ALU.add, op1=ALU.min),
                         reads=[R_pg, R_b], writes=[R_gc[w_]])
                    S.op("act", lambda a, w_=w_: a.activation(out=m1[w_][:], in_=gc[w_][:], func=AF.Sigmoid, scale=SW_ALPHA),
                         reads=[R_gc[w_]], writes=[R_m1[w_]])
                    S.op("dve", lambda v, w_=w_: v.tensor_tensor(out=m1[w_][:], in0=m1[w_][:], in1=gc[w_][:], op=ALU.mult),
                         reads=[R_gc[w_]], writes=[R_m1[w_]])
                    S.op("dve", lambda v, pl=pl, w_=w_, col_l=col_l: v.tensor_scalar(out=l2[w_][:], in0=pl[:, 0:CAP], scalar1=bl1[:, col_l:col_l + 1],
                                                                                     scalar2=1.0 - LIMIT, op0=ALU.add, op1=ALU.max),
                         reads=[R_pl, R_b], writes=[R_l2[w_]])
                    S.op("dve", lambda v, w_=w_, fb=fb, b=b: v.scalar_tensor_tensor(out=act[b][:, fb, :], in0=l2[w_][:], scalar=1.0 + LIMIT,
                                                                                    in1=m1[w_][:], op0=ALU.min, op1=ALU.mult),
                         reads=[R_l2[w_], R_m1[w_]], writes=[R_act[b][fb]])
                if e + 2 < ne:
                    issue_weights(e + 2, ("g", "l"))
                for sb_ in range(NSB):
                    yb, R_yb = ysb[(e * NSB + sb_) % 3], R_ysb[(e * NSB + sb_) % 3]
                    for h in range(2):
                        py, R_py = PY[ry % 2], R_PY[ry % 2]
                        ry += 1
                        for fc in range(DC):
                            S.op("pe", lambda p, fc=fc, sb_=sb_, h=h, py=py, wd=wd, b=b: p.matmul(
                                py[:], lhsT=act[b][:, fc, sb_ * 128:(sb_ + 1) * 128], rhs=wd[:, fc, h * 512:(h + 1) * 512],
                                start=(fc == 0), stop=(fc == DC - 1)), reads=[R_act[b][fc], R_wd], writes=[R_py])
                        S.op("act", lambda a, h=h, py=py, yb=yb: a.activation(out=yb[:, h * 512:(h + 1) * 512], in_=py[:], func=AF.Identity),
                             reads=[R_py], writes=[R_yb])
                    r0 = e * CAP + sb_ * 128
                    S.dma("sp", lambda g, r0=r0, yb=yb: g.dma_start(out=self.ys_d[r0:r0 + 128, :], in_=yb[:]), reads=[R_yb])
                if e + 2 < ne:
                    issue_weights(e + 2, ("d",))
            S.barrier()

    def stage_combine(self, li, dst, make_xT):
        S = self.S
        ne = self.ne
        with ExitStack() as es:
            self.alloc_eps(es)
            bd = self.sb(es, "c_bd", [32, D], BF16)
            g2 = self.sb(es, "c_g2", [128, D], F32)
            b2 = self.sb(es, "c_b2", [128, D], F32)
            R_p = Res("cp")
            S.dma("pool", lambda g: g.dma_start(out=bd[0:ne, :], in_=self.b_down[li]), writes=[R_p])
            self.bcast_load(g2[:], self.ln_g[li, 1], R_p)
            self.bcast_load(b2[:], self.ln_b[li, 1], R_p)
            NB = 3
            x1 = [self.sb(es, "c_x1", [128, D], F32) for _ in range(NB)]
            R_x1 = [Res("x1") for _ in range(NB)]
            yk = [[self.sb(es, "c_yk", [128, D], F32) for _ in range(TOPK)] for _ in range(NB)]
            R_yk = [[Res("yk") for _ in range(TOPK)] for _ in range(NB)]
            acc = self.sb(es, "c_acc", [128, D], F32)
            R_acc = Res("acc")
            xn = [self.sb(es, "c_xn", [128, D], F32) for _ in range(2)]
            R_xn = [Res("xn") for _ in range(2)]
            xb = [self.sb(es, "c_xb", [128, D], BF16) for _ in range(2)]
            R_xb = [Res("xb") for _ in range(2)]
            stage = [self.sb(es, "c_st", [128, DC, 512], BF16) for _ in range(2)]
            R_st = [Res("st") for _ in range(2)]
            ln = self.alloc_ln_bufs(es, "c")
            PB = [self.ps(es, "c_PB", [128, 512], F32) for _ in range(2)]
            R_PB = [Res("PB") for _ in range(2)]
            pst = [self.ps(es, "c_pst", [128, D], BF16) for _ in range(2)]
            R_pst = [Res("pst") for _ in range(2)]

            def prefetch(i):
                b = i % NB
                self.load("sp", x1[b][:], self.x1_d[i * 128:(i + 1) * 128, :], R_x1[b])
                for k in range(TOPK):
                    S.dma("pool", lambda g, i=i, k=k, b=b: g.indirect_dma_start(
                        out=yk[b][k][:, :], out_offset=None, in_=self.ys_d[:, :],
                        in_offset=bass.IndirectOffsetOnAxis(ap=self.slots_all[:, i, k:k + 1], axis=0),
                        bounds_check=self.bc_reg, oob_is_err=False), writes=[R_yk[b][k]], reads=[self.R_route])

            prefetch(0)
            prefetch(1)
            for i in range(NT):
                b = i % 2
                b3 = i % NB
                if i + 2 < NT:
                    prefetch(i + 2)
                for h in range(2):
                    S.op("pe", lambda p, h=h, i=i: p.matmul(PB[h][:], lhsT=self.GT_all[0:ne, i, :], rhs=bd[0:ne, h * 512:(h + 1) * 512],
                                                            start=True, stop=True), reads=[self.R_route, R_p], writes=[R_PB[h]])
                    S.op("dve", lambda v, h=h, b3=b3: v.scalar_tensor_tensor(out=acc[:, h * 512:(h + 1) * 512], in0=x1[b3][:, h * 512:(h + 1) * 512],
                                                                             scalar=ALPHA, in1=PB[h][:], op0=ALU.mult, op1=ALU.add),
                         reads=[R_x1[b3], R_PB[h]], writes=[R_acc])
                for k in range(TOPK):
                    S.op("dve", lambda v, k=k, b3=b3, i=i: v.scalar_tensor_tensor(out=acc[:], in0=yk[b3][k][:], scalar=self.gates_all[:, i, k:k + 1],
                                                                                  in1=acc[:], op0=ALU.mult, op1=ALU.add),
                         reads=[R_yk[b3][k], self.R_route], writes=[R_acc])
                self.ln_tile(ln, acc[:], R_acc, g2[:], b2[:], xn[b][:], R_xn[b], R_p)
                S.dma("sp", lambda g, i=i, b=b: g.dma_start(out=dst[i * 128:(i + 1) * 128, :], in_=xn[b][:]), reads=[R_xn[b]])
                if make_xT:
                    S.op("act", lambda a, b=b: a.activation(out=xb[b][:], in_=xn[b][:], func=AF.Identity), reads=[R_xn[b]], writes=[R_xb[b]])
                    sb_ = (i // 4) % 2
                    self.xT_from_tile(xb[b], R_xb[b], pst[b], R_pst[b], stage[sb_], R_st[sb_], i % 4, eng="act")
                    if i % 4 == 3:
                        self.store_xT_block(stage[sb_], R_st[sb_], i // 4)
            S.barrier()


def make_consts():
    c = np.zeros((128, 128 * 3 + 32 + 32 + 64), np.float32)
    c[:, 0:128] = np.eye(128, dtype=np.float32)
    p = np.arange(128)
    c[:, 128:256] = (p[:, None] < p[None, :]).astype(np.float32)
    c[:, 256:384] = 1.0
    c[:, 384:416] = np.arange(32, dtype=np.float32)[None, :]
    c[:, 416:448] = (np.arange(32, dtype=np.float32) * CAP)[None, :]
    for g in range(4):
        win = 2 ** (g + 1)
        c[:, 448 + g * 16:448 + (g + 1) * 16] = (1.0 / np.minimum(np.arange(16) + 1, win)).astype(np.float32)[None, :]
    return c


def fm(v):
    v = np.asarray(v, np.float32)
    return np.ascontiguousarray(v.reshape(-1, 128).T)


def prepare_shared(inp, layers, ne=NE):
    ev = [l // 2 for l in layers if l % 2 == 0]
    od = [l // 2 for l in layers if l % 2 == 1]
    ev_sel = ev if ev else [0]
    od_sel = od if od else [0]
    f32 = lambda a: np.ascontiguousarray(a, dtype=np.float32)
    out = {}
    out["ev_w_in"] = f32(inp["ev_w_in"][ev_sel])
    out["ev_w_out"] = f32(inp["ev_w_out"][ev_sel])
    out["pool_w"] = f32(inp["pool_w"][ev_sel])
    out["od_w_in"] = f32(inp["od_w_in"][od_sel])
    out["od_w_out"] = f32(inp["od_w_out"][od_sel])
    out["sgu_wT"] = f32(np.transpose(inp["sgu_w"][od_sel], (0, 1, 3, 2)))
    out["od_bv"] = f32(inp["od_b_in"][od_sel][:, 2048:4096])
    out["sgu_b"] = f32(inp["sgu_b"][od_sel].reshape(len(od_sel), 1024))
    out["b_out"] = f32(np.stack([inp["ev_b_out"][l // 2] if l % 2 == 0 else inp["od_b_out"][l // 2] for l in layers]))
    out["ln_g"] = f32(inp["ln_g"][layers])
    out["ln_b"] = f32(inp["ln_b"][layers])
    out["router_w"] = f32(inp["router_w"][layers][:, :, :ne])
    out["router_b"] = f32(inp["router_b"][layers][:, :ne])
    out["exp_w_up"] = f32(inp["exp_w_up"][layers][:, :ne])
    out["exp_w_down"] = f32(inp["exp_w_down"][layers][:, :ne])
    out["exp_b_down"] = f32(inp["exp_b_down"][layers][:, :ne])
    fe = []
    for i in ev_sel:
        cw = np.asarray(inp["conv_w"][i], np.float32)
        cw_fm = np.transpose(cw.reshape(31, 4, 128), (2, 1, 0)).reshape(128, 124)
        fe.append(np.concatenate([fm(inp["ev_b_in"][i]), fm(inp["pool_b"][i].reshape(-1)), fm(inp["pool_scale"][i]),
                                  fm(inp["conv_b"][i]), fm(inp["conv_ln_g"][i]), fm(inp["conv_ln_b"][i]), cw_fm], axis=1))
    out["fm_ev"] = f32(np.stack(fe))
    fo = []
    for i in od_sel:
        fo.append(np.concatenate([fm(inp["od_b_in"][i][:2048]), fm(inp["sgu_ln_g"][i]), fm(inp["sgu_ln_b"][i])], axis=1))
    out["fm_od"] = f32(np.stack(fo))
    fu = []
    for l in layers:
        bu = np.asarray(inp["exp_b_up"][l][:ne], np.float32)
        fu.append(np.transpose(bu.reshape(ne, 16, 128), (2, 0, 1)).reshape(128, ne * 16))
    out["fm_up"] = f32(np.stack(fu))
    out["consts"] = make_consts()
    return out


_PROG_CACHE = {}


def run_layers(inp, layers, cores, ne=NE, debug=False):
    key = (tuple(layers), ne, debug)
    if key not in _PROG_CACHE:
        _PROG_CACHE[key] = Prog(layers, ne, debug).build()
    nc = _PROG_CACHE[key]
    shared = prepare_shared(inp, layers, ne)
    x = np.asarray(inp["x"], np.float32)
    in_maps = []
    for c in cores:
        m = dict(shared)
        m["x"] = np.ascontiguousarray(x[c])
        in_maps.append(m)
    res = run_bass_kernel_spmd(nc, in_maps, core_ids=list(range(len(cores))))
    return res.results


def kernel(**inputs):
    inp = {k: np.asarray(v) for k, v in inputs.items()}
    res = run_layers(inp, list(range(DEPTH)), list(range(8)))
    return np.stack([r["y"] for r in res], axis=0).astype(np.float32)
```

```python
from contextlib import ExitStack

import numpy as np
import concourse.bass as bass
import concourse.mybir as mybir
from concourse.bass_utils import run_bass_kernel_spmd

F32 = mybir.dt.float32
BF16 = mybir.dt.bfloat16
I32 = mybir.dt.int32
U32 = mybir.dt.uint32
AF = mybir.ActivationFunctionType
ALU = mybir.AluOpType

T = 2048
D = 1024
NT = 16
DC = 8
NE = 32
TOPK = 4
import os
CAP = int(os.environ.get("K_CAP", "384"))
NSB = CAP // 128
DEPTH = 4
ALPHA = float((2 * DEPTH) ** 0.25)
EPS = 1e-5
LIMIT = 7.0
SW_ALPHA = 1.702

EV_FM = 12 + 4 + 4 + 4 + 4 + 4 + 124
OD_FM = 16 + 16 + 16
UP_FM = NE * 16


class Sem:
    _n = 0

    def __init__(self, h):
        self.h = h
        self.cnt = 0
        Sem._n += 1
        self.id = Sem._n


class Res:
    __slots__ = ("name", "w", "r", "dsem")

    def __init__(self, name):
        self.name = name
        self.w = {}
        self.r = {}
        self.dsem = None


class Sched:
    def __init__(self, nc, es):
        self.nc = nc
        self.es = es
        self.eng = {"pe": nc.tensor, "dve": nc.vector, "act": nc.scalar, "pool": nc.gpsimd, "sp": nc.sync}
        self.esem = {}
        self.waited = {k: {} for k in self.eng}
        self.dma_pending = {}
        self.bsem = None
        self.nsem = 0
        self.free_dsems = []
        self.stage_dsems = []

    def new_sem(self, name):
        s = Sem(self.es.enter_context(self.nc.semaphore(f"{name}{self.nsem}")))
        self.nsem += 1
        return s

    def _mark(self, e, instr):
        if e not in self.esem:
            self.esem[e] = self.new_sem("e" + e)
        s = self.esem[e]
        s.cnt += 1
        instr.then_inc(s.h, 1)
        return (s, s.cnt, e)

    def wait(self, e, tok):
        if tok is None:
            return
        s, val, prod = tok
        if prod == "pe" and e == "pe":
            return
        w = self.waited[e]
        if w.get(s.id, 0) >= val:
            return
        w[s.id] = val
        self.eng[e].wait_ge(s.h, val)

    def _pre(self, e, reads, writes, extra):
        for r in reads:
            for t in r.w.values():
                self.wait(e, t)
        for w in writes:
            for t in w.w.values():
                self.wait(e, t)
            for t in w.r.values():
                self.wait(e, t)
        for t in extra:
            self.wait(e, t)

    def _post(self, tok, reads, writes):
        for r in reads:
            r.r[tok[0].id] = tok
        for w in writes:
            w.w = {tok[0].id: tok}
            w.r = {}

    def op(self, e, fn, reads=(), writes=(), extra=()):
        self._pre(e, reads, writes, extra)
        instr = fn(self.eng[e])
        tok = self._mark(e, instr)
        self._post(tok, reads, writes)
        return tok

    def dma(self, e, fn, reads=(), writes=(), extra=()):
        self._pre(e, reads, writes, extra)
        owner = writes[0] if writes else reads[0]
        if owner.dsem is None:
            owner.dsem = self.free_dsems.pop() if self.free_dsems else self.new_sem("d")
            self.stage_dsems.append(owner.dsem)
        s = owner.dsem
        instr = fn(self.eng[e])
        s.cnt += 16
        instr.then_inc(s.h, 16)
        tok = (s, s.cnt, "dma")
        self._post(tok, reads, writes)
        self.dma_pending[s.id] = tok
        return tok

    def barrier(self):
        for e, s in self.esem.items():
            if e != "sp" and s.cnt:
                self.wait("sp", (s, s.cnt, e))
        for tok in self.dma_pending.values():
            self.wait("sp", tok)
        self.dma_pending = {}
        self.free_dsems.extend(self.stage_dsems)
        self.stage_dsems = []
        if self.bsem is None:
            self.bsem = self.new_sem("bar")
        b = self.bsem
        b.cnt += 1
        self.nc.sync.sem_inc(b.h, 1)
        tok = (b, b.cnt, "sp")
        for e in self.eng:
            if e != "sp":
                self.wait(e, tok)


class Prog:
    def __init__(self, layers, ne=NE, debug=False):
        self.layers = list(layers)
        self.ne = ne
        self.debug = debug
        self.n_even = sum(1 for l in self.layers if l % 2 == 0)
        self.n_odd = sum(1 for l in self.layers if l % 2 == 1)
        self.nl = len(self.layers)

    def sb(self, es, name, shape, dt):
        self._uid += 1
        return es.enter_context(self.nc.sbuf_tensor(f"{name}_{self._uid}", list(shape), dt))

    def ps(self, es, name, shape, dt):
        self._uid += 1
        return es.enter_context(self.nc.psum_tensor(f"{name}_{self._uid}", list(shape), dt))

    def din(self, name, shape, dt=F32):
        return self.nc.dram_tensor(name, list(shape), dt, kind="ExternalInput").ap()

    def dscratch(self, name, shape, dt):
        kind = "ExternalOutput" if self.debug else "Internal"
        return self.nc.dram_tensor(name, list(shape), dt, kind=kind).ap()

    def load(self, e, out, in_, res, extra=()):
        return self.S.dma(e, lambda g: g.dma_start(out=out, in_=in_), writes=[res], extra=extra)

    def bcast_load(self, out, row_ap, res):
        return self.S.dma("sp", lambda g: g.dma_start(out=out, in_=row_ap.partition_broadcast(128)), writes=[res])

    def build(self):
        nc = bass.Bass("TRN2", target_bir_lowering=False)
        self.nc = nc
        self._uid = 0
        nl, ne = self.nl, self.ne
        n_even, n_odd = max(self.n_even, 1), max(self.n_odd, 1)
        self.x_in = self.din("x", [T, D])
        self.ev_w_in = self.din("ev_w_in", [n_even, D, 1536])
        self.ev_w_out = self.din("ev_w_out", [n_even, D, D])
        self.pool_w = self.din("pool_w", [n_even, 4, 128, 128])
        self.od_w_in = self.din("od_w_in", [n_odd, D, 4096])
        self.od_w_out = self.din("od_w_out", [n_odd, 2048, D])
        self.sgu_wT = self.din("sgu_wT", [n_odd, 8, 128, 128])
        self.od_bv = self.din("od_bv", [n_odd, 2048])
        self.sgu_b = self.din("sgu_b", [n_odd, 1024])
        self.b_out = self.din("b_out", [nl, D])
        self.ln_g = self.din("ln_g", [nl, 2, D])
        self.ln_b = self.din("ln_b", [nl, 2, D])
        self.router_w = self.din("router_w", [nl, D, ne])
        self.router_b = self.din("router_b", [nl, ne])
        self.w_up = self.din("exp_w_up", [nl, ne, D, 2 * D])
        self.w_down = self.din("exp_w_down", [nl, ne, D, D])
        self.b_down = self.din("exp_b_down", [nl, ne, D])
        self.fm_ev = self.din("fm_ev", [n_even, 128, EV_FM])
        self.fm_od = self.din("fm_od", [n_odd, 128, OD_FM])
        self.fm_up = self.din("fm_up", [nl, 128, ne * 16])
        self.consts = self.din("consts", [128, 128 * 3 + 32 + 32 + 64])
        self.y_out = nc.dram_tensor("y", [T, D], F32, kind="ExternalOutput").ap()
        self.x1_d = self.dscratch("x1_d", [T, D], F32)
        self.x2_d = self.dscratch("x2_d", [T, D], F32)
        self.xT_d = self.dscratch("xT_d", [D, T], BF16)
        self.xs_d = self.dscratch("xs_d", [ne * CAP, D], BF16)
        self.ys_d = self.dscratch("ys_d", [ne * CAP, D], F32)

        with ExitStack() as top:
            S = self.S = Sched(nc, top)
            self.bc_reg = nc.gpsimd.to_reg(ne * CAP - 1)
            self.c_f32 = self.sb(top, "c_f32", [128, 128 * 3 + 32 + 32 + 64], F32)
            self.c_bf = self.sb(top, "c_bf", [128, 128 * 3], BF16)
            self.gates_all = self.sb(top, "gates_all", [128, NT, TOPK], F32)
            self.slots_all = self.sb(top, "slots_all", [128, NT, TOPK], I32)
            self.GT_all = self.sb(top, "GT_all", [32, NT, 128], BF16)
            self.R_const = Res("const")
            self.R_route = Res("route")
            self.load("sp", self.c_f32[:], self.consts, self.R_const)
            S.dma("pool", lambda g: g.dma_start(out=self.c_bf[:], in_=self.consts[:, 0:384]), writes=[self.R_const])
            self.ident_f = self.c_f32[:, 0:128]
            self.ident_b = self.c_bf[:, 0:128]
            self.tri_b = self.c_bf[:, 128:256]
            self.ones_b = self.c_bf[:, 256:384]
            self.iota_e = self.c_f32[:, 384:416]
            self.slotbase = self.c_f32[:, 416:448]
            self.invcnt = self.c_f32[:, 448:512]
            S.barrier()

            self.stage_xT0()
            x_res = self.x_in
            for li, l in enumerate(self.layers):
                last = li == nl - 1
                if l % 2 == 0:
                    self.stage_mix_even(li, l, x_res)
                else:
                    self.stage_mix_odd(li, l, x_res)
                self.stage_router(li)
                self.stage_experts(li)
                self.stage_combine(li, self.y_out if last else self.x2_d, make_xT=not last)
                x_res = self.x2_d
            S.barrier()
        return nc

    def ln_tile(self, es_bufs, s_ap, s_res, g_bc, b_bc, out_ap, out_res, p_res):
        S = self.S
        stats, mv, sc, z, st_res, z_res = es_bufs
        for h in range(2):
            S.op("dve", lambda v, h=h: v.bn_stats(stats[:, h * 6:(h + 1) * 6], s_ap[:, h * 512:(h + 1) * 512]),
                 reads=[s_res], writes=[st_res])
        S.op("dve", lambda v: v.bn_aggr(mv[:, 0:2], stats[:, 0:12]), reads=[st_res], writes=[st_res])
        S.op("act", lambda a: a.activation(out=sc[:, 0:1], in_=mv[:, 1:2], func=AF.Sqrt, bias=self.eps_ap, scale=1.0),
             reads=[st_res, self.R_eps], writes=[st_res])
        S.op("dve", lambda v: v.reciprocal(sc[:, 1:2], sc[:, 0:1]), reads=[st_res], writes=[st_res])
        S.op("dve", lambda v: v.tensor_scalar(out=sc[:, 2:3], in0=mv[:, 0:1], scalar1=sc[:, 1:2], scalar2=-1.0,
                                              op0=ALU.mult, op1=ALU.mult), reads=[st_res], writes=[st_res])
        S.op("act", lambda a: a.activation(out=z[:], in_=s_ap, func=AF.Identity, bias=sc[:, 2:3], scale=sc[:, 1:2]),
             reads=[s_res, st_res], writes=[z_res])
        S.op("pool", lambda v: v.tensor_tensor(out=z[:], in0=z[:], in1=g_bc, op=ALU.mult),
             reads=[p_res], writes=[z_res])
        S.op("dve", lambda v: v.tensor_tensor(out=out_ap, in0=z[:], in1=b_bc, op=ALU.add),
             reads=[z_res, p_res], writes=[out_res])

    def alloc_ln_bufs(self, es, tag):
        stats = self.sb(es, "lnstats" + tag, [128, 12], F32)
        mv = self.sb(es, "lnmv" + tag, [128, 2], F32)
        sc = self.sb(es, "lnsc" + tag, [128, 4], F32)
        z = self.sb(es, "lnz" + tag, [128, D], F32)
        return (stats, mv, sc, z, Res("lnst" + tag), Res("lnz" + tag))

    def alloc_eps(self, es):
        self.eps_t = self.sb(es, "eps", [128, 1], F32)
        self.R_eps = Res("eps")
        self.S.op("dve", lambda v: v.memset(self.eps_t[:], EPS), writes=[self.R_eps])
        self.eps_ap = self.eps_t[:, 0:1]

    def xT_from_tile(self, xb, xb_res, pst, pst_res, stage, stage_res, q, eng="dve"):
        S = self.S
        for c in range(DC):
            S.op("pe", lambda p, c=c: p.transpose(out=pst[:, c * 128:(c + 1) * 128], in_=xb[:, c * 128:(c + 1) * 128],
                                                  identity=self.ident_b),
                 reads=[xb_res, self.R_const], writes=[pst_res])
        if eng == "dve":
            S.op("dve", lambda v: v.tensor_copy(out=stage[:, :, q * 128:(q + 1) * 128],
                                                in_=pst[:].rearrange("p (c t) -> p c t", c=DC)),
                 reads=[pst_res], writes=[stage_res])
        else:
            S.op("act", lambda a: a.activation(out=stage[:, :, q * 128:(q + 1) * 128],
                                               in_=pst[:].rearrange("p (c t) -> p c t", c=DC), func=AF.Identity),
                 reads=[pst_res], writes=[stage_res])

    def store_xT_block(self, stage, stage_res, blk):
        dst = self.xT_d.rearrange("(c p) t -> p c t", p=128)[:, :, blk * 512:(blk + 1) * 512]
        self.S.dma("sp", lambda g: g.dma_start(out=dst, in_=stage[:]), reads=[stage_res])

    def stage_xT0(self):
        S = self.S
        with ExitStack() as es:
            xt = [self.sb(es, "s0x", [128, D], F32) for _ in range(2)]
            xb = [self.sb(es, "s0b", [128, D], BF16) for _ in range(2)]
            stage = [self.sb(es, "s0st", [128, DC, 512], BF16) for _ in range(2)]
            pst = [self.ps(es, "s0ps", [128, D], BF16) for _ in range(2)]
            R_xt = [Res("xt") for _ in range(2)]
            R_xb = [Res("xb") for _ in range(2)]
            R_st = [Res("st") for _ in range(2)]
            R_ps = [Res("ps") for _ in range(2)]
            for i in range(NT):
                b = i % 2
                self.load("sp", xt[b][:], self.x_in[i * 128:(i + 1) * 128, :], R_xt[b])
                S.op("act", lambda a, b=b: a.activation(out=xb[b][:], in_=xt[b][:], func=AF.Identity),
                     reads=[R_xt[b]], writes=[R_xb[b]])
                sb_ = (i // 4) % 2
                self.xT_from_tile(xb[b], R_xb[b], pst[b], R_ps[b], stage[sb_], R_st[sb_], i % 4)
                if i % 4 == 3:
                    self.store_xT_block(stage[sb_], R_st[sb_], i // 4)
            S.barrier()

    def alloc_epilogue(self, es, li, n_xn=2):
        S = self.S
        ep = {}
        ep["bpad"] = self.sb(es, "bpad", [128, D], BF16)
        ep["R_bp"] = Res("bpad")
        S.op("dve", lambda v: v.memset(ep["bpad"][:], 0.0), writes=[ep["R_bp"]])
        S.dma("pool", lambda g: g.dma_start(out=ep["bpad"][0:1, :], in_=self.b_out[li:li + 1, :]), writes=[ep["R_bp"]])
        ep["g"] = self.sb(es, "ln1g", [128, D], F32)
        ep["b"] = self.sb(es, "ln1b", [128, D], F32)
        ep["R_p"] = Res("ep_params")
        self.bcast_load(ep["g"][:], self.ln_g[li, 0], ep["R_p"])
        self.bcast_load(ep["b"][:], self.ln_b[li, 0], ep["R_p"])
        ep["xres"] = [self.sb(es, "xres", [128, D], F32) for _ in range(2)]
        ep["R_xres"] = [Res("xres") for _ in range(2)]
        ep["s"] = self.sb(es, "eps_s", [128, D], F32)
        ep["R_s"] = Res("s")
        ep["xn"] = [self.sb(es, "xn", [128, D], F32) for _ in range(n_xn)]
        ep["R_xn"] = [Res("xn") for _ in range(n_xn)]
        ep["ln"] = self.alloc_ln_bufs(es, "m")
        return ep

    def prefetch_xres(self, ep, x_src, i):
        b = i % 2
        self.load("sp", ep["xres"][b][:], x_src[i * 128:(i + 1) * 128, :], ep["R_xres"][b])

    def mixer_epilogue(self, ep, i, mix_ps, R_mix):
        S = self.S
        b = i % 2
        xres, R_x = ep["xres"][b], ep["R_xres"][b]
        s, R_s = ep["s"], ep["R_s"]
        for h in range(2):
            S.op("dve", lambda v, h=h: v.scalar_tensor_tensor(out=s[:, h * 512:(h + 1) * 512],
                                                              in0=xres[:, h * 512:(h + 1) * 512], scalar=ALPHA,
                                                              in1=mix_ps[h][:], op0=ALU.mult, op1=ALU.add),
                 reads=[R_x, R_mix[h]], writes=[R_s])
        nx = i % len(ep["xn"])
        xn, R_xn = ep["xn"][nx], ep["R_xn"][nx]
        self.ln_tile(ep["ln"], s[:], R_s, ep["g"][:], ep["b"][:], xn[:], R_xn, ep["R_p"])
        S.dma("sp", lambda g: g.dma_start(out=self.x1_d[i * 128:(i + 1) * 128, :], in_=xn[:]), reads=[R_xn])

    def stage_mix_even(self, li, l, x_src):
        S = self.S
        ei = sum(1 for q in self.layers[:li] if q % 2 == 0)
        with ExitStack() as es:
            self.alloc_eps(es)
            w_in = self.sb(es, "ev_win", [128, DC, 1536], BF16)
            w_out = self.sb(es, "ev_wout", [128, DC, D], BF16)
            pw = self.sb(es, "ev_pw", [128, 4, 128], BF16)
            fm = self.sb(es, "ev_fm", [128, EV_FM], F32)
            diag = self.sb(es, "ev_diag", [128, 4, 31, 128], BF16)
            R_w = Res("ev_w")
            R_diag = Res("diag")
            src = self.ev_w_in[ei].rearrange("(c p) n -> p c n", p=128)
            for c0 in range(0, DC, 4):
                S.dma("pool", lambda g, c0=c0: g.dma_start(out=w_in[:, c0:c0 + 4, :], in_=src[:, c0:c0 + 4, :]), writes=[R_w])
            src2 = self.ev_w_out[ei].rearrange("(c p) n -> p c n", p=128)
            for c0 in range(0, DC, 4):
                S.dma("pool", lambda g, c0=c0: g.dma_start(out=w_out[:, c0:c0 + 4, :], in_=src2[:, c0:c0 + 4, :]), writes=[R_w])
            S.dma("pool", lambda g: g.dma_start(out=pw[:], in_=self.pool_w[ei].rearrange("g c d -> c g d")), writes=[R_w])
            self.load("sp", fm[:], self.fm_ev[ei], R_w)
            b_in = fm[:, 0:12]
            pool_b = fm[:, 12:16]
            pool_s = fm[:, 16:20]
            conv_b = fm[:, 20:24]
            cln_g = fm[:, 24:28]
            cln_b = fm[:, 28:32]
            conv_w = fm[:, 32:156]
            for j in range(4):
                for k in range(31):
                    S.op("dve", lambda v, j=j, k=k: v.tensor_scalar(out=diag[:, j, k, :], in0=self.ident_b,
                                                                    scalar1=conv_w[:, j * 31 + k:j * 31 + k + 1],
                                                                    scalar2=None, op0=ALU.mult),
                         reads=[R_w, self.R_const], writes=[R_diag])
            ep = self.alloc_epilogue(es, li)
            xTb = [self.sb(es, "ev_xT", [128, DC, 512], BF16) for _ in range(2)]
            R_xTb = [Res("xTb") for _ in range(2)]
            pbuf = [self.sb(es, "ev_p", [128, 4, 528], F32) for _ in range(2)]
            R_pbuf = [Res("pbuf") for _ in range(2)]
            R_phalo = [Res("phalo") for _ in range(2)]
            glu = [self.sb(es, "ev_glu", [128, 4, 544], BF16) for _ in range(2)]
            R_glu = [Res("glu") for _ in range(2)]
            R_ghalo = [Res("ghalo") for _ in range(2)]
            tA = self.sb(es, "ev_tA", [128, 528], F32)
            tB = self.sb(es, "ev_tB", [128, 528], F32)
            R_tA, R_tB = Res("tA"), Res("tB")
            t16 = self.sb(es, "ev_t16", [128, 16], F32)
            R_t16 = Res("t16")
            pooled = self.sb(es, "ev_pooled", [128, 4, 512], BF16)
            R_pooled = [Res("pooled") for _ in range(4)]
            ymix = self.sb(es, "ev_ymix", [128, 8, 512], BF16)
            R_ymix = [Res("ymix") for _ in range(8)]
            sg = [self.sb(es, "ev_sg", [128, 512], F32) for _ in range(2)]
            R_sg = [Res("sg") for _ in range(2)]
            yc = self.sb(es, "ev_yc", [128, 4, 512], F32)
            ycb = self.sb(es, "ev_ycb", [128, 4, 512], BF16)
            ysq = self.sb(es, "ev_ysq", [128, 4, 512], BF16)
            R_yc = [Res("yc") for _ in range(4)]
            mean = self.sb(es, "ev_mean", [128, 512], F32)
            msq = self.sb(es, "ev_msq", [128, 512], F32)
            rstd = self.sb(es, "ev_rstd", [128, 512], F32)
            R_stat = Res("stat")
            t1 = [self.sb(es, "ev_t1", [128, 512], F32) for _ in range(2)]
            R_t1 = [Res("t1") for _ in range(2)]
            A = [self.ps(es, "ev_A", [128, 512], F32) for _ in range(4)]
            R_A = [Res("A") for _ in range(4)]
            S1 = self.ps(es, "ev_S1", [128, 512], F32)
            S2 = self.ps(es, "ev_S2", [128, 512], F32)
            R_S1, R_S2 = Res("S1"), Res("S2")
            M = [self.ps(es, "ev_M", [128, 512], F32) for _ in range(2)]
            R_M = [Res("M") for _ in range(2)]
            rot = [0]

            def nextA():
                k = rot[0] % 4
                rot[0] += 1
                return A[k], R_A[k]

            S.op("dve", lambda v: v.memset(pbuf[0][:, :, 0:16], 0.0), writes=[R_phalo[0]])
            S.op("dve", lambda v: v.memset(glu[0][:, :, 0:32], 0.0), writes=[R_ghalo[0]])
            xT_src = self.xT_d.rearrange("(c p) t -> p c t", p=128)
            self.load("sp", xTb[0][:], xT_src[:, :, 0:512], R_xTb[0])
            self.prefetch_xres(ep, x_src, 0)
            for tb in range(4):
                cb = tb % 2
                if tb + 1 < 4:
                    self.load("sp", xTb[1 - cb][:], xT_src[:, :, (tb + 1) * 512:(tb + 2) * 512], R_xTb[1 - cb])
                xt = xTb[cb]
                for j in range(4):
                    ps_, R_ps = nextA()
                    for c in range(DC):
                        S.op("pe", lambda p, c=c, j=j, ps_=ps_: p.matmul(ps_[:], lhsT=w_in[:, c, j * 128:(j + 1) * 128],
                                                                           rhs=xt[:, c, :], start=(c == 0), stop=(c == DC - 1)),
                             reads=[R_w, R_xTb[cb]], writes=[R_ps])
                    S.op("act", lambda a, j=j, ps_=ps_: a.activation(out=pbuf[cb][:, j, 16:528], in_=ps_[:], func=AF.Identity,
                                                                     bias=b_in[:, j:j + 1], scale=1.0),
                         reads=[R_ps, R_w], writes=[R_pbuf[cb]])
                for j in range(4):
                    psa, R_psa = nextA()
                    psg, R_psg = nextA()
                    for c in range(DC):
                        S.op("pe", lambda p, c=c, j=j, psa=psa: p.matmul(psa[:], lhsT=w_in[:, c, (4 + j) * 128:(5 + j) * 128],
                                                                           rhs=xt[:, c, :], start=(c == 0), stop=(c == DC - 1)),
                             reads=[R_w, R_xTb[cb]], writes=[R_psa])
                    for c in range(DC):
                        S.op("pe", lambda p, c=c, j=j, psg=psg: p.matmul(psg[:], lhsT=w_in[:, c, (8 + j) * 128:(9 + j) * 128],
                                                                           rhs=xt[:, c, :], start=(c == 0), stop=(c == DC - 1)),
                             reads=[R_w, R_xTb[cb]], writes=[R_psg])
                    sgb = sg[j % 2]
                    S.op("act", lambda a, j=j, psg=psg, sgb=sgb: a.activation(out=sgb[:], in_=psg[:], func=AF.Sigmoid,
                                                                               bias=b_in[:, 8 + j:9 + j], scale=1.0),
                         reads=[R_psg, R_w], writes=[R_sg[j % 2]])
                    S.op("dve", lambda v, j=j, psa=psa, sgb=sgb: v.scalar_tensor_tensor(
                        out=glu[cb][:, j, 32:544], in0=psa[:], scalar=b_in[:, 4 + j:5 + j], in1=sgb[:],
                        op0=ALU.add, op1=ALU.mult), reads=[R_psa, R_sg[j % 2], R_w], writes=[R_glu[cb]])
                for g in range(4):
                    win = 2 ** (g + 1)
                    cur, R_cur = pbuf[cb][:, g, :], None
                    bufs = [(tA, R_tA), (tB, R_tB)]
                    for s_ in range(g + 1):
                        sh = 2 ** s_
                        nxt, R_nxt = bufs[s_ % 2]
                        rd = [R_pbuf[cb], R_phalo[cb]] if R_cur is None else [R_cur]
                        S.op("dve", lambda v, cur=cur, nxt=nxt, sh=sh: v.tensor_tensor(
                            out=nxt[:, sh:528], in0=cur[:, sh:528], in1=cur[:, 0:528 - sh], op=ALU.add),
                            reads=rd, writes=[R_nxt])
                        cur, R_cur = nxt[:, :], R_nxt
                    S.op("dve", lambda v, cur=cur, g=g, win=win: v.scalar_tensor_tensor(
                        out=pooled[:, g, :], in0=cur[:, 16:528], scalar=1.0 / win, in1=pbuf[cb][:, g, 16:528],
                        op0=ALU.mult, op1=ALU.subtract), reads=[R_cur, R_pbuf[cb]], writes=[R_pooled[g]])
                    if tb == 0:
                        S.op("dve", lambda v, cur=cur, g=g: v.tensor_tensor(out=t16[:], in0=cur[:, 16:32],
                                                                           in1=self.invcnt[:, g * 16:(g + 1) * 16], op=ALU.mult),
                             reads=[R_cur, self.R_const], writes=[R_t16])
                        S.op("dve", lambda v, g=g: v.tensor_tensor(out=pooled[:, g, 0:16], in0=t16[:],
                                                                   in1=pbuf[cb][:, g, 16:32], op=ALU.subtract),
                             reads=[R_t16, R_pbuf[cb]], writes=[R_pooled[g]])
                if tb + 1 < 4:
                    S.op("act", lambda a: a.activation(out=pbuf[1 - cb][:, :, 0:16], in_=pbuf[cb][:, :, 512:528], func=AF.Identity),
                         reads=[R_pbuf[cb]], writes=[R_phalo[1 - cb]])
                    S.op("act", lambda a: a.activation(out=glu[1 - cb][:, :, 0:32], in_=glu[cb][:, :, 512:544], func=AF.Identity),
                         reads=[R_glu[cb]], writes=[R_ghalo[1 - cb]])
                for g in range(4):
                    ps_, R_ps = nextA()
                    S.op("pe", lambda p, g=g, ps_=ps_: p.matmul(ps_[:], lhsT=pw[:, g, :], rhs=pooled[:, g, :], start=True, stop=True),
                         reads=[R_w, R_pooled[g]], writes=[R_ps])
                    S.op("dve", lambda v, g=g, ps_=ps_: v.tensor_scalar(out=ymix[:, g, :], in0=ps_[:], scalar1=pool_b[:, g:g + 1],
                                                                        scalar2=pool_s[:, g:g + 1], op0=ALU.add, op1=ALU.mult),
                         reads=[R_ps, R_w], writes=[R_ymix[g]])
                for j in range(4):
                    ps_, R_ps = nextA()
                    for k in range(31):
                        S.op("pe", lambda p, j=j, k=k, ps_=ps_: p.matmul(ps_[:], lhsT=diag[:, j, k, :],
                                                                           rhs=glu[cb][:, j, 2 + k:2 + k + 512],
                                                                           start=(k == 0), stop=(k == 30)),
                             reads=[R_diag, R_glu[cb], R_ghalo[cb]], writes=[R_ps])
                    S.op("act", lambda a, j=j, ps_=ps_: a.activation(out=yc[:, j, :], in_=ps_[:], func=AF.Identity,
                                                                     bias=conv_b[:, j:j + 1], scale=1.0),
                         reads=[R_ps, R_w], writes=[R_yc[j]])
                    S.op("act", lambda a, j=j, ps_=ps_: a.activation(out=ysq[:, j, :], in_=ps_[:], func=AF.Square,
                                                                     bias=conv_b[:, j:j + 1], scale=1.0),
                         reads=[R_ps, R_w], writes=[R_yc[j]])
                    S.op("dve", lambda v, j=j: v.tensor_copy(out=ycb[:, j, :], in_=yc[:, j, :]), reads=[R_yc[j]], writes=[R_yc[j]])
                for j in range(4):
                    S.op("pe", lambda p, j=j: p.matmul(S1[:], lhsT=self.ones_b, rhs=ycb[:, j, :], start=(j == 0), stop=(j == 3)),
                         reads=[R_yc[j], self.R_const], writes=[R_S1])
                for j in range(4):
                    S.op("pe", lambda p, j=j: p.matmul(S2[:], lhsT=self.ones_b, rhs=ysq[:, j, :], start=(j == 0), stop=(j == 3)),
                         reads=[R_yc[j], self.R_const], writes=[R_S2])
                S.op("act", lambda a: a.activation(out=mean[:], in_=S1[:], func=AF.Identity, scale=1.0 / 512), reads=[R_S1], writes=[R_stat])
                S.op("act", lambda a: a.activation(out=msq[:], in_=S1[:], func=AF.Square, scale=1.0 / 512), reads=[R_S1], writes=[R_stat])
                S.op("dve", lambda v: v.scalar_tensor_tensor(out=rstd[:], in0=S2[:], scalar=1.0 / 512, in1=msq[:],
                                                             op0=ALU.mult, op1=ALU.subtract), reads=[R_S2, R_stat], writes=[R_stat])
                S.op("act", lambda a: a.activation(out=rstd[:], in_=rstd[:], func=AF.Ln, bias=self.eps_ap, scale=1.0),
                     reads=[self.R_eps], writes=[R_stat])
                S.op("act", lambda a: a.activation(out=rstd[:], in_=rstd[:], func=AF.Exp, scale=-0.5), writes=[R_stat])
                for j in range(4):
                    tt_, R_tt = t1[j % 2], R_t1[j % 2]
                    S.op("dve", lambda v, j=j, tt_=tt_: v.tensor_tensor(out=tt_[:], in0=yc[:, j, :], in1=mean[:], op=ALU.subtract),
                         reads=[R_yc[j], R_stat], writes=[R_tt])
                    S.op("dve", lambda v, tt_=tt_: v.tensor_tensor(out=tt_[:], in0=tt_[:], in1=rstd[:], op=ALU.mult),
                         reads=[R_stat], writes=[R_tt])
                    S.op("act", lambda a, j=j, tt_=tt_: a.activation(out=ymix[:, 4 + j, :], in_=tt_[:], func=AF.Silu,
                                                                     bias=cln_b[:, j:j + 1], scale=cln_g[:, j:j + 1]),
                         reads=[R_tt, R_w], writes=[R_ymix[4 + j]])
                for q in range(4):
                    i = tb * 4 + q
                    if i + 1 < NT:
                        self.prefetch_xres(ep, x_src, i + 1)
                    for h in range(2):
                        S.op("pe", lambda p, h=h: p.matmul(M[h][:], lhsT=self.ones_b, rhs=ep["bpad"][:, h * 512:(h + 1) * 512],
                                                           start=True, stop=False), reads=[ep["R_bp"], self.R_const], writes=[R_M[h]])
                        for e_ in range(8):
                            S.op("pe", lambda p, e_=e_, h=h, q=q: p.matmul(M[h][:], lhsT=ymix[:, e_, q * 128:(q + 1) * 128],
                                                                            rhs=w_out[:, e_, h * 512:(h + 1) * 512],
                                                                            start=False, stop=(e_ == 7)),
                                 reads=[R_ymix[e_], R_w], writes=[R_M[h]])
                    self.mixer_epilogue(ep, i, M, R_M)
            S.barrier()

    def stage_mix_odd(self, li, l, x_src):
        S = self.S
        oi = sum(1 for q in self.layers[:li] if q % 2 == 1)
        with ExitStack() as es:
            self.alloc_eps(es)
            w_in = self.sb(es, "od_win", [128, DC, 4096], BF16)
            w_out = self.sb(es, "od_wout", [128, 16, D], BF16)
            wT = self.sb(es, "od_wT", [128, 8, 128], BF16)
            fm = self.sb(es, "od_fm", [128, OD_FM], F32)
            bv = self.sb(es, "od_bv", [128, 2048], F32)
            bs = self.sb(es, "od_bs", [128, 1024], F32)
            Kc = self.sb(es, "od_Kc", [128, 16, 128], F32)
            R_w = Res("od_w")
            R_wT = Res("od_wT")
            R_Kc = Res("Kc")
            src = self.od_w_in[oi].rearrange("(c p) n -> p c n", p=128)
            for n0 in range(0, 4096, 1024):
                S.dma("pool", lambda g, n0=n0: g.dma_start(out=w_in[:, :, n0:n0 + 1024], in_=src[:, :, n0:n0 + 1024]), writes=[R_w])
            src2 = self.od_w_out[oi].rearrange("(c p) n -> p c n", p=128)
            for c0 in range(0, 16, 8):
                S.dma("pool", lambda g, c0=c0: g.dma_start(out=w_out[:, c0:c0 + 8, :], in_=src2[:, c0:c0 + 8, :]), writes=[R_w])
            S.dma("pool", lambda g: g.dma_start(out=wT[:], in_=self.sgu_wT[oi].rearrange("h j i -> j h i")), writes=[R_wT])
            self.load("sp", fm[:], self.fm_od[oi], R_w)
            self.bcast_load(bv[:], self.od_bv[oi], R_w)
            self.bcast_load(bs[:], self.sgu_b[oi], R_w)
            b_u = fm[:, 0:16]
            g_fm = fm[:, 16:32]
            b_fm = fm[:, 32:48]
            S.op("dve", lambda v: v.memset(wT[64:128, :, 0:64], 0.0), writes=[R_wT])
            ep = self.alloc_epilogue(es, li, n_xn=1)
            xTb = [self.sb(es, "od_xT", [128, DC, 256], BF16) for _ in range(2)]
            R_xTb = [Res("xTb") for _ in range(2)]
            u = [self.sb(es, "od_u", [128, 16, 256], BF16) for _ in range(2)]
            R_u = [[Res("u") for _ in range(16)] for _ in range(2)]
            t1 = [self.sb(es, "od_t1", [128, 2048], F32) for _ in range(2)]
            R_t1 = [Res("t1") for _ in range(2)]
            z = [self.sb(es, "od_z", [128, 2048], BF16) for _ in range(2)]
            R_z = [Res("z") for _ in range(2)]
            vst = [self.sb(es, "od_vst", [128, 24], F32) for _ in range(2)]
            vmv = [self.sb(es, "od_vmv", [128, 2], F32) for _ in range(2)]
            vsc = [self.sb(es, "od_vsc", [128, 4], F32) for _ in range(2)]
            R_vst = [Res("vst") for _ in range(2)]
            tmp = [self.sb(es, "od_tmp", [128, 128], F32) for _ in range(2)]
            R_tmp = [Res("tmp") for _ in range(2)]
            PU = [self.ps(es, "od_PU", [128, 512], F32) for _ in range(2)]
            R_PU = [Res("PU") for _ in range(2)]
            PV = [self.ps(es, "od_PV", [128, 512], F32) for _ in range(2)]
            R_PV = [Res("PV") for _ in range(2)]
            PR = [self.ps(es, "od_PR", [128, 512], F32) for _ in range(2)]
            R_PR = [Res("PR") for _ in range(2)]
            M = [self.ps(es, "od_M", [128, 512], F32) for _ in range(2)]
            R_M = [Res("M") for _ in range(2)]
            for hh in range(2):
                S.op("pe", lambda p, hh=hh: p.matmul(PR[hh][:], lhsT=self.ones_b,
                                                     rhs=wT[:, hh * 4:(hh + 1) * 4, :],
                                                     start=True, stop=True), reads=[R_wT, self.R_const], writes=[R_PR[hh]])
            for blk in range(16):
                h = blk // 2
                S.op("dve", lambda v, blk=blk, h=h: v.scalar_tensor_tensor(
                    out=Kc[:, blk, :], in0=PR[h // 4][:, (h % 4) * 128:(h % 4 + 1) * 128], scalar=b_fm[:, blk:blk + 1],
                    in1=bs[:, h * 128:(h + 1) * 128], op0=ALU.mult, op1=ALU.add),
                    reads=[R_PR[h // 4], R_w], writes=[R_Kc])
            xT_src = self.xT_d.rearrange("(c p) t -> p c t", p=128)
            self.load("sp", xTb[0][:], xT_src[:, :, 0:256], R_xTb[0])
            self.prefetch_xres(ep, x_src, 0)
            rot = {"u": 0, "v": 0}

            def U(b):
                cb = b % 2
                xt, ub = xTb[cb], u[cb]
                for ec in range(16):
                    ps_, R_ps = PU[rot["u"] % 2], R_PU[rot["u"] % 2]
                    rot["u"] += 1
                    for c in range(DC):
                        S.op("pe", lambda p, c=c, ec=ec, ps_=ps_: p.matmul(ps_[:, 0:256], lhsT=w_in[:, c, ec * 128:(ec + 1) * 128],
                                                                             rhs=xt[:, c, :], start=(c == 0), stop=(c == DC - 1)),
                             reads=[R_w, R_xTb[cb]], writes=[R_ps])
                    S.op("act", lambda a, ec=ec, ps_=ps_: a.activation(out=ub[:, ec, :], in_=ps_[:, 0:256], func=AF.Gelu,
                                                                       bias=b_u[:, ec:ec + 1], scale=1.0),
                         reads=[R_ps, R_w], writes=[R_u[cb][ec]])

            def A(i):
                b, q = divmod(i, 2)
                cb = b % 2
                xt = xTb[cb]
                pi = i % 2
                zb, R_zb = z[pi], R_z[pi]
                t1b, R_t1b = t1[pi], R_t1[pi]
                vst_, vmv_, vsc_, R_vs = vst[pi], vmv[pi], vsc[pi], R_vst[pi]
                for vb in range(4):
                    ps_, R_ps = PV[rot["v"] % 2], R_PV[rot["v"] % 2]
                    rot["v"] += 1
                    for c in range(DC):
                        S.op("pe", lambda p, c=c, vb=vb, ps_=ps_, q=q: p.matmul(
                            ps_[:], lhsT=xt[:, c, q * 128:(q + 1) * 128],
                            rhs=w_in[:, c, 2048 + vb * 512:2048 + (vb + 1) * 512],
                            start=(c == 0), stop=(c == DC - 1)), reads=[R_w, R_xTb[cb]], writes=[R_ps])
                    S.op("dve", lambda v, vb=vb, ps_=ps_: v.tensor_tensor(out=t1b[:, vb * 512:(vb + 1) * 512], in0=ps_[:],
                                                                          in1=bv[:, vb * 512:(vb + 1) * 512], op=ALU.add),
                         reads=[R_ps, R_w], writes=[R_t1b])
                S.op("act", lambda a: a.activation(out=t1b[:], in_=t1b[:], func=AF.Gelu), writes=[R_t1b])
                for vb in range(4):
                    S.op("dve", lambda v, vb=vb: v.bn_stats(vst_[:, vb * 6:(vb + 1) * 6], t1b[:, vb * 512:(vb + 1) * 512]),
                         reads=[R_t1b], writes=[R_vs])
                S.op("dve", lambda v: v.bn_aggr(vmv_[:, 0:2], vst_[:, 0:24]), writes=[R_vs])
                S.op("act", lambda a: a.activation(out=vsc_[:, 0:1], in_=vmv_[:, 1:2], func=AF.Sqrt, bias=self.eps_ap, scale=1.0),
                     reads=[self.R_eps], writes=[R_vs])
                S.op("dve", lambda v: v.reciprocal(vsc_[:, 1:2], vsc_[:, 0:1]), writes=[R_vs])
                S.op("dve", lambda v: v.tensor_scalar(out=vsc_[:, 2:3], in0=vmv_[:, 0:1], scalar1=vsc_[:, 1:2], scalar2=-1.0,
                                                      op0=ALU.mult, op1=ALU.mult), writes=[R_vs])
                S.op("act", lambda a: a.activation(out=zb[:], in_=t1b[:], func=AF.Identity, bias=vsc_[:, 2:3], scale=vsc_[:, 1:2]),
                     reads=[R_t1b, R_vs], writes=[R_zb])

            def B(i):
                b, q = divmod(i, 2)
                cb = b % 2
                ub = u[cb]
                zb, R_zb = z[i % 2], R_z[i % 2]
                if i + 1 < NT:
                    self.prefetch_xres(ep, x_src, i + 1)
                for blk in range(16):
                    pr, R_pr = PR[(blk // 4) % 2], R_PR[(blk // 4) % 2]
                    S.op("pe", lambda p, blk=blk, pr=pr: p.matmul(pr[:, (blk % 4) * 128:(blk % 4 + 1) * 128],
                                                                  lhsT=zb[:, blk * 128:(blk + 1) * 128],
                                                                  rhs=wT[:, blk // 2, :], start=True, stop=True),
                         reads=[R_zb, R_wT], writes=[R_pr])
                    if blk % 4 == 3:
                        for b2 in range(blk - 3, blk + 1):
                            tm, R_tm = tmp[b2 % 2], R_tmp[b2 % 2]
                            S.op("dve", lambda v, b2=b2, pr=pr, tm=tm: v.scalar_tensor_tensor(
                                out=tm[:], in0=pr[:, (b2 % 4) * 128:(b2 % 4 + 1) * 128], scalar=g_fm[:, b2:b2 + 1],
                                in1=Kc[:, b2, :], op0=ALU.mult, op1=ALU.add), reads=[R_pr, R_Kc, R_w], writes=[R_tm])
                            S.op("dve", lambda v, b2=b2, tm=tm: v.tensor_tensor(
                                out=ub[:, b2, q * 128:(q + 1) * 128], in0=tm[:], in1=ub[:, b2, q * 128:(q + 1) * 128],
                                op=ALU.mult), reads=[R_tm], writes=[R_u[cb][b2]])
                for h in range(2):
                    S.op("pe", lambda p, h=h: p.matmul(M[h][:], lhsT=self.ones_b, rhs=ep["bpad"][:, h * 512:(h + 1) * 512],
                                                       start=True, stop=False), reads=[ep["R_bp"], self.R_const], writes=[R_M[h]])
                    for ec in range(16):
                        S.op("pe", lambda p, ec=ec, h=h: p.matmul(M[h][:], lhsT=ub[:, ec, q * 128:(q + 1) * 128],
                                                                  rhs=w_out[:, ec, h * 512:(h + 1) * 512],
                                                                  start=False, stop=(ec == 15)),
                             reads=[R_u[cb][ec], R_w], writes=[R_M[h]])
                self.mixer_epilogue(ep, i, M, R_M)

            for i in range(NT):
                b, q = divmod(i, 2)
                if q == 0:
                    if b + 1 < 8:
                        nb_ = (b + 1) % 2
                        self.load("sp", xTb[nb_][:], xT_src[:, :, (b + 1) * 256:(b + 2) * 256], R_xTb[nb_])
                    U(b)
                A(i)
                if i >= 1:
                    B(i - 1)
            B(NT - 1)
            S.barrier()

    def stage_router(self, li):
        S = self.S
        ne = self.ne
        with ExitStack() as es:
            rw = self.sb(es, "rw", [128, DC, ne], F32)
            rb = self.sb(es, "rb", [128, ne], F32)
            R_p = Res("rp")
            self.load("sp", rw[:], self.router_w[li].rearrange("(c p) e -> p c e", p=128), R_p)
            self.bcast_load(rb[:], self.router_b[li], R_p)
            xn = [self.sb(es, "r_xn", [128, D], F32) for _ in range(2)]
            R_xn = [Res("xn") for _ in range(2)]
            xb = [self.sb(es, "r_xb", [128, D], BF16) for _ in range(2)]
            R_xb = [Res("xb") for _ in range(2)]
            xTf = self.sb(es, "r_xTf", [128, DC, 128], F32)
            R_xTf = [Res("xTf") for _ in range(2)]
            lg = self.sb(es, "r_lg", [128, ne], F32)
            mx = self.sb(es, "r_mx", [128, 8], F32)
            ix = self.sb(es, "r_ix", [128, 8], U32)
            ixf = self.sb(es, "r_ixf", [128, 8], F32)
            sm = self.sb(es, "r_sm", [128, 8], F32)
            e4 = self.sb(es, "r_e4", [128, 4], F32)
            mask = self.sb(es, "r_mask", [128, ne], BF16)
            cm = self.sb(es, "r_cm", [128, ne], BF16)
            slotfull = self.sb(es, "r_sf", [128, ne], F32)
            ovf = self.sb(es, "r_ovf", [128, ne], F32)
            oh4 = self.sb(es, "r_oh4", [128, TOPK, ne], F32)
            junk = self.sb(es, "r_junk", [128, ne], F32)
            slotf = self.sb(es, "r_slotf", [128, 4], F32)
            G = self.sb(es, "r_G", [128, ne], F32)
            R_t = Res("rt")
            R_cm = Res("cm")
            PT = [self.ps(es, "r_PT", [128, 512], F32) for _ in range(2)]
            R_PT = [Res("PT") for _ in range(2)]
            PL = self.ps(es, "r_PL", [128, ne], F32)
            PP = self.ps(es, "r_PP", [128, ne], F32)
            PG = self.ps(es, "r_PG", [32, 128], F32)
            R_PL, R_PP, R_PG = Res("PL"), Res("PP"), Res("PG")
            S.op("dve", lambda v: v.memset(cm[:], 0.0), writes=[R_cm])
            self.load("sp", xn[0][:], self.x1_d[0:128, :], R_xn[0])
            scat_toks = []
            for i in range(NT):
                b = i % 2
                if i + 1 < NT:
                    self.load("sp", xn[1 - b][:], self.x1_d[(i + 1) * 128:(i + 2) * 128, :], R_xn[1 - b])
                S.op("act", lambda a, b=b: a.activation(out=xb[b][:], in_=xn[b][:], func=AF.Identity), reads=[R_xn[b]], writes=[R_xb[b]])
                for half in range(2):
                    for cc in range(4):
                        c = half * 4 + cc
                        S.op("pe", lambda p, c=c, cc=cc, half=half, b=b: p.transpose(
                            out=PT[half][:, cc * 128:(cc + 1) * 128], in_=xn[b][:, c * 128:(c + 1) * 128], identity=self.ident_f),
                            reads=[R_xn[b], self.R_const], writes=[R_PT[half]])
                    S.op("dve", lambda v, half=half: v.tensor_copy(out=xTf[:, half * 4:(half + 1) * 4, :],
                                                                    in_=PT[half][:].rearrange("p (c t) -> p c t", c=4)),
                         reads=[R_PT[half]], writes=[R_xTf[half]])
                for c in range(DC):
                    S.op("pe", lambda p, c=c: p.matmul(PL[:], lhsT=xTf[:, c, :], rhs=rw[:, c, :], start=(c == 0), stop=(c == DC - 1)),
                         reads=[R_xTf[c // 4], R_p], writes=[R_PL])
                S.op("dve", lambda v: v.tensor_tensor(out=lg[:], in0=PL[:], in1=rb[:], op=ALU.add), reads=[R_PL, R_p], writes=[R_t])
                S.op("dve", lambda v: v.max(out=mx[:], in_=lg[:]), writes=[R_t])
                S.op("dve", lambda v: v.max_index(out=ix[:], in_max=mx[:], in_values=lg[:]), writes=[R_t])
                S.op("dve", lambda v: v.tensor_scalar(out=sm[:, 0:1], in0=mx[:, 0:1], scalar1=-1.0, scalar2=None, op0=ALU.mult), writes=[R_t])
                S.op("act", lambda a: a.activation(out=e4[:], in_=mx[:, 0:4], func=AF.Exp, bias=sm[:, 0:1], scale=1.0,
                                                   accum_out=sm[:, 1:2]), writes=[R_t])
                S.op("dve", lambda v: v.reciprocal(sm[:, 2:3], sm[:, 1:2]), writes=[R_t])
                S.op("dve", lambda v, i=i: v.tensor_scalar(out=self.gates_all[:, i, :], in0=e4[:], scalar1=sm[:, 2:3], scalar2=None,
                                                           op0=ALU.mult), reads=[R_t], writes=[self.R_route])
                S.op("dve", lambda v: v.tensor_scalar(out=mask[:], in0=lg[:], scalar1=mx[:, 3:4], scalar2=None, op0=ALU.is_ge), writes=[R_t])
                S.op("pe", lambda p: p.matmul(PP[:], lhsT=self.ones_b, rhs=cm[:], start=True, stop=False),
                     reads=[R_cm, self.R_const], writes=[R_PP])
                S.op("pe", lambda p: p.matmul(PP[:], lhsT=self.tri_b, rhs=mask[:], start=False, stop=True),
                     reads=[R_t, self.R_const], writes=[R_PP])
                S.op("dve", lambda v: v.tensor_tensor(out=cm[:], in0=cm[:], in1=mask[:], op=ALU.add), reads=[R_t], writes=[R_cm])
                S.op("dve", lambda v: v.tensor_tensor(out=slotfull[:], in0=PP[:], in1=self.slotbase[:, 0:ne], op=ALU.add),
                     reads=[R_PP, self.R_const], writes=[R_t])
                S.op("dve", lambda v: v.tensor_scalar(out=ovf[:], in0=PP[:], scalar1=float(CAP), scalar2=1.0e7, op0=ALU.is_ge, op1=ALU.mult),
                     reads=[R_PP], writes=[R_t])
                S.op("dve", lambda v: v.tensor_tensor(out=slotfull[:], in0=slotfull[:], in1=ovf[:], op=ALU.add), writes=[R_t])
                S.op("dve", lambda v: v.tensor_copy(out=ixf[:], in_=ix[:]), writes=[R_t])
                S.op("dve", lambda v: v.tensor_tensor(out=oh4[:], in0=self.iota_e[:, 0:ne].unsqueeze(1).to_broadcast([128, TOPK, ne]),
                                                      in1=ixf[:, 0:TOPK].unsqueeze(2).to_broadcast([128, TOPK, ne]), op=ALU.is_equal),
                     reads=[self.R_const], writes=[R_t])
                S.op("dve", lambda v: v.tensor_tensor(out=oh4[:], in0=oh4[:], in1=slotfull[:].unsqueeze(1).to_broadcast([128, TOPK, ne]),
                                                      op=ALU.mult), writes=[R_t])
                S.op("dve", lambda v: v.tensor_reduce(out=slotf[:, 0:TOPK], in_=oh4[:], axis=mybir.AxisListType.X, op=ALU.add), writes=[R_t])
                S.op("act", lambda a: a.activation(out=junk[:], in_=lg[:], func=AF.Exp, bias=sm[:, 0:1], scale=1.0), writes=[R_t])
                S.op("dve", lambda v: v.scalar_tensor_tensor(out=G[:], in0=junk[:], scalar=sm[:, 2:3], in1=mask[:], op0=ALU.mult, op1=ALU.mult),
                     writes=[R_t])
                tok_sl = S.op("dve", lambda v, i=i: v.tensor_copy(out=self.slots_all[:, i, :], in_=slotf[:]), reads=[R_t], writes=[self.R_route])
                S.op("pe", lambda p: p.transpose(out=PG[:], in_=G[:], identity=self.ident_f), reads=[R_t, self.R_const], writes=[R_PG])
                S.op("act", lambda a, i=i: a.activation(out=self.GT_all[:, i, :], in_=PG[:], func=AF.Identity), reads=[R_PG], writes=[self.R_route])
                for k in range(TOPK):
                    t_ = S.dma("pool", lambda g, i=i, k=k, b=b: g.indirect_dma_start(
                        out=self.xs_d[:, :], out_offset=bass.IndirectOffsetOnAxis(ap=self.slots_all[:, i, k:k + 1], axis=0),
                        in_=xb[b][:, :], in_offset=None, bounds_check=self.bc_reg, oob_is_err=False),
                        reads=[R_xb[b]], extra=[tok_sl])
                    scat_toks.append(t_)
            S.barrier()

    def stage_experts(self, li):
        S = self.S
        ne = self.ne
        NSLOT = 6
        with ExitStack() as es:
            ring = [self.sb(es, "ring", [128, DC, 1024], BF16) for _ in range(NSLOT)]
            R_ring = [Res("ring") for _ in range(NSLOT)]
            bup = self.sb(es, "bup", [128, ne * 16], F32)
            bl1 = self.sb(es, "bl1", [128, ne * 16], F32)
            R_b = Res("bup")
            self.load("sp", bup[:], self.fm_up[li], R_b)
            S.op("dve", lambda v: v.tensor_scalar(out=bl1[:], in0=bup[:], scalar1=1.0, scalar2=None, op0=ALU.add), reads=[R_b], writes=[R_b])
            rows = [self.sb(es, "rows", [128, NSB, D], BF16) for _ in range(2)]
            R_rows = [Res("rows") for _ in range(2)]
            xsT = [self.sb(es, "xsT", [128, DC, CAP], BF16) for _ in range(2)]
            R_xsT = [Res("xsT") for _ in range(2)]
            act = [self.sb(es, "act", [128, DC, CAP], BF16) for _ in range(2)]
            R_act = [[Res("act") for _ in range(DC)] for _ in range(2)]
            gc = [self.sb(es, "gc", [128, CAP], F32) for _ in range(2)]
            m1 = [self.sb(es, "m1", [128, CAP], F32) for _ in range(2)]
            l2 = [self.sb(es, "l2", [128, CAP], F32) for _ in range(2)]
            R_gc = [Res("gc") for _ in range(2)]
            R_m1 = [Res("m1") for _ in range(2)]
            R_l2 = [Res("l2") for _ in range(2)]
            ysb = [self.sb(es, "ysb", [128, D], F32) for _ in range(3)]
            R_ysb = [Res("ysb") for _ in range(3)]
            PGp = [self.ps(es, "e_PG", [128, 512], F32) for _ in range(2)]
            PLp = [self.ps(es, "e_PL", [128, 512], F32) for _ in range(2)]
            PY = [self.ps(es, "e_PY", [128, 512], F32) for _ in range(2)]
            PTp = [self.ps(es, "e_PT", [128, D], BF16) for _ in range(2)]
            R_PGp = [Res("PG") for _ in range(2)]
            R_PLp = [Res("PL") for _ in range(2)]
            R_PY = [Res("PY") for _ in range(2)]
            R_PTp = [Res("PT") for _ in range(2)]
            nload = [0]
            slot_of = {}

            def issue_weights(e, parts=("g", "l", "d")):
                srcu = self.w_up[li, e].rearrange("(c p) n -> p c n", p=128)
                srcd = self.w_down[li, e].rearrange("(c p) n -> p c n", p=128)
                srcs = {"g": srcu[:, :, 0:1024], "l": srcu[:, :, 1024:2048], "d": srcd}
                for part in parts:
                    src = srcs[part]
                    s_ = (3 * e + "gld".index(part)) % NSLOT
                    slot_of[(e, part)] = s_
                    S.dma("pool", lambda g, s_=s_, src=src: g.dma_start(out=ring[s_][:], in_=src), writes=[R_ring[s_]])

            def issue_rows(e):
                b = e % 2
                src = self.xs_d[e * CAP:(e + 1) * CAP, :].rearrange("(s p) d -> p s d", p=128)
                self.load("sp", rows[b][:], src, R_rows[b])

            issue_weights(0)
            issue_rows(0)
            issue_weights(1)
            rp = 0
            ry = 0
            rt = 0
            for e in range(ne):
                b = e % 2
                if e + 1 < ne:
                    issue_rows(e + 1)
                for sb_ in range(NSB):
                    pt, R_pt = PTp[rt % 2], R_PTp[rt % 2]
                    rt += 1
                    for c in range(DC):
                        S.op("pe", lambda p, c=c, sb_=sb_, pt=pt, b=b: p.transpose(out=pt[:, c * 128:(c + 1) * 128],
                                                                                    in_=rows[b][:, sb_, c * 128:(c + 1) * 128],
                                                                                    identity=self.ident_b),
                             reads=[R_rows[b], self.R_const], writes=[R_pt])
                    S.op("act", lambda a, sb_=sb_, pt=pt, b=b: a.activation(out=xsT[b][:, :, sb_ * 128:(sb_ + 1) * 128],
                                                                            in_=pt[:].rearrange("p (c t) -> p c t", c=DC), func=AF.Identity),
                         reads=[R_pt], writes=[R_xsT[b]])
                wg, R_wg = ring[slot_of[(e, "g")]], R_ring[slot_of[(e, "g")]]
                wl, R_wl = ring[slot_of[(e, "l")]], R_ring[slot_of[(e, "l")]]
                wd, R_wd = ring[slot_of[(e, "d")]], R_ring[slot_of[(e, "d")]]
                for fb in range(8):
                    pg, R_pg = PGp[rp % 2], R_PGp[rp % 2]
                    pl, R_pl = PLp[rp % 2], R_PLp[rp % 2]
                    w_ = rp % 2
                    rp += 1
                    for c in range(DC):
                        S.op("pe", lambda p, c=c, fb=fb, pg=pg, wg=wg, b=b: p.matmul(pg[:, 0:CAP], lhsT=wg[:, c, fb * 128:(fb + 1) * 128],
                                                                                      rhs=xsT[b][:, c, :], start=(c == 0), stop=(c == DC - 1)),
                             reads=[R_wg, R_xsT[b]], writes=[R_pg])
                    for c in range(DC):
                        S.op("pe", lambda p, c=c, fb=fb, pl=pl, wl=wl, b=b: p.matmul(pl[:, 0:CAP], lhsT=wl[:, c, fb * 128:(fb + 1) * 128],
                                                                                      rhs=xsT[b][:, c, :], start=(c == 0), stop=(c == DC - 1)),
                             reads=[R_wl, R_xsT[b]], writes=[R_pl])
                    col_g = e * 16 + fb
                    col_l = e * 16 + 8 + fb
                    S.op("dve", lambda v, pg=pg, w_=w_, col_g=col_g: v.tensor_scalar(out=gc[w_][:], in0=pg[:, 0:CAP], scalar1=bup[:, col_g:col_g + 1],
                                                                                     scalar2=LIMIT, op0=ALU.add, op1=ALU.min),
                         reads=[R_pg, R_b], writes=[R_gc[w_]])
                    S.op("act", lambda a, w_=w_: a.activation(out=m1[w_][:], in_=gc[w_][:], func=AF.Sigmoid, scale=SW_ALPHA),
                         reads=[R_gc[w_]], writes=[R_m1[w_]])
                    S.op("dve", lambda v, w_=w_: v.tensor_tensor(out=m1[w_][:], in0=m1[w_][:], in1=gc[w_][:], op=ALU.mult),
                         reads=[R_gc[w_]], writes=[R_m1[w_]])
                    S.op("dve", lambda v, pl=pl, w_=w_, col_l=col_l: v.tensor_scalar(out=l2[w_][:], in0=pl[:, 0:CAP], scalar1=bl1[:, col_l:col_l + 1],
                                                                                     scalar2=1.0 - LIMIT, op0=ALU.add, op1=ALU.max),
                         reads=[R_pl, R_b], writes=[R_l2[w_]])
                    S.op("dve", lambda v, w_=w_, fb=fb, b=b: v.scalar_tensor_tensor(out=act[b][:, fb, :], in0=l2[w_][:], scalar=1.0 + LIMIT,
                                                                                    in1=m1[w_][:], op0=ALU.min, op1=ALU.mult),
                         reads=[R_l2[w_], R_m1[w_]], writes=[R_act[b][fb]])
                if e + 2 < ne:
                    issue_weights(e + 2, ("g", "l"))
                for sb_ in range(NSB):
                    yb, R_yb = ysb[(e * NSB + sb_) % 3], R_ysb[(e * NSB + sb_) % 3]
                    for h in range(2):
                        py, R_py = PY[ry % 2], R_PY[ry % 2]
                        ry += 1
                        for fc in range(DC):
                            S.op("pe", lambda p, fc=fc, sb_=sb_, h=h, py=py, wd=wd, b=b: p.matmul(
                                py[:], lhsT=act[b][:, fc, sb_ * 128:(sb_ + 1) * 128], rhs=wd[:, fc, h * 512:(h + 1) * 512],
                                start=(fc == 0), stop=(fc == DC - 1)), reads=[R_act[b][fc], R_wd], writes=[R_py])
                        S.op("act", lambda a, h=h, py=py, yb=yb: a.activation(out=yb[:, h * 512:(h + 1) * 512], in_=py[:], func=AF.Identity),
                             reads=[R_py], writes=[R_yb])
                    r0 = e * CAP + sb_ * 128
                    S.dma("sp", lambda g, r0=r0, yb=yb: g.dma_start(out=self.ys_d[r0:r0 + 128, :], in_=yb[:]), reads=[R_yb])
                if e + 2 < ne:
                    issue_weights(e + 2, ("d",))
            S.barrier()

    def stage_combine(self, li, dst, make_xT):
        S = self.S
        ne = self.ne
        with ExitStack() as es:
            self.alloc_eps(es)
            bd = self.sb(es, "c_bd", [32, D], BF16)
            g2 = self.sb(es, "c_g2", [128, D], F32)
            b2 = self.sb(es, "c_b2", [128, D], F32)
            R_p = Res("cp")
            S.dma("pool", lambda g: g.dma_start(out=bd[0:ne, :], in_=self.b_down[li]), writes=[R_p])
            self.bcast_load(g2[:], self.ln_g[li, 1], R_p)
            self.bcast_load(b2[:], self.ln_b[li, 1], R_p)
            NB = 3
            x1 = [self.sb(es, "c_x1", [128, D], F32) for _ in range(NB)]
            R_x1 = [Res("x1") for _ in range(NB)]
            yk = [[self.sb(es, "c_yk", [128, D], F32) for _ in range(TOPK)] for _ in range(NB)]
            R_yk = [[Res("yk") for _ in range(TOPK)] for _ in range(NB)]
            acc = self.sb(es, "c_acc", [128, D], F32)
            R_acc = Res("acc")
            xn = [self.sb(es, "c_xn", [128, D], F32) for _ in range(2)]
            R_xn = [Res("xn") for _ in range(2)]
            xb = [self.sb(es, "c_xb", [128, D], BF16) for _ in range(2)]
            R_xb = [Res("xb") for _ in range(2)]
            stage = [self.sb(es, "c_st", [128, DC, 512], BF16) for _ in range(2)]
            R_st = [Res("st") for _ in range(2)]
            ln = self.alloc_ln_bufs(es, "c")
            PB = [self.ps(es, "c_PB", [128, 512], F32) for _ in range(2)]
            R_PB = [Res("PB") for _ in range(2)]
            pst = [self.ps(es, "c_pst", [128, D], BF16) for _ in range(2)]
            R_pst = [Res("pst") for _ in range(2)]

            def prefetch(i):
                b = i % NB
                self.load("sp", x1[b][:], self.x1_d[i * 128:(i + 1) * 128, :], R_x1[b])
                for k in range(TOPK):
                    S.dma("pool", lambda g, i=i, k=k, b=b: g.indirect_dma_start(
                        out=yk[b][k][:, :], out_offset=None, in_=self.ys_d[:, :],
                        in_offset=bass.IndirectOffsetOnAxis(ap=self.slots_all[:, i, k:k + 1], axis=0),
                        bounds_check=self.bc_reg, oob_is_err=False), writes=[R_yk[b][k]], reads=[self.R_route])

            prefetch(0)
            prefetch(1)
            for i in range(NT):
                b = i % 2
                b3 = i % NB
                if i + 2 < NT:
                    prefetch(i + 2)
                for h in range(2):
                    S.op("pe", lambda p, h=h, i=i: p.matmul(PB[h][:], lhsT=self.GT_all[0:ne, i, :], rhs=bd[0:ne, h * 512:(h + 1) * 512],
                                                            start=True, stop=True), reads=[self.R_route, R_p], writes=[R_PB[h]])
                    S.op("dve", lambda v, h=h, b3=b3: v.scalar_tensor_tensor(out=acc[:, h * 512:(h + 1) * 512], in0=x1[b3][:, h * 512:(h + 1) * 512],
                                                                             scalar=ALPHA, in1=PB[h][:], op0=ALU.mult, op1=ALU.add),
                         reads=[R_x1[b3], R_PB[h]], writes=[R_acc])
                for k in range(TOPK):
                    S.op("dve", lambda v, k=k, b3=b3, i=i: v.scalar_tensor_tensor(out=acc[:], in0=yk[b3][k][:], scalar=self.gates_all[:, i, k:k + 1],
                                                                                  in1=acc[:], op0=ALU.mult, op1=ALU.add),
                         reads=[R_yk[b3][k], self.R_route], writes=[R_acc])
                self.ln_tile(ln, acc[:], R_acc, g2[:], b2[:], xn[b][:], R_xn[b], R_p)
                S.dma("sp", lambda g, i=i, b=b: g.dma_start(out=dst[i * 128:(i + 1) * 128, :], in_=xn[b][:]), reads=[R_xn[b]])
                if make_xT:
                    S.op("act", lambda a, b=b: a.activation(out=xb[b][:], in_=xn[b][:], func=AF.Identity), reads=[R_xn[b]], writes=[R_xb[b]])
                    sb_ = (i // 4) % 2
                    self.xT_from_tile(xb[b], R_xb[b], pst[b], R_pst[b], stage[sb_], R_st[sb_], i % 4, eng="act")
                    if i % 4 == 3:
                        self.store_xT_block(stage[sb_], R_st[sb_], i // 4)
            S.barrier()


def make_consts():
    c = np.zeros((128, 128 * 3 + 32 + 32 + 64), np.float32)
    c[:, 0:128] = np.eye(128, dtype=np.float32)
    p = np.arange(128)
    c[:, 128:256] = (p[:, None] < p[None, :]).astype(np.float32)
    c[:, 256:384] = 1.0
    c[:, 384:416] = np.arange(32, dtype=np.float32)[None, :]
    c[:, 416:448] = (np.arange(32, dtype=np.float32) * CAP)[None, :]
    for g in range(4):
        win = 2 ** (g + 1)
        c[:, 448 + g * 16:448 + (g + 1) * 16] = (1.0 / np.minimum(np.arange(16) + 1, win)).astype(np.float32)[None, :]
    return c


def fm(v):
    v = np.asarray(v, np.float32)
    return np.ascontiguousarray(v.reshape(-1, 128).T)


def prepare_shared(inp, layers, ne=NE):
    ev = [l // 2 for l in layers if l % 2 == 0]
    od = [l // 2 for l in layers if l % 2 == 1]
    ev_sel = ev if ev else [0]
    od_sel = od if od else [0]
    f32 = lambda a: np.ascontiguousarray(a, dtype=np.float32)
    out = {}
    out["ev_w_in"] = f32(inp["ev_w_in"][ev_sel])
    out["ev_w_out"] = f32(inp["ev_w_out"][ev_sel])
    out["pool_w"] = f32(inp["pool_w"][ev_sel])
    out["od_w_in"] = f32(inp["od_w_in"][od_sel])
    out["od_w_out"] = f32(inp["od_w_out"][od_sel])
    out["sgu_wT"] = f32(np.transpose(inp["sgu_w"][od_sel], (0, 1, 3, 2)))
    out["od_bv"] = f32(inp["od_b_in"][od_sel][:, 2048:4096])
    out["sgu_b"] = f32(inp["sgu_b"][od_sel].reshape(len(od_sel), 1024))
    out["b_out"] = f32(np.stack([inp["ev_b_out"][l // 2] if l % 2 == 0 else inp["od_b_out"][l // 2] for l in layers]))
    out["ln_g"] = f32(inp["ln_g"][layers])
    out["ln_b"] = f32(inp["ln_b"][layers])
    out["router_w"] = f32(inp["router_w"][layers][:, :, :ne])
    out["router_b"] = f32(inp["router_b"][layers][:, :ne])
    out["exp_w_up"] = f32(inp["exp_w_up"][layers][:, :ne])
    out["exp_w_down"] = f32(inp["exp_w_down"][layers][:, :ne])
    out["exp_b_down"] = f32(inp["exp_b_down"][layers][:, :ne])
    fe = []
    for i in ev_sel:
        cw = np.asarray(inp["conv_w"][i], np.float32)
        cw_fm = np.transpose(cw.reshape(31, 4, 128), (2, 1, 0)).reshape(128, 124)
        fe.append(np.concatenate([fm(inp["ev_b_in"][i]), fm(inp["pool_b"][i].reshape(-1)), fm(inp["pool_scale"][i]),
                                  fm(inp["conv_b"][i]), fm(inp["conv_ln_g"][i]), fm(inp["conv_ln_b"][i]), cw_fm], axis=1))
    out["fm_ev"] = f32(np.stack(fe))
    fo = []
    for i in od_sel:
        fo.append(np.concatenate([fm(inp["od_b_in"][i][:2048]), fm(inp["sgu_ln_g"][i]), fm(inp["sgu_ln_b"][i])], axis=1))
    out["fm_od"] = f32(np.stack(fo))
    fu = []
    for l in layers:
        bu = np.asarray(inp["exp_b_up"][l][:ne], np.float32)
        fu.append(np.transpose(bu.reshape(ne, 16, 128), (2, 0, 1)).reshape(128, ne * 16))
    out["fm_up"] = f32(np.stack(fu))
    out["consts"] = make_consts()
    return out


_PROG_CACHE = {}


def run_layers(inp, layers, cores, ne=NE, debug=False):
    key = (tuple(layers), ne, debug)
    if key not in _PROG_CACHE:
        _PROG_CACHE[key] = Prog(layers, ne, debug).build()
    nc = _PROG_CACHE[key]
    shared = prepare_shared(inp, layers, ne)
    x = np.asarray(inp["x"], np.float32)
    in_maps = []
    for c in cores:
        m = dict(shared)
        m["x"] = np.ascontiguousarray(x[c])
        in_maps.append(m)
    res = run_bass_kernel_spmd(nc, in_maps, core_ids=list(range(len(cores))))
    return res.results


def kernel(**inputs):
    inp = {k: np.asarray(v) for k, v in inputs.items()}
    res = run_layers(inp, list(range(DEPTH)), list(range(8)))
    return np.stack([r["y"] for r in res], axis=0).astype(np.float32)
```

```python
from contextlib import ExitStack

import numpy as np
import concourse.bass as bass
import concourse.mybir as mybir
from concourse.bass_utils import run_bass_kernel_spmd

F32 = mybir.dt.float32
BF16 = mybir.dt.bfloat16
I32 = mybir.dt.int32
U32 = mybir.dt.uint32
AF = mybir.ActivationFunctionType
ALU = mybir.AluOpType

T = 2048
D = 1024
NT = 16
DC = 8
NE = 32
TOPK = 4
import os
CAP = int(os.environ.get("K_CAP", "384"))
NSB = CAP // 128
DEPTH = 4
ALPHA = float((2 * DEPTH) ** 0.25)
EPS = 1e-5
LIMIT = 7.0
SW_ALPHA = 1.702

EV_FM = 12 + 4 + 4 + 4 + 4 + 4 + 124
OD_FM = 16 + 16 + 16
UP_FM = NE * 16


class Sem:
    _n = 0

    def __init__(self, h):
        self.h = h
        self.cnt = 0
        Sem._n += 1
        self.id = Sem._n


class Res:
    __slots__ = ("name", "w", "r", "dsem")

    def __init__(self, name):
        self.name = name
        self.w = {}
        self.r = {}
        self.dsem = None


class Sched:
    def __init__(self, nc, es):
        self.nc = nc
        self.es = es
        self.eng = {"pe": nc.tensor, "dve": nc.vector, "act": nc.scalar, "pool": nc.gpsimd, "sp": nc.sync}
        self.esem = {}
        self.waited = {k: {} for k in self.eng}
        self.dma_pending = {}
        self.bsem = None
        self.nsem = 0
        self.free_dsems = []
        self.stage_dsems = []

    def new_sem(self, name):
        s = Sem(self.es.enter_context(self.nc.semaphore(f"{name}{self.nsem}")))
        self.nsem += 1
        return s

    def _mark(self, e, instr):
        if e not in self.esem:
            self.esem[e] = self.new_sem("e" + e)
        s = self.esem[e]
        s.cnt += 1
        instr.then_inc(s.h, 1)
        return (s, s.cnt, e)

    def wait(self, e, tok):
        if tok is None:
            return
        s, val, prod = tok
        if prod == "pe" and e == "pe":
            return
        w = self.waited[e]
        if w.get(s.id, 0) >= val:
            return
        w[s.id] = val
        self.eng[e].wait_ge(s.h, val)

    def _pre(self, e, reads, writes, extra):
        for r in reads:
            for t in r.w.values():
                self.wait(e, t)
        for w in writes:
            for t in w.w.values():
                self.wait(e, t)
            for t in w.r.values():
                self.wait(e, t)
        for t in extra:
            self.wait(e, t)

    def _post(self, tok, reads, writes):
        for r in reads:
            r.r[tok[0].id] = tok
        for w in writes:
            w.w = {tok[0].id: tok}
            w.r = {}

    def op(self, e, fn, reads=(), writes=(), extra=()):
        self._pre(e, reads, writes, extra)
        instr = fn(self.eng[e])
        tok = self._mark(e, instr)
        self._post(tok, reads, writes)
        return tok

    def dma(self, e, fn, reads=(), writes=(), extra=()):
        self._pre(e, reads, writes, extra)
        owner = writes[0] if writes else reads[0]
        if owner.dsem is None:
            owner.dsem = self.free_dsems.pop() if self.free_dsems else self.new_sem("d")
            self.stage_dsems.append(owner.dsem)
        s = owner.dsem
        instr = fn(self.eng[e])
        s.cnt += 16
        instr.then_inc(s.h, 16)
        tok = (s, s.cnt, "dma")
        self._post(tok, reads, writes)
        self.dma_pending[s.id] = tok
        return tok

    def barrier(self):
        for e, s in self.esem.items():
            if e != "sp" and s.cnt:
                self.wait("sp", (s, s.cnt, e))
        for tok in self.dma_pending.values():
            self.wait("sp", tok)
        self.dma_pending = {}
        self.free_dsems.extend(self.stage_dsems)
        self.stage_dsems = []
        if self.bsem is None:
            self.bsem = self.new_sem("bar")
        b = self.bsem
        b.cnt += 1
        self.nc.sync.sem_inc(b.h, 1)
        tok = (b, b.cnt, "sp")
        for e in self.eng:
            if e != "sp":
                self.wait(e, tok)


class Prog:
    def __init__(self, layers, ne=NE, debug=False):
        self.layers = list(layers)
        self.ne = ne
        self.debug = debug
        self.n_even = sum(1 for l in self.layers if l % 2 == 0)
        self.n_odd = sum(1 for l in self.layers if l % 2 == 1)
        self.nl = len(self.layers)

    def sb(self, es, name, shape, dt):
        self._uid += 1
        return es.enter_context(self.nc.sbuf_tensor(f"{name}_{self._uid}", list(shape), dt))

    def ps(self, es, name, shape, dt):
        self._uid += 1
        return es.enter_context(self.nc.psum_tensor(f"{name}_{self._uid}", list(shape), dt))

    def din(self, name, shape, dt=F32):
        return self.nc.dram_tensor(name, list(shape), dt, kind="ExternalInput").ap()

    def dscratch(self, name, shape, dt):
        kind = "ExternalOutput" if self.debug else "Internal"
        return self.nc.dram_tensor(name, list(shape), dt, kind=kind).ap()

    def load(self, e, out, in_, res, extra=()):
        return self.S.dma(e, lambda g: g.dma_start(out=out, in_=in_), writes=[res], extra=extra)

    def bcast_load(self, out, row_ap, res):
        return self.S.dma("sp", lambda g: g.dma_start(out=out, in_=row_ap.partition_broadcast(128)), writes=[res])

    def build(self):
        nc = bass.Bass("TRN2", target_bir_lowering=False)
        self.nc = nc
        self._uid = 0
        nl, ne = self.nl, self.ne
        n_even, n_odd = max(self.n_even, 1), max(self.n_odd, 1)
        self.x_in = self.din("x", [T, D])
        self.ev_w_in = self.din("ev_w_in", [n_even, D, 1536])
        self.ev_w_out = self.din("ev_w_out", [n_even, D, D])
        self.pool_w = self.din("pool_w", [n_even, 4, 128, 128])
        self.od_w_in = self.din("od_w_in", [n_odd, D, 4096])
        self.od_w_out = self.din("od_w_out", [n_odd, 2048, D])
        self.sgu_wT = self.din("sgu_wT", [n_odd, 8, 128, 128])
        self.od_bv = self.din("od_bv", [n_odd, 2048])
        self.sgu_b = self.din("sgu_b", [n_odd, 1024])
        self.b_out = self.din("b_out", [nl, D])
        self.ln_g = self.din("ln_g", [nl, 2, D])
        self.ln_b = self.din("ln_b", [nl, 2, D])
        self.router_w = self.din("router_w", [nl, D, ne])
        self.router_b = self.din("router_b", [nl, ne])
        self.w_up = self.din("exp_w_up", [nl, ne, D, 2 * D])
        self.w_down = self.din("exp_w_down", [nl, ne, D, D])
        self.b_down = self.din("exp_b_down", [nl, ne, D])
        self.fm_ev = self.din("fm_ev", [n_even, 128, EV_FM])
        self.fm_od = self.din("fm_od", [n_odd, 128, OD_FM])
        self.fm_up = self.din("fm_up", [nl, 128, ne * 16])
        self.consts = self.din("consts", [128, 128 * 3 + 32 + 32 + 64])
        self.y_out = nc.dram_tensor("y", [T, D], F32, kind="ExternalOutput").ap()
        self.x1_d = self.dscratch("x1_d", [T, D], F32)
        self.x2_d = self.dscratch("x2_d", [T, D], F32)
        self.xT_d = self.dscratch("xT_d", [D, T], BF16)
        self.xs_d = self.dscratch("xs_d", [ne * CAP, D], BF16)
        self.ys_d = self.dscratch("ys_d", [ne * CAP, D], F32)

        with ExitStack() as top:
            S = self.S = Sched(nc, top)
            self.bc_reg = nc.gpsimd.to_reg(ne * CAP - 1)
            self.c_f32 = self.sb(top, "c_f32", [128, 128 * 3 + 32 + 32 + 64], F32)
            self.c_bf = self.sb(top, "c_bf", [128, 128 * 3], BF16)
            self.gates_all = self.sb(top, "gates_all", [128, NT, TOPK], F32)
            self.slots_all = self.sb(top, "slots_all", [128, NT, TOPK], I32)
            self.GT_all = self.sb(top, "GT_all", [32, NT, 128], BF16)
            self.R_const = Res("const")
            self.R_route = Res("route")
            self.load("sp", self.c_f32[:], self.consts, self.R_const)
            S.dma("pool", lambda g: g.dma_start(out=self.c_bf[:], in_=self.consts[:, 0:384]), writes=[self.R_const])
            self.ident_f = self.c_f32[:, 0:128]
            self.ident_b = self.c_bf[:, 0:128]
            self.tri_b = self.c_bf[:, 128:256]
            self.ones_b = self.c_bf[:, 256:384]
            self.iota_e = self.c_f32[:, 384:416]
            self.slotbase = self.c_f32[:, 416:448]
            self.invcnt = self.c_f32[:, 448:512]
            S.barrier()

            self.stage_xT0()
            x_res = self.x_in
            for li, l in enumerate(self.layers):
                last = li == nl - 1
                if l % 2 == 0:
                    self.stage_mix_even(li, l, x_res)
                else:
                    self.stage_mix_odd(li, l, x_res)
                self.stage_experts(li)
                self.stage_combine(li, self.y_out if last else self.x2_d, make_xT=not last)
                x_res = self.x2_d
            S.barrier()
        return nc

    def ln_tile(self, es_bufs, s_ap, s_res, g_bc, b_bc, out_ap, out_res, p_res):
        S = self.S
        stats, mv, sc, z, st_res, z_res = es_bufs
        for h in range(2):
            S.op("dve", lambda v, h=h: v.bn_stats(stats[:, h * 6:(h + 1) * 6], s_ap[:, h * 512:(h + 1) * 512]),
                 reads=[s_res], writes=[st_res])
        S.op("dve", lambda v: v.bn_aggr(mv[:, 0:2], stats[:, 0:12]), reads=[st_res], writes=[st_res])
        S.op("act", lambda a: a.activation(out=sc[:, 0:1], in_=mv[:, 1:2], func=AF.Sqrt, bias=self.eps_ap, scale=1.0),
             reads=[st_res, self.R_eps], writes=[st_res])
        S.op("dve", lambda v: v.reciprocal(sc[:, 1:2], sc[:, 0:1]), reads=[st_res], writes=[st_res])
        S.op("dve", lambda v: v.tensor_scalar(out=sc[:, 2:3], in0=mv[:, 0:1], scalar1=sc[:, 1:2], scalar2=-1.0,
                                              op0=ALU.mult, op1=ALU.mult), reads=[st_res], writes=[st_res])
        S.op("act", lambda a: a.activation(out=z[:], in_=s_ap, func=AF.Identity, bias=sc[:, 2:3], scale=sc[:, 1:2]),
             reads=[s_res, st_res], writes=[z_res])
        S.op("dve", lambda v: v.tensor_tensor(out=z[:], in0=z[:], in1=g_bc, op=ALU.mult),
             reads=[p_res], writes=[z_res])
        S.op("dve", lambda v: v.tensor_tensor(out=out_ap, in0=z[:], in1=b_bc, op=ALU.add),
             reads=[z_res, p_res], writes=[out_res])

    def alloc_ln_bufs(self, es, tag):
        stats = self.sb(es, "lnstats" + tag, [128, 12], F32)
        mv = self.sb(es, "lnmv" + tag, [128, 2], F32)
        sc = self.sb(es, "lnsc" + tag, [128, 4], F32)
        z = self.sb(es, "lnz" + tag, [128, D], F32)
        return (stats, mv, sc, z, Res("lnst" + tag), Res("lnz" + tag))

    def alloc_eps(self, es):
        self.eps_t = self.sb(es, "eps", [128, 1], F32)
        self.R_eps = Res("eps")
        self.S.op("dve", lambda v: v.memset(self.eps_t[:], EPS), writes=[self.R_eps])
        self.eps_ap = self.eps_t[:, 0:1]

    def xT_from_tile(self, xb, xb_res, pst, pst_res, stage, stage_res, q, eng="dve"):
        S = self.S
        for c in range(DC):
            S.op("pe", lambda p, c=c: p.transpose(out=pst[:, c * 128:(c + 1) * 128], in_=xb[:, c * 128:(c + 1) * 128],
                                                  identity=self.ident_b),
                 reads=[xb_res, self.R_const], writes=[pst_res])
        if eng == "dve":
            S.op("dve", lambda v: v.tensor_copy(out=stage[:, :, q * 128:(q + 1) * 128],
                                                in_=pst[:].rearrange("p (c t) -> p c t", c=DC)),
                 reads=[pst_res], writes=[stage_res])
        else:
            S.op("act", lambda a: a.activation(out=stage[:, :, q * 128:(q + 1) * 128],
                                               in_=pst[:].rearrange("p (c t) -> p c t", c=DC), func=AF.Identity),
                 reads=[pst_res], writes=[stage_res])

    def store_xT_block(self, stage, stage_res, blk):
        dst = self.xT_d.rearrange("(c p) t -> p c t", p=128)[:, :, blk * 512:(blk + 1) * 512]
        self.S.dma("sp", lambda g: g.dma_start(out=dst, in_=stage[:]), reads=[stage_res])

    def stage_xT0(self):
        S = self.S
        with ExitStack() as es:
            xt = [self.sb(es, "s0x", [128, D], F32) for _ in range(2)]
            xb = [self.sb(es, "s0b", [128, D], BF16) for _ in range(2)]
            stage = [self.sb(es, "s0st", [128, DC, 512], BF16) for _ in range(2)]
            pst = [self.ps(es, "s0ps", [128, D], BF16) for _ in range(2)]
            R_xt = [Res("xt") for _ in range(2)]
            R_xb = [Res("xb") for _ in range(2)]
            R_st = [Res("st") for _ in range(2)]
            R_ps = [Res("ps") for _ in range(2)]
            for i in range(NT):
                b = i % 2
                self.load("sp", xt[b][:], self.x_in[i * 128:(i + 1) * 128, :], R_xt[b])
                S.op("act", lambda a, b=b: a.activation(out=xb[b][:], in_=xt[b][:], func=AF.Identity),
                     reads=[R_xt[b]], writes=[R_xb[b]])
                sb_ = (i // 4) % 2
                self.xT_from_tile(xb[b], R_xb[b], pst[b], R_ps[b], stage[sb_], R_st[sb_], i % 4)
                if i % 4 == 3:
                    self.store_xT_block(stage[sb_], R_st[sb_], i // 4)
            S.barrier()

    def alloc_epilogue(self, es, li, n_xn=2):
        S = self.S
        ep = {}
        ep["bpad"] = self.sb(es, "bpad", [128, D], BF16)
        ep["R_bp"] = Res("bpad")
        S.op("dve", lambda v: v.memset(ep["bpad"][:], 0.0), writes=[ep["R_bp"]])
        S.dma("pool", lambda g: g.dma_start(out=ep["bpad"][0:1, :], in_=self.b_out[li:li + 1, :]), writes=[ep["R_bp"]])
        ep["g"] = self.sb(es, "ln1g", [128, D], F32)
        ep["b"] = self.sb(es, "ln1b", [128, D], F32)
        ep["R_p"] = Res("ep_params")
        self.bcast_load(ep["g"][:], self.ln_g[li, 0], ep["R_p"])
        self.bcast_load(ep["b"][:], self.ln_b[li, 0], ep["R_p"])
        ep["xres"] = [self.sb(es, "xres", [128, D], F32) for _ in range(2)]
        ep["R_xres"] = [Res("xres") for _ in range(2)]
        ep["s"] = self.sb(es, "eps_s", [128, D], F32)
        ep["R_s"] = Res("s")
        ep["xn"] = [self.sb(es, "xn", [128, D], F32) for _ in range(n_xn)]
        ep["R_xn"] = [Res("xn") for _ in range(n_xn)]
        ep["ln"] = self.alloc_ln_bufs(es, "m")
        return ep

    def prefetch_xres(self, ep, x_src, i):
        b = i % 2
        self.load("sp", ep["xres"][b][:], x_src[i * 128:(i + 1) * 128, :], ep["R_xres"][b])

    def mixer_epilogue(self, ep, i, mix_ps, R_mix):
        S = self.S
        b = i % 2
        xres, R_x = ep["xres"][b], ep["R_xres"][b]
        s, R_s = ep["s"], ep["R_s"]
        for h in range(2):
            S.op("dve", lambda v, h=h: v.scalar_tensor_tensor(out=s[:, h * 512:(h + 1) * 512],
                                                              in0=xres[:, h * 512:(h + 1) * 512], scalar=ALPHA,
                                                              in1=mix_ps[h][:], op0=ALU.mult, op1=ALU.add),
                 reads=[R_x, R_mix[h]], writes=[R_s])
        nx = i % len(ep["xn"])
        xn, R_xn = ep["xn"][nx], ep["R_xn"][nx]
        self.ln_tile(ep["ln"], s[:], R_s, ep["g"][:], ep["b"][:], xn[:], R_xn, ep["R_p"])
        S.dma("sp", lambda g: g.dma_start(out=self.x1_d[i * 128:(i + 1) * 128, :], in_=xn[:]), reads=[R_xn])

    def stage_mix_even(self, li, l, x_src):
        S = self.S
        ei = sum(1 for q in self.layers[:li] if q % 2 == 0)
        with ExitStack() as es:
            self.alloc_eps(es)
            w_in = self.sb(es, "ev_win", [128, DC, 1536], BF16)
            w_out = self.sb(es, "ev_wout", [128, DC, D], BF16)
            pw = self.sb(es, "ev_pw", [128, 4, 128], BF16)
            fm = self.sb(es, "ev_fm", [128, EV_FM], F32)
            diag = self.sb(es, "ev_diag", [128, 4, 31, 128], BF16)
            R_w = Res("ev_w")
            R_diag = Res("diag")
            src = self.ev_w_in[ei].rearrange("(c p) n -> p c n", p=128)
            for c0 in range(0, DC, 4):
                S.dma("pool", lambda g, c0=c0: g.dma_start(out=w_in[:, c0:c0 + 4, :], in_=src[:, c0:c0 + 4, :]), writes=[R_w])
            src2 = self.ev_w_out[ei].rearrange("(c p) n -> p c n", p=128)
            for c0 in range(0, DC, 4):
                S.dma("pool", lambda g, c0=c0: g.dma_start(out=w_out[:, c0:c0 + 4, :], in_=src2[:, c0:c0 + 4, :]), writes=[R_w])
            S.dma("pool", lambda g: g.dma_start(out=pw[:], in_=self.pool_w[ei].rearrange("g c d -> c g d")), writes=[R_w])
            self.load("sp", fm[:], self.fm_ev[ei], R_w)
            b_in = fm[:, 0:12]
            pool_b = fm[:, 12:16]
            pool_s = fm[:, 16:20]
            conv_b = fm[:, 20:24]
            cln_g = fm[:, 24:28]
            cln_b = fm[:, 28:32]
            conv_w = fm[:, 32:156]
            for j in range(4):
                for k in range(31):
                    S.op("dve", lambda v, j=j, k=k: v.tensor_scalar(out=diag[:, j, k, :], in0=self.ident_b,
                                                                    scalar1=conv_w[:, j * 31 + k:j * 31 + k + 1],
                                                                    scalar2=None, op0=ALU.mult),
                         reads=[R_w, self.R_const], writes=[R_diag])
            ep = self.alloc_epilogue(es, li)
            xTb = [self.sb(es, "ev_xT", [128, DC, 512], BF16) for _ in range(2)]
            R_xTb = [Res("xTb") for _ in range(2)]
            pbuf = [self.sb(es, "ev_p", [128, 4, 528], F32) for _ in range(2)]
            R_pbuf = [Res("pbuf") for _ in range(2)]
            R_phalo = [Res("phalo") for _ in range(2)]
            glu = [self.sb(es, "ev_glu", [128, 4, 544], BF16) for _ in range(2)]
            R_glu = [Res("glu") for _ in range(2)]
            R_ghalo = [Res("ghalo") for _ in range(2)]
            tA = self.sb(es, "ev_tA", [128, 528], F32)
            tB = self.sb(es, "ev_tB", [128, 528], F32)
            R_tA, R_tB = Res("tA"), Res("tB")
            t16 = self.sb(es, "ev_t16", [128, 16], F32)
            R_t16 = Res("t16")
            pooled = self.sb(es, "ev_pooled", [128, 4, 512], BF16)
            R_pooled = [Res("pooled") for _ in range(4)]
            ymix = self.sb(es, "ev_ymix", [128, 8, 512], BF16)
            R_ymix = [Res("ymix") for _ in range(8)]
            sg = [self.sb(es, "ev_sg", [128, 512], F32) for _ in range(2)]
            R_sg = [Res("sg") for _ in range(2)]
            yc = self.sb(es, "ev_yc", [128, 4, 512], F32)
            ycb = self.sb(es, "ev_ycb", [128, 4, 512], BF16)
            ysq = self.sb(es, "ev_ysq", [128, 4, 512], BF16)
            R_yc = [Res("yc") for _ in range(4)]
            mean = self.sb(es, "ev_mean", [128, 512], F32)
            msq = self.sb(es, "ev_msq", [128, 512], F32)
            rstd = self.sb(es, "ev_rstd", [128, 512], F32)
            R_stat = Res("stat")
            t1 = [self.sb(es, "ev_t1", [128, 512], F32) for _ in range(2)]
            R_t1 = [Res("t1") for _ in range(2)]
            A = [self.ps(es, "ev_A", [128, 512], F32) for _ in range(4)]
            R_A = [Res("A") for _ in range(4)]
            S1 = self.ps(es, "ev_S1", [128, 512], F32)
            S2 = self.ps(es, "ev_S2", [128, 512], F32)
            R_S1, R_S2 = Res("S1"), Res("S2")
            M = [self.ps(es, "ev_M", [128, 512], F32) for _ in range(2)]
            R_M = [Res("M") for _ in range(2)]
            rot = [0]

            def nextA():
                k = rot[0] % 4
                rot[0] += 1
                return A[k], R_A[k]

            S.op("dve", lambda v: v.memset(pbuf[0][:, :, 0:16], 0.0), writes=[R_phalo[0]])
            S.op("dve", lambda v: v.memset(glu[0][:, :, 0:32], 0.0), writes=[R_ghalo[0]])
            xT_src = self.xT_d.rearrange("(c p) t -> p c t", p=128)
            self.load("sp", xTb[0][:], xT_src[:, :, 0:512], R_xTb[0])
            self.prefetch_xres(ep, x_src, 0)
            for tb in range(4):
                cb = tb % 2
                if tb + 1 < 4:
                    self.load("sp", xTb[1 - cb][:], xT_src[:, :, (tb + 1) * 512:(tb + 2) * 512], R_xTb[1 - cb])
                xt = xTb[cb]
                for j in range(4):
                    ps_, R_ps = nextA()
                    for c in range(DC):
                        S.op("pe", lambda p, c=c, j=j, ps_=ps_: p.matmul(ps_[:], lhsT=w_in[:, c, j * 128:(j + 1) * 128],
                                                                           rhs=xt[:, c, :], start=(c == 0), stop=(c == DC - 1)),
                             reads=[R_w, R_xTb[cb]], writes=[R_ps])
                    S.op("act", lambda a, j=j, ps_=ps_: a.activation(out=pbuf[cb][:, j, 16:528], in_=ps_[:], func=AF.Identity,
                                                                     bias=b_in[:, j:j + 1], scale=1.0),
                         reads=[R_ps, R_w], writes=[R_pbuf[cb]])
                for j in range(4):
                    psa, R_psa = nextA()
                    psg, R_psg = nextA()
                    for c in range(DC):
                        S.op("pe", lambda p, c=c, j=j, psa=psa: p.matmul(psa[:], lhsT=w_in[:, c, (4 + j) * 128:(5 + j) * 128],
                                                                           rhs=xt[:, c, :], start=(c == 0), stop=(c == DC - 1)),
                             reads=[R_w, R_xTb[cb]], writes=[R_psa])
                    for c in range(DC):
                        S.op("pe", lambda p, c=c, j=j, psg=psg: p.matmul(psg[:], lhsT=w_in[:, c, (8 + j) * 128:(9 + j) * 128],
                                                                           rhs=xt[:, c, :], start=(c == 0), stop=(c == DC - 1)),
                             reads=[R_w, R_xTb[cb]], writes=[R_psg])
                    sgb = sg[j % 2]
                    S.op("act", lambda a, j=j, psg=psg, sgb=sgb: a.activation(out=sgb[:], in_=psg[:], func=AF.Sigmoid,
                                                                               bias=b_in[:, 8 + j:9 + j], scale=1.0),
                         reads=[R_psg, R_w], writes=[R_sg[j % 2]])
                    S.op("dve", lambda v, j=j, psa=psa, sgb=sgb: v.scalar_tensor_tensor(
                        out=glu[cb][:, j, 32:544], in0=psa[:], scalar=b_in[:, 4 + j:5 + j], in1=sgb[:],
                        op0=ALU.add, op1=ALU.mult), reads=[R_psa, R_sg[j % 2], R_w], writes=[R_glu[cb]])
                for g in range(4):
                    win = 2 ** (g + 1)
                    cur, R_cur = pbuf[cb][:, g, :], None
                    bufs = [(tA, R_tA), (tB, R_tB)]
                    for s_ in range(g + 1):
                        sh = 2 ** s_
                        nxt, R_nxt = bufs[s_ % 2]
                        rd = [R_pbuf[cb], R_phalo[cb]] if R_cur is None else [R_cur]
                        S.op("dve", lambda v, cur=cur, nxt=nxt, sh=sh: v.tensor_tensor(
                            out=nxt[:, sh:528], in0=cur[:, sh:528], in1=cur[:, 0:528 - sh], op=ALU.add),
                            reads=rd, writes=[R_nxt])
                        cur, R_cur = nxt[:, :], R_nxt
                    S.op("dve", lambda v, cur=cur, g=g, win=win: v.scalar_tensor_tensor(
                        out=pooled[:, g, :], in0=cur[:, 16:528], scalar=1.0 / win, in1=pbuf[cb][:, g, 16:528],
                        op0=ALU.mult, op1=ALU.subtract), reads=[R_cur, R_pbuf[cb]], writes=[R_pooled[g]])
                    if tb == 0:
                        S.op("dve", lambda v, cur=cur, g=g: v.tensor_tensor(out=t16[:], in0=cur[:, 16:32],
                                                                           in1=self.invcnt[:, g * 16:(g + 1) * 16], op=ALU.mult),
                             reads=[R_cur, self.R_const], writes=[R_t16])
                        S.op("dve", lambda v, g=g: v.tensor_tensor(out=pooled[:, g, 0:16], in0=t16[:],
                                                                   in1=pbuf[cb][:, g, 16:32], op=ALU.subtract),
                             reads=[R_t16, R_pbuf[cb]], writes=[R_pooled[g]])
                if tb + 1 < 4:
                    S.op("act", lambda a: a.activation(out=pbuf[1 - cb][:, :, 0:16], in_=pbuf[cb][:, :, 512:528], func=AF.Identity),
                         reads=[R_pbuf[cb]], writes=[R_phalo[1 - cb]])
                    S.op("act", lambda a: a.activation(out=glu[1 - cb][:, :, 0:32], in_=glu[cb][:, :, 512:544], func=AF.Identity),
                         reads=[R_glu[cb]], writes=[R_ghalo[1 - cb]])
                for g in range(4):
                    ps_, R_ps = nextA()
                    S.op("pe", lambda p, g=g, ps_=ps_: p.matmul(ps_[:], lhsT=pw[:, g, :], rhs=pooled[:, g, :], start=True, stop=True),
                         reads=[R_w, R_pooled[g]], writes=[R_ps])
                    S.op("dve", lambda v, g=g, ps_=ps_: v.tensor_scalar(out=ymix[:, g, :], in0=ps_[:], scalar1=pool_b[:, g:g + 1],
                                                                        scalar2=pool_s[:, g:g + 1], op0=ALU.add, op1=ALU.mult),
                         reads=[R_ps, R_w], writes=[R_ymix[g]])
                for j in range(4):
                    ps_, R_ps = nextA()
                    for k in range(31):
                        S.op("pe", lambda p, j=j, k=k, ps_=ps_: p.matmul(ps_[:], lhsT=diag[:, j, k, :],
                                                                           rhs=glu[cb][:, j, 2 + k:2 + k + 512],
                                                                           start=(k == 0), stop=(k == 30)),
                             reads=[R_diag, R_glu[cb], R_ghalo[cb]], writes=[R_ps])
                    S.op("act", lambda a, j=j, ps_=ps_: a.activation(out=yc[:, j, :], in_=ps_[:], func=AF.Identity,
                                                                     bias=conv_b[:, j:j + 1], scale=1.0),
                         reads=[R_ps, R_w], writes=[R_yc[j]])
                    S.op("act", lambda a, j=j, ps_=ps_: a.activation(out=ysq[:, j, :], in_=ps_[:], func=AF.Square,
                                                                     bias=conv_b[:, j:j + 1], scale=1.0),
                         reads=[R_ps, R_w], writes=[R_yc[j]])
                    S.op("dve", lambda v, j=j: v.tensor_copy(out=ycb[:, j, :], in_=yc[:, j, :]), reads=[R_yc[j]], writes=[R_yc[j]])
                for j in range(4):
                    S.op("pe", lambda p, j=j: p.matmul(S1[:], lhsT=self.ones_b, rhs=ycb[:, j, :], start=(j == 0), stop=(j == 3)),
                         reads=[R_yc[j], self.R_const], writes=[R_S1])
                for j in range(4):
                    S.op("pe", lambda p, j=j: p.matmul(S2[:], lhsT=self.ones_b, rhs=ysq[:, j, :], start=(j == 0), stop=(j == 3)),
                         reads=[R_yc[j], self.R_const], writes=[R_S2])
                S.op("act", lambda a: a.activation(out=mean[:], in_=S1[:], func=AF.Identity, scale=1.0 / 512), reads=[R_S1], writes=[R_stat])
                S.op("act", lambda a: a.activation(out=msq[:], in_=S1[:], func=AF.Square, scale=1.0 / 512), reads=[R_S1], writes=[R_stat])
                S.op("dve", lambda v: v.scalar_tensor_tensor(out=rstd[:], in0=S2[:], scalar=1.0 / 512, in1=msq[:],
                                                             op0=ALU.mult, op1=ALU.subtract), reads=[R_S2, R_stat], writes=[R_stat])
                S.op("act", lambda a: a.activation(out=rstd[:], in_=rstd[:], func=AF.Ln, bias=self.eps_ap, scale=1.0),
                     reads=[self.R_eps], writes=[R_stat])
                S.op("act", lambda a: a.activation(out=rstd[:], in_=rstd[:], func=AF.Exp, scale=-0.5), writes=[R_stat])
                for j in range(4):
                    tt_, R_tt = t1[j % 2], R_t1[j % 2]
                    S.op("dve", lambda v, j=j, tt_=tt_: v.tensor_tensor(out=tt_[:], in0=yc[:, j, :], in1=mean[:], op=ALU.subtract),
                         reads=[R_yc[j], R_stat], writes=[R_tt])
                    S.op("dve", lambda v, tt_=tt_: v.tensor_tensor(out=tt_[:], in0=tt_[:], in1=rstd[:], op=ALU.mult),
                         reads=[R_stat], writes=[R_tt])
                    S.op("act", lambda a, j=j, tt_=tt_: a.activation(out=ymix[:, 4 + j, :], in_=tt_[:], func=AF.Silu,
                                                                     bias=cln_b[:, j:j + 1], scale=cln_g[:, j:j + 1]),
                         reads=[R_tt, R_w], writes=[R_ymix[4 + j]])
                for q in range(4):
                    i = tb * 4 + q
                    if i + 1 < NT:
                        self.prefetch_xres(ep, x_src, i + 1)
                    for h in range(2):
                        S.op("pe", lambda p, h=h: p.matmul(M[h][:], lhsT=self.ones_b, rhs=ep["bpad"][:, h * 512:(h + 1) * 512],
                                                           start=True, stop=False), reads=[ep["R_bp"], self.R_const], writes=[R_M[h]])
                        for e_ in range(8):
                            S.op("pe", lambda p, e_=e_, h=h, q=q: p.matmul(M[h][:], lhsT=ymix[:, e_, q * 128:(q + 1) * 128],
                                                                            rhs=w_out[:, e_, h * 512:(h + 1) * 512],
                                                                            start=False, stop=(e_ == 7)),
                                 reads=[R_ymix[e_], R_w], writes=[R_M[h]])
                    self.mixer_epilogue(ep, i, M, R_M)
            S.barrier()

    def stage_mix_odd(self, li, l, x_src):
        S = self.S
        oi = sum(1 for q in self.layers[:li] if q % 2 == 1)
        with ExitStack() as es:
            self.alloc_eps(es)
            w_in = self.sb(es, "od_win", [128, DC, 4096], BF16)
            w_out = self.sb(es, "od_wout", [128, 16, D], BF16)
            wT = self.sb(es, "od_wT", [128, 8, 128], BF16)
            fm = self.sb(es, "od_fm", [128, OD_FM], F32)
            bv = self.sb(es, "od_bv", [128, 2048], F32)
            bs = self.sb(es, "od_bs", [128, 1024], F32)
            Kc = self.sb(es, "od_Kc", [128, 16, 128], F32)
            R_w = Res("od_w")
            R_wT = Res("od_wT")
            R_Kc = Res("Kc")
            src = self.od_w_in[oi].rearrange("(c p) n -> p c n", p=128)
            for n0 in range(0, 4096, 1024):
                S.dma("pool", lambda g, n0=n0: g.dma_start(out=w_in[:, :, n0:n0 + 1024], in_=src[:, :, n0:n0 + 1024]), writes=[R_w])
            src2 = self.od_w_out[oi].rearrange("(c p) n -> p c n", p=128)
            for c0 in range(0, 16, 8):
                S.dma("pool", lambda g, c0=c0: g.dma_start(out=w_out[:, c0:c0 + 8, :], in_=src2[:, c0:c0 + 8, :]), writes=[R_w])
            S.dma("pool", lambda g: g.dma_start(out=wT[:], in_=self.sgu_wT[oi].rearrange("h j i -> j h i")), writes=[R_wT])
            self.load("sp", fm[:], self.fm_od[oi], R_w)
            self.bcast_load(bv[:], self.od_bv[oi], R_w)
            self.bcast_load(bs[:], self.sgu_b[oi], R_w)
            b_u = fm[:, 0:16]
            g_fm = fm[:, 16:32]
            b_fm = fm[:, 32:48]
            S.op("dve", lambda v: v.memset(wT[64:128, :, 0:64], 0.0), writes=[R_wT])
            ep = self.alloc_epilogue(es, li, n_xn=1)
            xTb = [self.sb(es, "od_xT", [128, DC, 256], BF16) for _ in range(2)]
            R_xTb = [Res("xTb") for _ in range(2)]
            u = [self.sb(es, "od_u", [128, 16, 256], BF16) for _ in range(2)]
            R_u = [[Res("u") for _ in range(16)] for _ in range(2)]
            t1 = [self.sb(es, "od_t1", [128, 2048], F32) for _ in range(2)]
            R_t1 = [Res("t1") for _ in range(2)]
            z = [self.sb(es, "od_z", [128, 2048], BF16) for _ in range(2)]
            R_z = [Res("z") for _ in range(2)]
            vst = [self.sb(es, "od_vst", [128, 24], F32) for _ in range(2)]
            vmv = [self.sb(es, "od_vmv", [128, 2], F32) for _ in range(2)]
            vsc = [self.sb(es, "od_vsc", [128, 4], F32) for _ in range(2)]
            R_vst = [Res("vst") for _ in range(2)]
            tmp = [self.sb(es, "od_tmp", [128, 128], F32) for _ in range(2)]
            R_tmp = [Res("tmp") for _ in range(2)]
            PU = [self.ps(es, "od_PU", [128, 512], F32) for _ in range(2)]
            R_PU = [Res("PU") for _ in range(2)]
            PV = [self.ps(es, "od_PV", [128, 512], F32) for _ in range(2)]
            R_PV = [Res("PV") for _ in range(2)]
            PR = [self.ps(es, "od_PR", [128, 512], F32) for _ in range(2)]
            R_PR = [Res("PR") for _ in range(2)]
            M = [self.ps(es, "od_M", [128, 512], F32) for _ in range(2)]
            R_M = [Res("M") for _ in range(2)]
            for hh in range(2):
                S.op("pe", lambda p, hh=hh: p.matmul(PR[hh][:], lhsT=self.ones_b,
                                                     rhs=wT[:, hh * 4:(hh + 1) * 4, :],
                                                     start=True, stop=True), reads=[R_wT, self.R_const], writes=[R_PR[hh]])
            for blk in range(16):
                h = blk // 2
                S.op("dve", lambda v, blk=blk, h=h: v.scalar_tensor_tensor(
                    out=Kc[:, blk, :], in0=PR[h // 4][:, (h % 4) * 128:(h % 4 + 1) * 128], scalar=b_fm[:, blk:blk + 1],
                    in1=bs[:, h * 128:(h + 1) * 128], op0=ALU.mult, op1=ALU.add),
                    reads=[R_PR[h // 4], R_w], writes=[R_Kc])
            xT_src = self.xT_d.rearrange("(c p) t -> p c t", p=128)
            self.load("sp", xTb[0][:], xT_src[:, :, 0:256], R_xTb[0])
            self.prefetch_xres(ep, x_src, 0)
            rot = {"u": 0, "v": 0}

            def U(b):
                cb = b % 2
                xt, ub = xTb[cb], u[cb]
                for ec in range(16):
                    ps_, R_ps = PU[rot["u"] % 2], R_PU[rot["u"] % 2]
                    rot["u"] += 1
                    for c in range(DC):
                        S.op("pe", lambda p, c=c, ec=ec, ps_=ps_: p.matmul(ps_[:, 0:256], lhsT=w_in[:, c, ec * 128:(ec + 1) * 128],
                                                                             rhs=xt[:, c, :], start=(c == 0), stop=(c == DC - 1)),
                             reads=[R_w, R_xTb[cb]], writes=[R_ps])
                    S.op("act", lambda a, ec=ec, ps_=ps_: a.activation(out=ub[:, ec, :], in_=ps_[:, 0:256], func=AF.Gelu,
                                                                       bias=b_u[:, ec:ec + 1], scale=1.0),
                         reads=[R_ps, R_w], writes=[R_u[cb][ec]])

            def A(i):
                b, q = divmod(i, 2)
                cb = b % 2
                xt = xTb[cb]
                pi = i % 2
                zb, R_zb = z[pi], R_z[pi]
                t1b, R_t1b = t1[pi], R_t1[pi]
                vst_, vmv_, vsc_, R_vs = vst[pi], vmv[pi], vsc[pi], R_vst[pi]
                for vb in range(4):
                    ps_, R_ps = PV[rot["v"] % 2], R_PV[rot["v"] % 2]
                    rot["v"] += 1
                    for c in range(DC):
                        S.op("pe", lambda p, c=c, vb=vb, ps_=ps_, q=q: p.matmul(
                            ps_[:], lhsT=xt[:, c, q * 128:(q + 1) * 128],
                            rhs=w_in[:, c, 2048 + vb * 512:2048 + (vb + 1) * 512],
                            start=(c == 0), stop=(c == DC - 1)), reads=[R_w, R_xTb[cb]], writes=[R_ps])
                    S.op("dve", lambda v, vb=vb, ps_=ps_: v.tensor_tensor(out=t1b[:, vb * 512:(vb + 1) * 512], in0=ps_[:],
                                                                          in1=bv[:, vb * 512:(vb + 1) * 512], op=ALU.add),
                         reads=[R_ps, R_w], writes=[R_t1b])
                S.op("act", lambda a: a.activation(out=t1b[:], in_=t1b[:], func=AF.Gelu), writes=[R_t1b])
                for vb in range(4):
                    S.op("dve", lambda v, vb=vb: v.bn_stats(vst_[:, vb * 6:(vb + 1) * 6], t1b[:, vb * 512:(vb + 1) * 512]),
                         reads=[R_t1b], writes=[R_vs])
                S.op("dve", lambda v: v.bn_aggr(vmv_[:, 0:2], vst_[:, 0:24]), writes=[R_vs])
                S.op("act", lambda a: a.activation(out=vsc_[:, 0:1], in_=vmv_[:, 1:2], func=AF.Sqrt, bias=self.eps_ap, scale=1.0),
                     reads=[self.R_eps], writes=[R_vs])
                S.op("dve", lambda v: v.reciprocal(vsc_[:, 1:2], vsc_[:, 0:1]), writes=[R_vs])
                S.op("dve", lambda v: v.tensor_scalar(out=vsc_[:, 2:3], in0=vmv_[:, 0:1], scalar1=vsc_[:, 1:2], scalar2=-1.0,
                                                      op0=ALU.mult, op1=ALU.mult), writes=[R_vs])
                S.op("act", lambda a: a.activation(out=zb[:], in_=t1b[:], func=AF.Identity, bias=vsc_[:, 2:3], scale=vsc_[:, 1:2]),
                     reads=[R_t1b, R_vs], writes=[R_zb])

            def B(i):
                b, q = divmod(i, 2)
                cb = b % 2
                ub = u[cb]
                zb, R_zb = z[i % 2], R_z[i % 2]
                if i + 1 < NT:
                    self.prefetch_xres(ep, x_src, i + 1)
                for blk in range(16):
                    pr, R_pr = PR[(blk // 4) % 2], R_PR[(blk // 4) % 2]
                    S.op("pe", lambda p, blk=blk, pr=pr: p.matmul(pr[:, (blk % 4) * 128:(blk % 4 + 1) * 128],
                                                                  lhsT=zb[:, blk * 128:(blk + 1) * 128],
                                                                  rhs=wT[:, blk // 2, :], start=True, stop=True),
                         reads=[R_zb, R_wT], writes=[R_pr])
                    if blk % 4 == 3:
                        for b2 in range(blk - 3, blk + 1):
                            tm, R_tm = tmp[b2 % 2], R_tmp[b2 % 2]
                            S.op("dve", lambda v, b2=b2, pr=pr, tm=tm: v.scalar_tensor_tensor(
                                out=tm[:], in0=pr[:, (b2 % 4) * 128:(b2 % 4 + 1) * 128], scalar=g_fm[:, b2:b2 + 1],
                                in1=Kc[:, b2, :], op0=ALU.mult, op1=ALU.add), reads=[R_pr, R_Kc, R_w], writes=[R_tm])
                            S.op("dve", lambda v, b2=b2, tm=tm: v.tensor_tensor(
                                out=ub[:, b2, q * 128:(q + 1) * 128], in0=tm[:], in1=ub[:, b2, q * 128:(q + 1) * 128],
                                op=ALU.mult), reads=[R_tm], writes=[R_u[cb][b2]])
                for h in range(2):
                    S.op("pe", lambda p, h=h: p.matmul(M[h][:], lhsT=self.ones_b, rhs=ep["bpad"][:, h * 512:(h + 1) * 512],
                                                       start=True, stop=False), reads=[ep["R_bp"], self.R_const], writes=[R_M[h]])
                    for ec in range(16):
                        S.op("pe", lambda p, ec=ec, h=h: p.matmul(M[h][:], lhsT=ub[:, ec, q * 128:(q + 1) * 128],
                                                                  rhs=w_out[:, ec, h * 512:(h + 1) * 512],
                                                                  start=False, stop=(ec == 15)),
                             reads=[R_u[cb][ec], R_w], writes=[R_M[h]])
                self.mixer_epilogue(ep, i, M, R_M)

            for i in range(NT):
                b, q = divmod(i, 2)
                if q == 0:
                    if b + 1 < 8:
                        nb_ = (b + 1) % 2
                        self.load("sp", xTb[nb_][:], xT_src[:, :, (b + 1) * 256:(b + 2) * 256], R_xTb[nb_])
                    U(b)
                A(i)
                if i >= 1:
                    B(i - 1)
            B(NT - 1)
            S.barrier()

    def router_body(self, li):
        S = self.S
        ne = self.ne
        with ExitStack() as es:
            rw = self.sb(es, "rw", [128, DC, ne], F32)
            rb = self.sb(es, "rb", [128, ne], F32)
            R_p = Res("rp")
            self.load("sp", rw[:], self.router_w[li].rearrange("(c p) e -> p c e", p=128), R_p)
            self.bcast_load(rb[:], self.router_b[li], R_p)
            xn = [self.sb(es, "r_xn", [128, D], F32) for _ in range(2)]
            R_xn = [Res("xn") for _ in range(2)]
            xb = [self.sb(es, "r_xb", [128, D], BF16) for _ in range(2)]
            R_xb = [Res("xb") for _ in range(2)]
            xTf = self.sb(es, "r_xTf", [128, DC, 128], F32)
            R_xTf = [Res("xTf") for _ in range(2)]
            lg = self.sb(es, "r_lg", [128, ne], F32)
            mx = self.sb(es, "r_mx", [128, 8], F32)
            ix = self.sb(es, "r_ix", [128, 8], U32)
            ixf = self.sb(es, "r_ixf", [128, 8], F32)
            sm = self.sb(es, "r_sm", [128, 8], F32)
            e4 = self.sb(es, "r_e4", [128, 4], F32)
            mask = self.sb(es, "r_mask", [128, ne], BF16)
            cm = self.sb(es, "r_cm", [128, ne], BF16)
            slotfull = self.sb(es, "r_sf", [128, ne], F32)
            ovf = self.sb(es, "r_ovf", [128, ne], F32)
            oh4 = self.sb(es, "r_oh4", [128, TOPK, ne], F32)
            junk = self.sb(es, "r_junk", [128, ne], F32)
            slotf = self.sb(es, "r_slotf", [128, 4], F32)
            G = self.sb(es, "r_G", [128, ne], F32)
            R_t = Res("rt")
            R_cm = Res("cm")
            PT = [self.ps(es, "r_PT", [128, 512], F32) for _ in range(2)]
            R_PT = [Res("PT") for _ in range(2)]
            PL = self.ps(es, "r_PL", [128, ne], F32)
            PP = self.ps(es, "r_PP", [128, ne], F32)
            PG = self.ps(es, "r_PG", [32, 128], F32)
            R_PL, R_PP, R_PG = Res("PL"), Res("PP"), Res("PG")
            S.op("dve", lambda v: v.memset(cm[:], 0.0), writes=[R_cm])
            self.load("sp", xn[0][:], self.x1_d[0:128, :], R_xn[0])
            scat_toks = []
            for i in range(NT):
                b = i % 2
                if i + 1 < NT:
                    self.load("sp", xn[1 - b][:], self.x1_d[(i + 1) * 128:(i + 2) * 128, :], R_xn[1 - b])
                S.op("act", lambda a, b=b: a.activation(out=xb[b][:], in_=xn[b][:], func=AF.Identity), reads=[R_xn[b]], writes=[R_xb[b]])
                for half in range(2):
                    for cc in range(4):
                        c = half * 4 + cc
                        S.op("pe", lambda p, c=c, cc=cc, half=half, b=b: p.transpose(
                            out=PT[half][:, cc * 128:(cc + 1) * 128], in_=xn[b][:, c * 128:(c + 1) * 128], identity=self.ident_f),
                            reads=[R_xn[b], self.R_const], writes=[R_PT[half]])
                    S.op("dve", lambda v, half=half: v.tensor_copy(out=xTf[:, half * 4:(half + 1) * 4, :],
                                                                    in_=PT[half][:].rearrange("p (c t) -> p c t", c=4)),
                         reads=[R_PT[half]], writes=[R_xTf[half]])
                for c in range(DC):
                    S.op("pe", lambda p, c=c: p.matmul(PL[:], lhsT=xTf[:, c, :], rhs=rw[:, c, :], start=(c == 0), stop=(c == DC - 1)),
                         reads=[R_xTf[c // 4], R_p], writes=[R_PL])
                S.op("dve", lambda v: v.tensor_tensor(out=lg[:], in0=PL[:], in1=rb[:], op=ALU.add), reads=[R_PL, R_p], writes=[R_t])
                S.op("dve", lambda v: v.max(out=mx[:], in_=lg[:]), writes=[R_t])
                S.op("dve", lambda v: v.max_index(out=ix[:], in_max=mx[:], in_values=lg[:]), writes=[R_t])
                S.op("dve", lambda v: v.tensor_scalar(out=sm[:, 0:1], in0=mx[:, 0:1], scalar1=-1.0, scalar2=None, op0=ALU.mult), writes=[R_t])
                S.op("act", lambda a: a.activation(out=e4[:], in_=mx[:, 0:4], func=AF.Exp, bias=sm[:, 0:1], scale=1.0,
                                                   accum_out=sm[:, 1:2]), writes=[R_t])
                S.op("dve", lambda v: v.reciprocal(sm[:, 2:3], sm[:, 1:2]), writes=[R_t])
                S.op("dve", lambda v, i=i: v.tensor_scalar(out=self.gates_all[:, i, :], in0=e4[:], scalar1=sm[:, 2:3], scalar2=None,
                                                           op0=ALU.mult), reads=[R_t], writes=[self.R_route])
                S.op("dve", lambda v: v.tensor_scalar(out=mask[:], in0=lg[:], scalar1=mx[:, 3:4], scalar2=None, op0=ALU.is_ge), writes=[R_t])
                S.op("pe", lambda p: p.matmul(PP[:], lhsT=self.ones_b, rhs=cm[:], start=True, stop=False),
                     reads=[R_cm, self.R_const], writes=[R_PP])
                S.op("pe", lambda p: p.matmul(PP[:], lhsT=self.tri_b, rhs=mask[:], start=False, stop=True),
                     reads=[R_t, self.R_const], writes=[R_PP])
                S.op("dve", lambda v: v.tensor_tensor(out=cm[:], in0=cm[:], in1=mask[:], op=ALU.add), reads=[R_t], writes=[R_cm])
                S.op("dve", lambda v: v.tensor_tensor(out=slotfull[:], in0=PP[:], in1=self.slotbase[:, 0:ne], op=ALU.add),
                     reads=[R_PP, self.R_const], writes=[R_t])
                S.op("dve", lambda v: v.tensor_scalar(out=ovf[:], in0=PP[:], scalar1=float(CAP), scalar2=1.0e7, op0=ALU.is_ge, op1=ALU.mult),
                     reads=[R_PP], writes=[R_t])
                S.op("dve", lambda v: v.tensor_tensor(out=slotfull[:], in0=slotfull[:], in1=ovf[:], op=ALU.add), writes=[R_t])
                S.op("dve", lambda v: v.tensor_copy(out=ixf[:], in_=ix[:]), writes=[R_t])
                S.op("dve", lambda v: v.tensor_tensor(out=oh4[:], in0=self.iota_e[:, 0:ne].unsqueeze(1).to_broadcast([128, TOPK, ne]),
                                                      in1=ixf[:, 0:TOPK].unsqueeze(2).to_broadcast([128, TOPK, ne]), op=ALU.is_equal),
                     reads=[self.R_const], writes=[R_t])
                S.op("dve", lambda v: v.tensor_tensor(out=oh4[:], in0=oh4[:], in1=slotfull[:].unsqueeze(1).to_broadcast([128, TOPK, ne]),
                                                      op=ALU.mult), writes=[R_t])
                S.op("dve", lambda v: v.tensor_reduce(out=slotf[:, 0:TOPK], in_=oh4[:], axis=mybir.AxisListType.X, op=ALU.add), writes=[R_t])
                S.op("act", lambda a: a.activation(out=junk[:], in_=lg[:], func=AF.Exp, bias=sm[:, 0:1], scale=1.0), writes=[R_t])
                S.op("dve", lambda v: v.scalar_tensor_tensor(out=G[:], in0=junk[:], scalar=sm[:, 2:3], in1=mask[:], op0=ALU.mult, op1=ALU.mult),
                     writes=[R_t])
                tok_sl = S.op("dve", lambda v, i=i: v.tensor_copy(out=self.slots_all[:, i, :], in_=slotf[:]), reads=[R_t], writes=[self.R_route])
                S.op("pe", lambda p: p.transpose(out=PG[:], in_=G[:], identity=self.ident_f), reads=[R_t, self.R_const], writes=[R_PG])
                S.op("act", lambda a, i=i: a.activation(out=self.GT_all[:, i, :], in_=PG[:], func=AF.Identity), reads=[R_PG], writes=[self.R_route])
                for k in range(TOPK):
                    t_ = S.dma("pool", lambda g, i=i, k=k, b=b: g.indirect_dma_start(
                        out=self.xs_d[:, :], out_offset=bass.IndirectOffsetOnAxis(ap=self.slots_all[:, i, k:k + 1], axis=0),
                        in_=xb[b][:, :], in_offset=None, bounds_check=self.bc_reg, oob_is_err=False),
                        reads=[R_xb[b]], extra=[tok_sl])
                    scat_toks.append(t_)
            S.barrier()

    def stage_experts(self, li):
        S = self.S
        ne = self.ne
        NSLOT = 6
        with ExitStack() as es:
            ring = [self.sb(es, "ring", [128, DC, 1024], BF16) for _ in range(NSLOT)]
            R_ring = [Res("ring") for _ in range(NSLOT)]
            bup = self.sb(es, "bup", [128, ne * 16], F32)
            bl1 = self.sb(es, "bl1", [128, ne * 16], F32)
            R_b = Res("bup")
            self.load("sp", bup[:], self.fm_up[li], R_b)
            S.op("dve", lambda v: v.tensor_scalar(out=bl1[:], in0=bup[:], scalar1=1.0, scalar2=None, op0=ALU.add), reads=[R_b], writes=[R_b])
            slot_of = {}

            def issue_weights(e, parts=("g", "l", "d")):
                srcu = self.w_up[li, e].rearrange("(c p) n -> p c n", p=128)
                srcd = self.w_down[li, e].rearrange("(c p) n -> p c n", p=128)
                srcs = {"g": srcu[:, :, 0:1024], "l": srcu[:, :, 1024:2048], "d": srcd}
                for part in parts:
                    src = srcs[part]
                    s_ = (3 * e + "gld".index(part)) % NSLOT
                    slot_of[(e, part)] = s_
                    S.dma("pool", lambda g, s_=s_, src=src: g.dma_start(out=ring[s_][:], in_=src), writes=[R_ring[s_]])

            issue_weights(0)
            issue_weights(1)
            self.router_body(li)
            rows = [self.sb(es, "rows", [128, NSB, D], BF16) for _ in range(2)]
            R_rows = [Res("rows") for _ in range(2)]
            xsT = [self.sb(es, "xsT", [128, DC, CAP], BF16) for _ in range(2)]
            R_xsT = [Res("xsT") for _ in range(2)]
            act = [self.sb(es, "act", [128, DC, CAP], BF16) for _ in range(2)]
            R_act = [[Res("act") for _ in range(DC)] for _ in range(2)]
            gc = [self.sb(es, "gc", [128, CAP], F32) for _ in range(2)]
            m1 = [self.sb(es, "m1", [128, CAP], F32) for _ in range(2)]
            l2 = [self.sb(es, "l2", [128, CAP], F32) for _ in range(2)]
            R_gc = [Res("gc") for _ in range(2)]
            R_m1 = [Res("m1") for _ in range(2)]
            R_l2 = [Res("l2") for _ in range(2)]
            ysb = [self.sb(es, "ysb", [128, D], F32) for _ in range(3)]
            R_ysb = [Res("ysb") for _ in range(3)]
            PGp = [self.ps(es, "e_PG", [128, 512], F32) for _ in range(2)]
            PLp = [self.ps(es, "e_PL", [128, 512], F32) for _ in range(2)]
            PY = [self.ps(es, "e_PY", [128, 512], F32) for _ in range(2)]
            PTp = [self.ps(es, "e_PT", [128, D], BF16) for _ in range(2)]
            R_PGp = [Res("PG") for _ in range(2)]
            R_PLp = [Res("PL") for _ in range(2)]
            R_PY = [Res("PY") for _ in range(2)]
            R_PTp = [Res("PT") for _ in range(2)]
            def issue_rows(e):
                b = e % 2
                src = self.xs_d[e * CAP:(e + 1) * CAP, :].rearrange("(s p) d -> p s d", p=128)
                self.load("sp", rows[b][:], src, R_rows[b])

            issue_rows(0)
            rp = 0
            ry = 0
            rt = 0
            for e in range(ne):
                b = e % 2
                if e + 1 < ne:
                    issue_rows(e + 1)
                for sb_ in range(NSB):
                    pt, R_pt = PTp[rt % 2], R_PTp[rt % 2]
                    rt += 1
                    for c in range(DC):
                        S.op("pe", lambda p, c=c, sb_=sb_, pt=pt, b=b: p.transpose(out=pt[:, c * 128:(c + 1) * 128],
                                                                                    in_=rows[b][:, sb_, c * 128:(c + 1) * 128],
                                                                                    identity=self.ident_b),
                             reads=[R_rows[b], self.R_const], writes=[R_pt])
                    S.op("act", lambda a, sb_=sb_, pt=pt, b=b: a.activation(out=xsT[b][:, :, sb_ * 128:(sb_ + 1) * 128],
                                                                            in_=pt[:].rearrange("p (c t) -> p c t", c=DC), func=AF.Identity),
                         reads=[R_pt], writes=[R_xsT[b]])
                wg, R_wg = ring[slot_of[(e, "g")]], R_ring[slot_of[(e, "g")]]
                wl, R_wl = ring[slot_of[(e, "l")]], R_ring[slot_of[(e, "l")]]
                wd, R_wd = ring[slot_of[(e, "d")]], R_ring[slot_of[(e, "d")]]
                for fb in range(8):
                    pg, R_pg = PGp[rp % 2], R_PGp[rp % 2]
                    pl, R_pl = PLp[rp % 2], R_PLp[rp % 2]
                    w_ = rp % 2
                    rp += 1
                    for c in range(DC):
                        S.op("pe", lambda p, c=c, fb=fb, pg=pg, wg=wg, b=b: p.matmul(pg[:, 0:CAP], lhsT=wg[:, c, fb * 128:(fb + 1) * 128],
                                                                                      rhs=xsT[b][:, c, :], start=(c == 0), stop=(c == DC - 1)),
                             reads=[R_wg, R_xsT[b]], writes=[R_pg])
                    for c in range(DC):
                        S.op("pe", lambda p, c=c, fb=fb, pl=pl, wl=wl, b=b: p.matmul(pl[:, 0:CAP], lhsT=wl[:, c, fb * 128:(fb + 1) * 128],
                                                                                      rhs=xsT[b][:, c, :], start=(c == 0), stop=(c == DC - 1)),
                             reads=[R_wl, R_xsT[b]], writes=[R_pl])
                    col_g = e * 16 + fb
                    col_l = e * 16 + 8 + fb
                    S.op("dve", lambda v, pg=pg, w_=w_, col_g=col_g: v.tensor_scalar(out=gc[w_][:], in0=pg[:, 0:CAP], scalar1=bup[:, col_g:col_g + 1],
                                                                                     scalar2=LIMIT, op0=ALU.add, op1=ALU.min),
                         reads=[R_pg, R_b], writes=[R_gc[w_]])
                    S.op("act", lambda a, w_=w_: a.activation(out=m1[w_][:], in_=gc[w_][:], func=AF.Sigmoid, scale=SW_ALPHA),
                         reads=[R_gc[w_]], writes=[R_m1[w_]])
                    S.op("dve", lambda v, w_=w_: v.tensor_tensor(out=m1[w_][:], in0=m1[w_][:], in1=gc[w_][:], op=ALU.mult),
                         reads=[R_gc[w_]], writes=[R_m1[w_]])
                    S.op("dve", lambda v, pl=pl, w_=w_, col_l=col_l: v.tensor_scalar(out=l2[w_][:], in0=pl[:, 0:CAP], scalar1=bl1[:, col_l:col_l + 1],
                                                                                     scalar2=1.0 - LIMIT, op0=ALU.add, op1=ALU.max),
                         reads=[R_pl, R_b], writes=[R_l2[w_]])
                    S.op("dve", lambda v, w_=w_, fb=fb, b=b: v.scalar_tensor_tensor(out=act[b][:, fb, :], in0=l2[w_][:], scalar=1.0 + LIMIT,
                                                                                    in1=m1[w_][:], op0=ALU.min, op1=ALU.mult),
                         reads=[R_l2[w_], R_m1[w_]], writes=[R_act[b][fb]])
                if e + 2 < ne:
                    issue_weights(e + 2, ("g", "l"))
                for sb_ in range(NSB):
                    yb, R_yb = ysb[(e * NSB + sb_) % 3], R_ysb[(e * NSB + sb_) % 3]
                    for h in range(2):
                        py, R_py = PY[ry % 2], R_PY[ry % 2]
                        ry += 1
                        for fc in range(DC):
                            S.op("pe", lambda p, fc=fc, sb_=sb_, h=h, py=py, wd=wd, b=b: p.matmul(
                                py[:], lhsT=act[b][:, fc, sb_ * 128:(sb_ + 1) * 128], rhs=wd[:, fc, h * 512:(h + 1) * 512],
                                start=(fc == 0), stop=(fc == DC - 1)), reads=[R_act[b][fc], R_wd], writes=[R_py])
                        S.op("act", lambda a, h=h, py=py, yb=yb: a.activation(out=yb[:, h * 512:(h + 1) * 512], in_=py[:], func=AF.Identity),
                             reads=[R_py], writes=[R_yb])
                    r0 = e * CAP + sb_ * 128
                    S.dma("sp", lambda g, r0=r0, yb=yb: g.dma_start(out=self.ys_d[r0:r0 + 128, :], in_=yb[:]), reads=[R_yb])
                if e + 2 < ne:
                    issue_weights(e + 2, ("d",))
            S.barrier()

    def stage_combine(self, li, dst, make_xT):
        S = self.S
        ne = self.ne
        with ExitStack() as es:
            self.alloc_eps(es)
            bd = self.sb(es, "c_bd", [32, D], BF16)
            g2 = self.sb(es, "c_g2", [128, D], F32)
            b2 = self.sb(es, "c_b2", [128, D], F32)
            R_p = Res("cp")
            S.dma("pool", lambda g: g.dma_start(out=bd[0:ne, :], in_=self.b_down[li]), writes=[R_p])
            self.bcast_load(g2[:], self.ln_g[li, 1], R_p)
            self.bcast_load(b2[:], self.ln_b[li, 1], R_p)
            NB = 3
            x1 = [self.sb(es, "c_x1", [128, D], F32) for _ in range(NB)]
            R_x1 = [Res("x1") for _ in range(NB)]
            yk = [[self.sb(es, "c_yk", [128, D], F32) for _ in range(TOPK)] for _ in range(NB)]
            R_yk = [[Res("yk") for _ in range(TOPK)] for _ in range(NB)]
            acc = self.sb(es, "c_acc", [128, D], F32)
            R_acc = Res("acc")
            xn = [self.sb(es, "c_xn", [128, D], F32) for _ in range(2)]
            R_xn = [Res("xn") for _ in range(2)]
            xb = [self.sb(es, "c_xb", [128, D], BF16) for _ in range(2)]
            R_xb = [Res("xb") for _ in range(2)]
            stage = [self.sb(es, "c_st", [128, DC, 512], BF16) for _ in range(2)]
            R_st = [Res("st") for _ in range(2)]
            ln = self.alloc_ln_bufs(es, "c")
            PB = [self.ps(es, "c_PB", [128, 512], F32) for _ in range(2)]
            R_PB = [Res("PB") for _ in range(2)]
            pst = [self.ps(es, "c_pst", [128, D], BF16) for _ in range(2)]
            R_pst = [Res("pst") for _ in range(2)]

            def prefetch(i):
                b = i % NB
                self.load("sp", x1[b][:], self.x1_d[i * 128:(i + 1) * 128, :], R_x1[b])
                for k in range(TOPK):
                    S.dma("pool", lambda g, i=i, k=k, b=b: g.indirect_dma_start(
                        out=yk[b][k][:, :], out_offset=None, in_=self.ys_d[:, :],
                        in_offset=bass.IndirectOffsetOnAxis(ap=self.slots_all[:, i, k:k + 1], axis=0),
                        bounds_check=self.bc_reg, oob_is_err=False), writes=[R_yk[b][k]], reads=[self.R_route])

            prefetch(0)
            prefetch(1)
            for i in range(NT):
                b = i % 2
                b3 = i % NB
                if i + 2 < NT:
                    prefetch(i + 2)
                for h in range(2):
                    S.op("pe", lambda p, h=h, i=i: p.matmul(PB[h][:], lhsT=self.GT_all[0:ne, i, :], rhs=bd[0:ne, h * 512:(h + 1) * 512],
                                                            start=True, stop=True), reads=[self.R_route, R_p], writes=[R_PB[h]])
                    S.op("dve", lambda v, h=h, b3=b3: v.scalar_tensor_tensor(out=acc[:, h * 512:(h + 1) * 512], in0=x1[b3][:, h * 512:(h + 1) * 512],
                                                                             scalar=ALPHA, in1=PB[h][:], op0=ALU.mult, op1=ALU.add),
                         reads=[R_x1[b3], R_PB[h]], writes=[R_acc])
                for k in range(TOPK):
                    S.op("dve", lambda v, k=k, b3=b3, i=i: v.scalar_tensor_tensor(out=acc[:], in0=yk[b3][k][:], scalar=self.gates_all[:, i, k:k + 1],
                                                                                  in1=acc[:], op0=ALU.mult, op1=ALU.add),
                         reads=[R_yk[b3][k], self.R_route], writes=[R_acc])
                self.ln_tile(ln, acc[:], R_acc, g2[:], b2[:], xn[b][:], R_xn[b], R_p)
                S.dma("sp", lambda g, i=i, b=b: g.dma_start(out=dst[i * 128:(i + 1) * 128, :], in_=xn[b][:]), reads=[R_xn[b]])
                if make_xT:
                    S.op("act", lambda a, b=b: a.activation(out=xb[b][:], in_=xn[b][:], func=AF.Identity), reads=[R_xn[b]], writes=[R_xb[b]])
                    sb_ = (i // 4) % 2
                    self.xT_from_tile(xb[b], R_xb[b], pst[b], R_pst[b], stage[sb_], R_st[sb_], i % 4, eng="act")
                    if i % 4 == 3:
                        self.store_xT_block(stage[sb_], R_st[sb_], i // 4)
            S.barrier()


def make_consts():
    c = np.zeros((128, 128 * 3 + 32 + 32 + 64), np.float32)
    c[:, 0:128] = np.eye(128, dtype=np.float32)
    p = np.arange(128)
    c[:, 128:256] = (p[:, None] < p[None, :]).astype(np.float32)
    c[:, 256:384] = 1.0
    c[:, 384:416] = np.arange(32, dtype=np.float32)[None, :]
    c[:, 416:448] = (np.arange(32, dtype=np.float32) * CAP)[None, :]
    for g in range(4):
        win = 2 ** (g + 1)
        c[:, 448 + g * 16:448 + (g + 1) * 16] = (1.0 / np.minimum(np.arange(16) + 1, win)).astype(np.float32)[None, :]
    return c


def fm(v):
    v = np.asarray(v, np.float32)
    return np.ascontiguousarray(v.reshape(-1, 128).T)


def prepare_shared(inp, layers, ne=NE):
    ev = [l // 2 for l in layers if l % 2 == 0]
    od = [l // 2 for l in layers if l % 2 == 1]
    ev_sel = ev if ev else [0]
    od_sel = od if od else [0]
    f32 = lambda a: np.ascontiguousarray(a, dtype=np.float32)
    out = {}
    out["ev_w_in"] = f32(inp["ev_w_in"][ev_sel])
    out["ev_w_out"] = f32(inp["ev_w_out"][ev_sel])
    out["pool_w"] = f32(inp["pool_w"][ev_sel])
    out["od_w_in"] = f32(inp["od_w_in"][od_sel])
    out["od_w_out"] = f32(inp["od_w_out"][od_sel])
    out["sgu_wT"] = f32(np.transpose(inp["sgu_w"][od_sel], (0, 1, 3, 2)))
    out["od_bv"] = f32(inp["od_b_in"][od_sel][:, 2048:4096])
    out["sgu_b"] = f32(inp["sgu_b"][od_sel].reshape(len(od_sel), 1024))
    out["b_out"] = f32(np.stack([inp["ev_b_out"][l // 2] if l % 2 == 0 else inp["od_b_out"][l // 2] for l in layers]))
    out["ln_g"] = f32(inp["ln_g"][layers])
    out["ln_b"] = f32(inp["ln_b"][layers])
    out["router_w"] = f32(inp["router_w"][layers][:, :, :ne])
    out["router_b"] = f32(inp["router_b"][layers][:, :ne])
    out["exp_w_up"] = f32(inp["exp_w_up"][layers][:, :ne])
    out["exp_w_down"] = f32(inp["exp_w_down"][layers][:, :ne])
    out["exp_b_down"] = f32(inp["exp_b_down"][layers][:, :ne])
    fe = []
    for i in ev_sel:
        cw = np.asarray(inp["conv_w"][i], np.float32)
        cw_fm = np.transpose(cw.reshape(31, 4, 128), (2, 1, 0)).reshape(128, 124)
        fe.append(np.concatenate([fm(inp["ev_b_in"][i]), fm(inp["pool_b"][i].reshape(-1)), fm(inp["pool_scale"][i]),
                                  fm(inp["conv_b"][i]), fm(inp["conv_ln_g"][i]), fm(inp["conv_ln_b"][i]), cw_fm], axis=1))
    out["fm_ev"] = f32(np.stack(fe))
    fo = []
    for i in od_sel:
        fo.append(np.concatenate([fm(inp["od_b_in"][i][:2048]), fm(inp["sgu_ln_g"][i]), fm(inp["sgu_ln_b"][i])], axis=1))
    out["fm_od"] = f32(np.stack(fo))
    fu = []
    for l in layers:
        bu = np.asarray(inp["exp_b_up"][l][:ne], np.float32)
        fu.append(np.transpose(bu.reshape(ne, 16, 128), (2, 0, 1)).reshape(128, ne * 16))
    out["fm_up"] = f32(np.stack(fu))
    out["consts"] = make_consts()
    return out


_PROG_CACHE = {}


def run_layers(inp, layers, cores, ne=NE, debug=False):
    key = (tuple(layers), ne, debug)
    if key not in _PROG_CACHE:
        _PROG_CACHE[key] = Prog(layers, ne, debug).build()
    nc = _PROG_CACHE[key]
    shared = prepare_shared(inp, layers, ne)
    x = np.asarray(inp["x"], np.float32)
    in_maps = []
    for c in cores:
        m = dict(shared)
        m["x"] = np.ascontiguousarray(x[c])
        in_maps.append(m)
    res = run_bass_kernel_spmd(nc, in_maps, core_ids=list(range(len(cores))))
    return res.results


def kernel(**inputs):
    inp = {k: np.asarray(v) for k, v in inputs.items()}
    res = run_layers(inp, list(range(DEPTH)), list(range(8)))
    return np.stack([r["y"] for r in res], axis=0).astype(np.float32)
```
